# Optimizing a Trainium2 kernel written in Bass

```python
import jax, jax.numpy as jnp
from jax import lax
import numpy as np

D_MODEL = 1024
BATCH = 2
SEQ = 8192
DEPTH = 2

MLA_HEADS = 4
QK_NOPE = 128
QK_ROPE = 64
V_HEAD = 128
Q_RANK = 384
KV_RANK = 256
ROPE_THETA = 10000.0
Q_BLOCK = 128
HG_HEADS = 4
HG_KEY = 128
HG_VAL = 128
HG_CHUNK = 64
MLA_WIDTH = MLA_HEADS * V_HEAD
HG_WIDTH = HG_HEADS * HG_VAL
HG_FDIM = HG_HEADS * HG_KEY
MIX_WIDTH = MLA_WIDTH + HG_WIDTH
IN_COLS = Q_RANK + KV_RANK + QK_ROPE + 2 * HG_FDIM + 2 * HG_WIDTH
DENSE_FF = 2816
N_EXPERTS = 8
TOP_K = 2
EXPERT_FF = 3584
N_DENSE = (DEPTH + 1) // 2
N_MOE = DEPTH // 2
EPS = 1e-6
F_FLOOR = 1e-30

kernel_name = "hybrid_mla_hgrn2_moe_block"

F32 = jnp.float32
NEG_BIG = float(np.finfo(np.float32).min)


def _rmsnorm(x, gain):
    xf = x.astype(F32)
    y = xf * lax.rsqrt(jnp.mean(xf * xf, axis=-1, keepdims=True) + EPS)
    return (y * gain.astype(F32)).astype(x.dtype)


def _rope_tables(positions):
    inv = ROPE_THETA ** (-jnp.arange(0, QK_ROPE, 2, dtype=F32) / QK_ROPE)
    ang = positions.astype(F32)[..., None] * inv
    return jnp.cos(ang), jnp.sin(ang)


def _apply_rope(x, cos, sin):
    x1, x2 = jnp.split(x, 2, axis=-1)
    cos = cos.astype(x.dtype)
    sin = sin.astype(x.dtype)
    return jnp.concatenate([x1 * cos - x2 * sin, x1 * sin + x2 * cos], axis=-1)


def _mla(c_q, c_kv, k_pe, positions, q_a_norm, w_q_b, kv_a_norm, w_kv_b):
    B, S, _ = c_q.shape
    q = (_rmsnorm(c_q, q_a_norm) @ w_q_b).reshape(B, S, MLA_HEADS, QK_NOPE + QK_ROPE)
    q_nope, q_pe = q[..., :QK_NOPE], q[..., QK_NOPE:]
    cos, sin = _rope_tables(positions)
    q_pe = _apply_rope(q_pe, cos[:, :, None, :], sin[:, :, None, :])
    k_pe = _apply_rope(k_pe, cos, sin)
    kv = (_rmsnorm(c_kv, kv_a_norm) @ w_kv_b).reshape(B, S, MLA_HEADS, QK_NOPE + V_HEAD)
    k_nope, v = kv[..., :QK_NOPE], kv[..., QK_NOPE:]
    scale = (QK_NOPE + QK_ROPE) ** -0.5
    nb = S // Q_BLOCK
    qn_blk = (q_nope * scale).reshape(B, nb, Q_BLOCK, MLA_HEADS, QK_NOPE).transpose(1, 0, 2, 3, 4)
    qp_blk = (q_pe * scale).reshape(B, nb, Q_BLOCK, MLA_HEADS, QK_ROPE).transpose(1, 0, 2, 3, 4)
    starts = jnp.arange(nb, dtype=jnp.int32) * Q_BLOCK
    key_idx = jnp.arange(S, dtype=jnp.int32)

    def block(args):
        qn, qp, start = args
        s = jnp.einsum('bqhd,bkhd->bhqk', qn, k_nope, preferred_element_type=F32)
        s = s + jnp.einsum('bqhr,bkr->bhqk', qp, k_pe, preferred_element_type=F32)
        q_idx = start + jnp.arange(Q_BLOCK, dtype=jnp.int32)
        s = jnp.where(key_idx[None, :] <= q_idx[:, None], s, NEG_BIG)
        p = jax.nn.softmax(s, axis=-1).astype(v.dtype)
        return jnp.einsum('bhqk,bkhd->bqhd', p, v)

    o = lax.map(block, (qn_blk, qp_blk, starts))
    return o.transpose(1, 0, 2, 3, 4).reshape(B, S, MLA_WIDTH)


def _hgrn2(q_raw, f_raw, i_in, g_raw, lower_bound, out_norm):
    B, S, _ = q_raw.shape
    nc = S // HG_CHUNK
    q = jax.nn.silu(q_raw.astype(F32))
    z = f_raw.astype(F32)
    lb = lower_bound.astype(F32)
    f = lb + (1.0 - lb) * jax.nn.sigmoid(z)
    log_f = jnp.log(jnp.maximum(f, F_FLOOR))
    k = (1.0 - lb) * jax.nn.sigmoid(-z)

    def to_chunks(t, d):
        return t.reshape(B, nc, HG_CHUNK, HG_HEADS, d).transpose(1, 0, 3, 2, 4)

    qc = to_chunks(q, HG_KEY)
    kc = to_chunks(k, HG_KEY)
    lfc = to_chunks(log_f, HG_KEY)
    vc = to_chunks(i_in.astype(F32), HG_VAL)
    tri = jnp.tril(jnp.ones((HG_CHUNK, HG_CHUNK), dtype=bool))[:, :, None]

    def step(state, inp):
        qb, kb, vb, lfb = inp
        b = jnp.cumsum(lfb, axis=2)
        diff = b[:, :, :, None, :] - b[:, :, None, :, :]
        decay = jnp.where(tri, jnp.exp(jnp.where(tri, diff, 0.0)), 0.0)
        attn = jnp.einsum('bhtd,bhsd,bhtsd->bhts', qb, kb, decay)
        o = jnp.einsum('bhts,bhsv->bhtv', attn, vb) + jnp.einsum('bhtd,bhdv->bhtv', qb * jnp.exp(b), state)
        b_last = b[:, :, -1:, :]
        new_state = jnp.exp(b_last[:, :, 0, :])[..., None] * state + jnp.einsum('bhsd,bhsv->bhdv', kb * jnp.exp(b_last - b), vb)
        return new_state, o

    s0 = jnp.zeros((B, HG_HEADS, HG_KEY, HG_VAL), F32)
    _, o = lax.scan(step, s0, (qc, kc, vc, lfc))
    o = o.transpose(1, 0, 3, 2, 4).reshape(B, S, HG_HEADS, HG_VAL)
    o = _rmsnorm(o, out_norm) * jax.nn.silu(g_raw.astype(F32).reshape(B, S, HG_HEADS, HG_VAL))
    return o.reshape(B, S, HG_WIDTH)


def _swiglu(h, w_gate, w_up, w_down):
    return (jax.nn.silu(h @ w_gate) * (h @ w_up)) @ w_down


def _moe(h, w_router, w_gate, w_up, w_down):
    B, S, D = h.shape
    T = B * S
    xf = h.reshape(T, D)
    logits = jnp.einsum('td,de->te', xf, w_router, preferred_element_type=F32)
    top_logits, top_idx = lax.top_k(logits, TOP_K)
    gates = jax.nn.softmax(top_logits, axis=-1)
    flat_e = top_idx.reshape(-1)
    flat_tok = jnp.repeat(jnp.arange(T, dtype=jnp.int32), TOP_K)
    order = jnp.argsort(flat_e)
    tok_sorted = flat_tok[order]
    xs = xf[tok_sorted]
    group_sizes = jnp.bincount(flat_e, length=N_EXPERTS).astype(jnp.int32)
    hid = jax.nn.silu(lax.ragged_dot(xs, w_gate, group_sizes)) * lax.ragged_dot(xs, w_up, group_sizes)
    ys = lax.ragged_dot(hid, w_down, group_sizes)
    ys = ys * gates.reshape(-1)[order][:, None].astype(ys.dtype)
    out = jnp.zeros_like(xf).at[tok_sorted].add(ys)
    return out.reshape(B, S, D)


def _normal(key, shape, fan_in):
    return jax.random.normal(key, shape, F32) * (fan_in ** -0.5)


def _gain(key, shape):
    return 1.0 + 0.02 * jax.random.normal(key, shape, F32)


def setup_inputs(seed: int = 0) -> dict:
    key = jax.random.key(seed)
    ks = jax.random.split(key, 24)
    x = jax.random.normal(ks[0], (BATCH, SEQ, D_MODEL), F32)
    offset = jax.random.randint(ks[1], (BATCH, 1), 0, 4096, dtype=jnp.int32)
    positions = offset + jnp.arange(SEQ, dtype=jnp.int32)[None, :]
    return {
        "x": x,
        "positions": positions,
        "mix_norm": _gain(ks[2], (DEPTH, D_MODEL)),
        "w_in": _normal(ks[3], (DEPTH, D_MODEL, IN_COLS), D_MODEL),
        "q_a_norm": _gain(ks[4], (DEPTH, Q_RANK)),
        "w_q_b": _normal(ks[5], (DEPTH, Q_RANK, MLA_HEADS * (QK_NOPE + QK_ROPE)), Q_RANK),
        "kv_a_norm": _gain(ks[6], (DEPTH, KV_RANK)),
        "w_kv_b": _normal(ks[7], (DEPTH, KV_RANK, MLA_HEADS * (QK_NOPE + V_HEAD)), KV_RANK),
        "hg_lower_bounds": 0.5 * jax.random.normal(ks[8], (DEPTH, HG_FDIM), F32),
        "hg_out_norm": _gain(ks[9], (DEPTH, HG_VAL)),
        "w_out": _normal(ks[10], (DEPTH, MIX_WIDTH, D_MODEL), MIX_WIDTH),
        "ffn_norm": _gain(ks[11], (DEPTH, D_MODEL)),
        "dense_w_gate": _normal(ks[12], (N_DENSE, D_MODEL, DENSE_FF), D_MODEL),
        "dense_w_up": _normal(ks[13], (N_DENSE, D_MODEL, DENSE_FF), D_MODEL),
        "dense_w_down": _normal(ks[14], (N_DENSE, DENSE_FF, D_MODEL), DENSE_FF),
        "moe_router": _normal(ks[15], (N_MOE, D_MODEL, N_EXPERTS), D_MODEL),
        "moe_w_gate": _normal(ks[16], (N_MOE, N_EXPERTS, D_MODEL, EXPERT_FF), D_MODEL),
        "moe_w_up": _normal(ks[17], (N_MOE, N_EXPERTS, D_MODEL, EXPERT_FF), D_MODEL),
        "moe_w_down": _normal(ks[18], (N_MOE, N_EXPERTS, EXPERT_FF, D_MODEL), EXPERT_FF),
        "final_norm": _gain(ks[19], (D_MODEL,)),
    }


def reference(x, positions, mix_norm, w_in, q_a_norm, w_q_b, kv_a_norm, w_kv_b, hg_lower_bounds, hg_out_norm, w_out, ffn_norm, dense_w_gate, dense_w_up, dense_w_down, moe_router, moe_w_gate, moe_w_up, moe_w_down, final_norm):
    lb_p = jax.nn.softmax(hg_lower_bounds.astype(F32), axis=0)
    lower = jnp.cumsum(lb_p, axis=0) - lb_p[0:1]
    splits = np.cumsum([Q_RANK, KV_RANK, QK_ROPE, HG_FDIM, HG_FDIM, HG_WIDTH]).tolist()
    for l in range(DEPTH):
        h = _rmsnorm(x, mix_norm[l])
        proj = h @ w_in[l]
        c_q, c_kv, k_pe, hq, hf, hi, hg = jnp.split(proj, splits, axis=-1)
        a = _mla(c_q, c_kv, k_pe, positions, q_a_norm[l], w_q_b[l], kv_a_norm[l], w_kv_b[l])
        r = _hgrn2(hq, hf, hi, hg, lower[l], hg_out_norm[l]).astype(x.dtype)
        x = x + jnp.concatenate([a, r], axis=-1) @ w_out[l]
        h = _rmsnorm(x, ffn_norm[l])
        if l % 2 == 0:
            j = l // 2
            x = x + _swiglu(h, dense_w_gate[j], dense_w_up[j], dense_w_down[j])
        else:
            j = l // 2
            x = x + _moe(h, moe_router[j], moe_w_gate[j], moe_w_up[j], moe_w_down[j])
    return _rmsnorm(x, final_norm)
```

```python
import contextlib
import math
import numpy as np
import ml_dtypes
import concourse.bass as bass
import concourse.mybir as mybir
from concourse.bass_utils import run_bass_kernel_spmd

F32 = mybir.dt.float32
BF16 = mybir.dt.bfloat16
I32 = mybir.dt.int32
AF = mybir.ActivationFunctionType
ALU = mybir.AluOpType
AX = mybir.AxisListType
NPBF = ml_dtypes.bfloat16

D = 1024
NTOK = 2048
SEQ = 8192
TB = 512
NTB = NTOK // TB
EPS = 1e-6
SCALE = 192.0 ** -0.5
IN_COLS = 2752
SEM_CAP = 12000
DUMP = False
TWO_PI = 2.0 * math.pi
C1 = 6.28125
C2 = TWO_PI - C1


class Buf:
    __slots__ = ("name", "w", "readers", "psum")

    def __init__(self, name="", psum=False):
        self.name = name
        self.w = None
        self.readers = {}
        self.psum = psum


class Op:
    __slots__ = ("eng", "fn", "deps", "signal", "sem", "val", "dma", "idx", "gidx", "inc")


class Sched:
    ENGS = ("sync", "scalar", "vector", "gpsimd", "tensor")

    def __init__(self, nc):
        self.nc = nc
        self.q = {e: [] for e in self.ENGS}
        self.n = 0
        self.dmas_since_barrier = []
        self.barrier_marks = []

    def add(self, eng, fn, reads=(), writes=(), dma=None, after=(), inc=16):
        op = Op()
        op.inc = inc
        op.eng = eng
        op.fn = fn
        op.signal = False
        op.sem = None
        op.val = None
        op.dma = dma
        op.gidx = self.n
        self.n += 1
        deps = {}

        def dep(p):
            if p is None:
                return
            if p.dma is None and p.eng == eng and eng == "tensor" and dma is None:
                return
            key = ("dma", id(p)) if p.dma is not None else p.eng
            o = deps.get(key)
            if o is None or o.gidx < p.gidx:
                deps[key] = p

        for r in reads:
            dep(r.w)
        for w in writes:
            dep(w.w)
            for p in w.readers.values():
                if p.dma is None and p.eng == eng and dma is None:
                    continue
                dep(p)
        for p in after:
            dep(p)
        for p in deps.values():
            p.signal = True
        op.deps = list(deps.values())
        for r in reads:
            key = ("dma", id(op)) if dma is not None else eng
            r.readers[key] = op
            assert not (r.psum and len(r.readers) > 1), "PSUM tile %s read by two engines" % r.name
        for w in writes:
            w.w = op
            w.readers = {}
        if dma is not None:
            op.signal = True
            self.dmas_since_barrier.append(op)
        op.idx = len(self.q[eng])
        self.q[eng].append(op)
        return op

    def op(self, eng, meth, reads=(), writes=(), dma=None, after=(), inc=16, **kw):
        if meth == "dma_nc":
            def fn(e, kw=kw):
                with self.nc.allow_non_contiguous_dma(reason="tiny strided vector"):
                    return e.dma_start(**kw)
        else:
            def fn(e, meth=meth, kw=kw):
                return getattr(e, meth)(**kw)
        return self.add(eng, fn, reads=reads, writes=writes, dma=dma, after=after, inc=inc)

    def gather_rows(self, out, src, idx_col, reads=(), writes=(), dma=None):
        def fn(e, out=out, src=src, idx_col=idx_col):
            return e.indirect_dma_start(out=out, out_offset=None, in_=src, in_offset=bass.IndirectOffsetOnAxis(ap=idx_col, axis=0))
        return self.add("gpsimd", fn, reads=reads, writes=writes, dma=dma)

    def all_gather(self, src, dst, reads=(), writes=(), dma=None):
        def fn(e, src=src, dst=dst):
            return e.collective_compute("AllGather", ALU.bypass, replica_groups=[list(range(8))], ins=[src], outs=[dst])
        return self.add("gpsimd", fn, reads=reads, writes=writes, dma=dma, inc=1)

    def barrier(self):
        lasts = [self.q[e][-1] for e in self.ENGS if self.q[e] and self.q[e][-1].dma is None]
        tails = []
        for e in ("scalar", "vector", "gpsimd", "tensor"):
            ops = [o for o in self.q[e] if o.dma is None]
            if ops:
                tails.append(ops[-1])
        dmas = list(self.dmas_since_barrier)
        self.dmas_since_barrier = []
        self.barrier_marks.append(self.n)
        for e in self.ENGS:
            self.add(e, lambda eng: eng.nop(), after=tails + dmas)

    def emit(self, final_waits=()):
        nc = self.nc
        sems = []
        stack = contextlib.ExitStack()
        with stack:
            def newsem(name):
                s = stack.enter_context(nc.semaphore(name))
                sems.append(s)
                return s
            dsem = {}
            free = []
            marks = list(self.barrier_marks)
            allops = sorted((op for e in self.ENGS for op in self.q[e] if op.dma is not None), key=lambda o: o.gidx)
            for op in allops:
                while marks and op.gidx >= marks[0]:
                    marks.pop(0)
                    free.extend(dsem.values())
                    dsem = {}
                ent = dsem.get(op.dma)
                if ent is None:
                    ent = free.pop() if free else [newsem("d%d" % len(sems)), 0]
                    dsem[op.dma] = ent
                ent[1] += op.inc
                op.sem = ent[0]
                op.val = ent[1]
            for e in self.ENGS:
                cur = None
                cnt = 0
                k = 0
                for op in self.q[e]:
                    if op.dma is None and op.signal:
                        if cur is None or cnt >= SEM_CAP:
                            cur = newsem("c_%s_%d" % (e, k))
                            k += 1
                            cnt = 0
                        cnt += 1
                        op.sem = cur
                        op.val = cnt
            self.nsems = len(sems)
            with nc.Block() as block:
                def run(e):
                    def body(eng):
                        known = {}
                        for op in self.q[e]:
                            best = {}
                            for p in op.deps:
                                o = best.get(id(p.sem))
                                if o is None or o.val < p.val:
                                    best[id(p.sem)] = p
                            for p in best.values():
                                kk = id(p.sem)
                                if known.get(kk, 0) < p.val:
                                    eng.wait_ge(p.sem, p.val)
                                    known[kk] = p.val
                                    if DUMP:
                                        print(e, "  wait", p.sem, p.val)
                            ins = op.fn(eng)
                            if op.signal:
                                ins.then_inc(op.sem, op.inc if op.dma is not None else 1)
                            if DUMP:
                                print(e, "op", op.gidx, "signal" if op.signal else "", op.sem if op.signal else "", op.val if op.signal else "")
                        if e == "sync":
                            for p in final_waits:
                                eng.wait_ge(p.sem, p.val)
                    return body
                block.sync(run("sync"))
                block.scalar(run("scalar"))
                block.vector(run("vector"))
                block.gpsimd(run("gpsimd"))
                block.tensor(run("tensor"))


class Ring:
    def __init__(self, ctx, name, shape, dt, n, psum=False):
        self.tiles = []
        for i in range(n):
            if psum:
                t = ctx.st.enter_context(ctx.nc.psum_tensor("p_%s%d" % (name, i), shape, dt))
            else:
                t = ctx.st.enter_context(ctx.nc.sbuf_tensor("s_%s%d" % (name, i), shape, dt))
            self.tiles.append((t, Buf("%s%d" % (name, i), psum=psum)))
        self.i = 0
        self.name = name

    @classmethod
    def of(cls, name, tiles):
        r = cls.__new__(cls)
        r.tiles = list(tiles)
        r.i = 0
        r.name = name
        return r

    def next(self):
        t = self.tiles[self.i % len(self.tiles)]
        slot = self.i % len(self.tiles)
        self.i += 1
        return t[0], t[1], "%s_%d" % (self.name, slot)


class Ctx:
    gkey = 0
    SHARED = ("ident", "B_ident", "ones", "B_ones", "psT", "psF", "eps_t", "B_eps", "cosT", "sinT", "B_cos", "B_sin", "idx", "B_idx")

    def sub(self, st):
        c2 = Ctx(self.nc, self.S, st)
        for a in Ctx.SHARED:
            if hasattr(self, a):
                setattr(c2, a, getattr(self, a))
        return c2

    def __init__(self, nc, S, st):
        self.nc = nc
        self.S = S
        self.st = st
        self.dkey = 0

    def sb(self, name, shape, dt):
        return self.st.enter_context(self.nc.sbuf_tensor("s_" + name, shape, dt))

    def key(self, base):
        Ctx.gkey += 1
        return "%s_%d" % (base, Ctx.gkey)


def new_ctx(nc, S):
    st = contextlib.ExitStack()
    return Ctx(nc, S, st)


def load_consts(c, ident_d, ones_d):
    S = c.S
    c.ident = c.sb("ident", [128, 128], BF16)
    c.B_ident = Buf()
    c.ones = c.sb("ones", [128, 128], BF16)
    c.B_ones = Buf()
    S.op("sync", "dma_start", out=c.ident[:], in_=ident_d, writes=[c.B_ident], dma="ident")
    S.op("sync", "dma_start", out=c.ones[:], in_=ones_d, writes=[c.B_ones], dma="ones")
    c.psT = Ring(c, "psT", [128, 8, 128], BF16, 2, psum=True)
    c.psF = Ring(c, "psF", [128, 512], F32, 6, psum=True)
    c.eps_t = c.sb("eps_t", [128, 1], F32)
    c.B_eps = Buf()
    S.op("vector", "memset", ap=c.eps_t[:], constant=EPS, writes=[c.B_eps])


def load_gain_cols(c, name, vec_d, nk):
    t = c.sb(name, [128, nk], F32)
    B = Buf(name)
    c.S.op("sync", "dma_nc", out=t[:], in_=vec_d.rearrange("(k p) -> p k", p=128), writes=[B], dma=name)
    return t, B


class NormRings:
    def __init__(self, c):
        self.junk = Ring(c, c.key("junk"), [128, D], BF16, 2)
        self.small = Ring(c, c.key("small"), [128, 4], F32, 6)
        self.xn = Ring(c, c.key("xn"), [128, D], BF16, 2)


def norm_transpose_tile(c, x_ap, B_x, hT_ap, B_hT, col0, nr, xn_dt=BF16):
    S = c.S
    junk, B_junk, _ = nr.junk.next()
    sm, B_sm, _ = nr.small.next()
    xn, B_xn, _ = nr.xn.next()
    S.op("scalar", "activation", out=junk[:], in_=x_ap, func=AF.Square, accum_out=sm[:, 0:1], reads=[B_x], writes=[B_junk, B_sm])
    S.op("scalar", "activation", out=sm[:, 1:2], in_=sm[:, 0:1], func=AF.Sqrt, scale=1.0 / D, bias=c.eps_t[:, 0:1],
         reads=[B_sm, c.B_eps], writes=[B_sm])
    S.op("vector", "reciprocal", out=sm[:, 2:3], in_=sm[:, 1:2], reads=[B_sm], writes=[B_sm])
    S.op("vector", "tensor_scalar", out=xn[:], in0=x_ap, scalar1=sm[:, 2:3], scalar2=None, op0=ALU.mult, reads=[B_x, B_sm], writes=[B_xn])
    pT, B_pT, _ = c.psT.next()
    for k in range(8):
        S.op("tensor", "transpose", out=pT[:, k, :], in_=xn[:, k * 128:(k + 1) * 128], identity=c.ident[:], reads=[B_xn, c.B_ident], writes=[B_pT])
    S.op("scalar", "copy", out=hT_ap[:, :, col0:col0 + 128], in_=pT[:], reads=[B_pT], writes=[B_hT])
    return sm, B_sm


def rope_tables(c, pos_d, inv_d):
    S = c.S
    nc = c.nc
    c.cosT = c.sb("cosT", [64, NTOK], F32)
    c.sinT = c.sb("sinT", [64, NTOK], F32)
    c.B_cos = Buf()
    c.B_sin = Buf()
    with contextlib.ExitStack() as st2:
        def sb(name, shape, dt):
            return st2.enter_context(nc.sbuf_tensor("s_" + name, shape, dt))
        pos_i = sb("pos_i", [64, NTOK], I32)
        B_pi = Buf()
        inv = sb("inv", [64, 1], F32)
        B_inv = Buf()
        ang = sb("ang", [64, NTOK], F32)
        B_ang = Buf()
        ni = sb("ni", [64, NTOK], I32)
        B_ni = Buf()
        nf = sb("nf", [64, NTOK], F32)
        B_nf = Buf()
        r1 = sb("r1", [64, NTOK], F32)
        B_r1 = Buf()
        S.op("sync", "dma_start", out=pos_i[:], in_=pos_d.partition_broadcast(64), writes=[B_pi], dma="pos")
        S.op("sync", "dma_start", out=inv[:], in_=inv_d, writes=[B_inv], dma="inv")
        S.op("vector", "tensor_copy", out=ang[:], in_=pos_i[:], reads=[B_pi], writes=[B_ang])
        S.op("vector", "tensor_scalar", out=ang[:], in0=ang[:], scalar1=inv[:, 0:1], scalar2=None, op0=ALU.mult, reads=[B_ang, B_inv], writes=[B_ang])
        for which, (dst, B_dst) in enumerate(((c.sinT, c.B_sin), (c.cosT, c.B_cos))):
            off = 0.0 if which == 0 else math.pi / 2
            S.op("vector", "tensor_scalar", out=ni[:], in0=ang[:], scalar1=off, scalar2=1.0 / TWO_PI, op0=ALU.add, op1=ALU.mult, reads=[B_ang], writes=[B_ni])
            S.op("vector", "tensor_copy", out=nf[:], in_=ni[:], reads=[B_ni], writes=[B_nf])
            S.op("vector", "scalar_tensor_tensor", out=r1[:], in0=nf[:], scalar=-C1, in1=ang[:], op0=ALU.mult, op1=ALU.add, reads=[B_nf, B_ang], writes=[B_r1])
            S.op("vector", "scalar_tensor_tensor", out=r1[:], in0=nf[:], scalar=-C2, in1=r1[:], op0=ALU.mult, op1=ALU.add, reads=[B_nf, B_r1], writes=[B_r1])
            S.op("vector", "tensor_scalar", out=r1[:], in0=r1[:], scalar1=off, scalar2=math.pi, op0=ALU.add, op1=ALU.min, reads=[B_r1], writes=[B_r1])
            S.op("vector", "tensor_scalar", out=r1[:], in0=r1[:], scalar1=-math.pi, scalar2=None, op0=ALU.max, reads=[B_r1], writes=[B_r1])
            S.op("scalar", "activation", out=dst[:], in_=r1[:], func=AF.Sin, reads=[B_r1], writes=[B_dst])
        S.barrier()


def load_phase_a_weights(c, w_in_d, mix_norm_d, w_q_b_d, q_a_norm_d, w_kv_b_d, kv_a_norm_d):
    S = c.S
    c.win = c.sb(c.key("win"), [128, 8, IN_COLS + 64], BF16)
    c.B_win = Buf()
    c.wqb = c.sb(c.key("wqb"), [128, 3, 768 + 256], BF16)
    c.B_wqb = Buf()
    c.wk = c.sb(c.key("wk"), [128, 2, 512], BF16)
    c.wv = c.sb(c.key("wv"), [128, 2, 512], BF16)
    c.B_wkv = Buf()
    gmix, B_gmix = load_gain_cols(c, c.key("gmix"), mix_norm_d, 8)
    gq, B_gq = load_gain_cols(c, c.key("gq"), q_a_norm_d, 3)
    gkv, B_gkv = load_gain_cols(c, c.key("gkv"), kv_a_norm_d, 2)
    stage = Ring(c, c.key("wstage"), [128, IN_COLS], F32, 2)
    for k in range(8):
        st_t, B_st, key = stage.next()
        S.op("sync", "dma_start", out=st_t[:], in_=w_in_d[k * 128:(k + 1) * 128, :], writes=[B_st], dma=key)
        S.op("vector" if k % 2 == 0 else "gpsimd", "tensor_scalar", out=c.win[:, k, 0:IN_COLS], in0=st_t[:], scalar1=gmix[:, k:k + 1], scalar2=None,
             op0=ALU.mult, reads=[B_st, B_gmix], writes=[c.B_win])
    S.op("vector", "tensor_scalar", out=c.win[:, :, IN_COLS:IN_COLS + 32], in0=c.win[:, :, 672:704], scalar1=-1.0, scalar2=None, op0=ALU.mult,
         reads=[c.B_win], writes=[c.B_win])
    S.op("vector", "tensor_copy", out=c.win[:, :, IN_COLS + 32:IN_COLS + 64], in_=c.win[:, :, 640:672], reads=[c.B_win], writes=[c.B_win])
    for k in range(3):
        st_t, B_st, key = stage.next()
        S.op("sync", "dma_start", out=st_t[:, 0:768], in_=w_q_b_d[k * 128:(k + 1) * 128, :], writes=[B_st], dma=key)
        S.op("vector", "tensor_scalar", out=c.wqb[:, k, 0:768], in0=st_t[:, 0:768], scalar1=gq[:, k:k + 1], scalar2=None, op0=ALU.mult,
             reads=[B_st, B_gq], writes=[c.B_wqb])
    for h in range(4):
        b0 = h * 192 + 128
        S.op("vector", "tensor_scalar", out=c.wqb[:, :, 768 + h * 64:768 + h * 64 + 32], in0=c.wqb[:, :, b0 + 32:b0 + 64], scalar1=-1.0, scalar2=None,
             op0=ALU.mult, reads=[c.B_wqb], writes=[c.B_wqb])
        S.op("vector", "tensor_copy", out=c.wqb[:, :, 768 + h * 64 + 32:768 + h * 64 + 64], in_=c.wqb[:, :, b0:b0 + 32], reads=[c.B_wqb], writes=[c.B_wqb])
    for k in range(2):
        st_t, B_st, key = stage.next()
        S.op("sync", "dma_start", out=st_t[:, 0:1024], in_=w_kv_b_d[k * 128:(k + 1) * 128, :], writes=[B_st], dma=key)
        src = st_t[:, 0:1024].rearrange("p (h t c) -> p h t c", h=4, t=2)
        S.op("vector", "tensor_scalar", out=c.wk[:, k, :].rearrange("p (h c) -> p h c", h=4), in0=src[:, :, 0, :], scalar1=gkv[:, k:k + 1], scalar2=None,
             op0=ALU.mult, reads=[B_st, B_gkv], writes=[c.B_wkv])
        S.op("vector", "tensor_scalar", out=c.wv[:, k, :].rearrange("p (h c) -> p h c", h=4), in0=src[:, :, 1, :], scalar1=gkv[:, k:k + 1], scalar2=None,
             op0=ALU.mult, reads=[B_st, B_gkv], writes=[c.B_wkv])


def phase_a(c, x_src, outs):
    S = c.S
    hT_ring = Ring(c, c.key("hT"), [128, 8, TB], BF16, 2)
    nr = NormRings(c)
    cq_ring = Ring(c, c.key("cqbf"), [128, 3, TB], BF16, 2)
    cqs_ring = Ring(c, c.key("cqsq"), [128, 3, TB], BF16, 2)
    ckv_ring = Ring(c, c.key("ckvbf"), [128, 2, TB], BF16, 2)
    ckvs_ring = Ring(c, c.key("ckvsq"), [128, 2, TB], BF16, 2)
    rbc_ring = Ring(c, c.key("rbc"), [128, TB], F32, 3)
    o16_ring = Ring(c, c.key("o16"), [128, TB], BF16, 8)
    o32_ring = Ring(c, c.key("o32"), [128, TB], F32, 3)
    t32_ring = Ring(c, c.key("t32"), [64, TB], F32, 4)
    final = []

    def store(dst_ap, src_ap, B_src, key):
        final.append(S.op("gpsimd", "dma_start", out=dst_ap, in_=src_ap, reads=[B_src], dma=key))

    def proj_fm(hT, B_hT, c0, M):
        ps, B_ps, _ = c.psF.next()
        for k in range(8):
            S.op("tensor", "matmul", out=ps[0:M, :], lhsT=c.win[:, k, c0:c0 + M], rhs=hT[:, k, :], start=(k == 0), stop=(k == 7),
                 reads=[c.B_win, B_hT], writes=[B_ps])
        return ps, B_ps

    def rms_bc(sq, B_sq, nk, dim):
        ps, B_ps, _ = c.psF.next()
        for k in range(nk):
            S.op("tensor", "matmul", out=ps[:], lhsT=c.ones[:], rhs=sq[:, k, :], start=(k == 0), stop=(k == nk - 1), reads=[c.B_ones, B_sq], writes=[B_ps])
        r, B_r, _ = rbc_ring.next()
        S.op("scalar", "activation", out=r[:], in_=ps[:], func=AF.Sqrt, scale=1.0 / dim, bias=c.eps_t[:, 0:1], reads=[B_ps, c.B_eps], writes=[B_r])
        S.op("vector", "reciprocal", out=r[:], in_=r[:], reads=[B_r], writes=[B_r])
        return r, B_r

    def rope_out(ps_a, B_a, ps_b, B_b, t0, mul_ap, B_mul, dst_ap, scale):
        t1, B_t1, _ = t32_ring.next()
        t2, B_t2, _ = t32_ring.next()
        S.op("vector", "tensor_tensor", out=t1[:], in0=ps_a[0:64, :], in1=c.cosT[:, t0:t0 + TB], op=ALU.mult, reads=[B_a, c.B_cos], writes=[B_t1])
        S.op("vector", "tensor_tensor", out=t2[:], in0=ps_b[0:64, :], in1=c.sinT[:, t0:t0 + TB], op=ALU.mult, reads=[B_b, c.B_sin], writes=[B_t2])
        o, B_o, key = o16_ring.next()
        if mul_ap is None:
            S.op("vector", "tensor_tensor", out=o[0:64, :], in0=t1[:], in1=t2[:], op=ALU.add, reads=[B_t1, B_t2], writes=[B_o])
        else:
            S.op("vector", "tensor_tensor", out=t1[:], in0=t1[:], in1=t2[:], op=ALU.add, reads=[B_t1, B_t2], writes=[B_t1])
            S.op("vector", "scalar_tensor_tensor", out=o[0:64, :], in0=t1[:], scalar=scale, in1=mul_ap[0:64, :], op0=ALU.mult, op1=ALU.mult,
                 reads=[B_t1, B_mul], writes=[B_o])
        store(dst_ap, o[0:64, :], B_o, key)

    for tb in range(NTB):
        t0 = tb * TB
        hT, B_hT, _ = hT_ring.next()
        for j in range(4):
            x_ap, B_x = x_src(tb * 4 + j)
            norm_transpose_tile(c, x_ap, B_x, hT, B_hT, j * 128, nr)
        if getattr(c, "dbg", 0) == 3:
            store(outs["QN"][0, :, t0:t0 + TB], hT[:, 0, :], B_hT, "dbg")
            return final
        cq, B_cq, _ = cq_ring.next()
        cqs, B_cqs, _ = cqs_ring.next()
        for ch in range(3):
            ps, B_ps = proj_fm(hT, B_hT, ch * 128, 128)
            S.op("scalar", "copy", out=cq[:, ch, :], in_=ps[:], reads=[B_ps], writes=[B_cq])
            S.op("vector", "tensor_tensor", out=cqs[:, ch, :], in0=cq[:, ch, :], in1=cq[:, ch, :], op=ALU.mult, reads=[B_cq], writes=[B_cqs])
        if getattr(c, "dbg", 0) in (41, 411, 412):
            return final
        rq, B_rq = rms_bc(cqs, B_cqs, 3, 384.0)
        if getattr(c, "dbg", 0) == 42:
            return final
        for h in range(4):
            ps, B_ps, _ = c.psF.next()
            for k in range(3):
                S.op("tensor", "matmul", out=ps[:], lhsT=c.wqb[:, k, h * 192:h * 192 + 128], rhs=cq[:, k, :], start=(k == 0), stop=(k == 2),
                     reads=[c.B_wqb, B_cq], writes=[B_ps])
            o, B_o, key = o16_ring.next()
            S.op("vector", "scalar_tensor_tensor", out=o[:], in0=ps[:], scalar=SCALE, in1=rq[:], op0=ALU.mult, op1=ALU.mult, reads=[B_ps, B_rq], writes=[B_o])
            store(outs["QN"][h, :, t0:t0 + TB], o[:], B_o, key)
            if getattr(c, "dbg", 0) == 43:
                return final
            psa, B_psa, _ = c.psF.next()
            psb, B_psb, _ = c.psF.next()
            for k in range(3):
                S.op("tensor", "matmul", out=psa[0:64, :], lhsT=c.wqb[:, k, h * 192 + 128:h * 192 + 192], rhs=cq[:, k, :], start=(k == 0), stop=(k == 2),
                     reads=[c.B_wqb, B_cq], writes=[B_psa])
            for k in range(3):
                S.op("tensor", "matmul", out=psb[0:64, :], lhsT=c.wqb[:, k, 768 + h * 64:768 + h * 64 + 64], rhs=cq[:, k, :], start=(k == 0), stop=(k == 2),
                     reads=[c.B_wqb, B_cq], writes=[B_psb])
            if getattr(c, "dbg", 0) == 44:
                return final
            rope_out(psa, B_psa, psb, B_psb, t0, rq, B_rq, outs["QP"][h, :, t0:t0 + TB], SCALE)
        if getattr(c, "dbg", 0) == 4:
            return final
        ckv, B_ckv, _ = ckv_ring.next()
        ckvs, B_ckvs, _ = ckvs_ring.next()
        for ch in range(2):
            ps, B_ps = proj_fm(hT, B_hT, 384 + ch * 128, 128)
            S.op("scalar", "copy", out=ckv[:, ch, :], in_=ps[:], reads=[B_ps], writes=[B_ckv])
            S.op("vector", "tensor_tensor", out=ckvs[:, ch, :], in0=ckv[:, ch, :], in1=ckv[:, ch, :], op=ALU.mult, reads=[B_ckv], writes=[B_ckvs])
        rkv, B_rkv = rms_bc(ckvs, B_ckvs, 2, 256.0)
        for h in range(4):
            ps, B_ps, _ = c.psF.next()
            for k in range(2):
                S.op("tensor", "matmul", out=ps[:], lhsT=c.wk[:, k, h * 128:(h + 1) * 128], rhs=ckv[:, k, :], start=(k == 0), stop=(k == 1),
                     reads=[c.B_wkv, B_ckv], writes=[B_ps])
            o, B_o, key = o16_ring.next()
            S.op("vector", "tensor_tensor", out=o[:], in0=ps[:], in1=rkv[:], op=ALU.mult, reads=[B_ps, B_rkv], writes=[B_o])
            store(outs["KN"][h, :, t0:t0 + TB], o[:], B_o, key)
        for h in range(4):
            ps, B_ps, _ = c.psF.next()
            for k in range(2):
                S.op("tensor", "matmul", out=ps[:], lhsT=c.wv[:, k, h * 128:(h + 1) * 128], rhs=ckv[:, k, :], start=(k == 0), stop=(k == 1),
                     reads=[c.B_wkv, B_ckv], writes=[B_ps])
            o, B_o, key = o16_ring.next()
            S.op("vector", "tensor_tensor", out=o[:], in0=ps[:], in1=rkv[:], op=ALU.mult, reads=[B_ps, B_rkv], writes=[B_o])
            store(outs["VT"][h, :, t0:t0 + TB], o[:], B_o, key)
        if getattr(c, "dbg", 0) == 5:
            return final
        psa, B_psa = proj_fm(hT, B_hT, 640, 64)
        psb, B_psb = proj_fm(hT, B_hT, IN_COLS, 64)
        rope_out(psa, B_psa, psb, B_psb, t0, None, None, outs["KPE"][:, t0:t0 + TB], 1.0)
        if getattr(c, "dbg", 0) == 6:
            return final
        for h in range(4):
            ps, B_ps = proj_fm(hT, B_hT, 704 + h * 128, 128)
            o, B_o, key = o16_ring.next()
            S.op("scalar", "activation", out=o[:], in_=ps[:], func=AF.Silu, reads=[B_ps], writes=[B_o])
            store(outs["HQ"][h, :, t0:t0 + TB], o[:], B_o, key)
        for h in range(4):
            ps, B_ps = proj_fm(hT, B_hT, 1216 + h * 128, 128)
            o, B_o, key = o32_ring.next()
            S.op("vector", "tensor_copy", out=o[:], in_=ps[:], reads=[B_ps], writes=[B_o])
            store(outs["ZF"][h, :, t0:t0 + TB], o[:], B_o, key)
        if getattr(c, "dbg", 0) == 7:
            return final
        for h in range(4):
            ps, B_ps = proj_fm(hT, B_hT, 1728 + h * 128, 128)
            o, B_o, key = o16_ring.next()
            S.op("vector", "tensor_copy", out=o[:], in_=ps[:], reads=[B_ps], writes=[B_o])
            store(outs["HIT"][h, :, t0:t0 + TB], o[:], B_o, key)
        for h in range(4):
            ps, B_ps = proj_fm(hT, B_hT, 2240 + h * 128, 128)
            o, B_o, key = o16_ring.next()
            S.op("scalar", "activation", out=o[:], in_=ps[:], func=AF.Silu, reads=[B_ps], writes=[B_o])
            store(outs["HGT"][h, :, t0:t0 + TB], o[:], B_o, key)
    return final


PA16_ROWS = 3392
PA16_BASE = {"QN": 0, "KN": 512, "VT": 1024, "HQ": 1536, "HIT": 2048, "HGT": 2560, "QP": 3072, "KPE": 3328}


def pa_views(pa16, pa32):
    o = {}
    for n in ("QN", "KN", "VT", "HQ", "HIT", "HGT"):
        b0 = PA16_BASE[n]
        o[n] = pa16[b0:b0 + 512, :].rearrange("(h p) t -> h p t", p=128)
    o["QP"] = pa16[3072:3328, :].rearrange("(h p) t -> h p t", p=64)
    o["KPE"] = pa16[3328:3392, :]
    o["ZF"] = pa32.rearrange("(h p) t -> h p t", p=128)
    return o


def const_inputs():
    inv = (np.float32(10000.0) ** (-(np.arange(0, 64, 2, dtype=np.float32)) / np.float32(64))).astype(np.float32)
    return {
        "ident": np.eye(128, dtype=np.float32).astype(NPBF),
        "ones": np.ones((128, 128), dtype=np.float32).astype(NPBF),
        "inv": np.concatenate([inv, inv]).reshape(64, 1).astype(np.float32),
    }


def dram_in(nc, name, shape, dt):
    return nc.dram_tensor(name, shape, dt, kind="ExternalInput").ap()


def build_l1(stop=0):
    nc = bass.Bass("TRN2", target_bir_lowering=False)
    x_d = dram_in(nc, "x", [NTOK, D], F32)
    pos_d = dram_in(nc, "pos", [1, NTOK], I32)
    ident_d = dram_in(nc, "ident", [128, 128], BF16)
    ones_d = dram_in(nc, "ones", [128, 128], BF16)
    inv_d = dram_in(nc, "inv", [64, 1], F32)
    w_in_d = dram_in(nc, "w_in", [D, IN_COLS], F32)
    mixn_d = dram_in(nc, "mix_norm", [D], F32)
    wqb_d = dram_in(nc, "w_q_b", [384, 768], F32)
    qan_d = dram_in(nc, "q_a_norm", [384], F32)
    wkvb_d = dram_in(nc, "w_kv_b", [256, 1024], F32)
    kvan_d = dram_in(nc, "kv_a_norm", [256], F32)
    pa16 = nc.dram_tensor("PA16", [PA16_ROWS, NTOK], BF16, kind="ExternalOutput").ap()
    pa32 = nc.dram_tensor("PA32", [512, NTOK], F32, kind="ExternalOutput").ap()
    outs = pa_views(pa16, pa32)
    S = Sched(nc)
    c = new_ctx(nc, S)
    with c.st:
        load_consts(c, ident_d, ones_d)
        rope_tables(c, pos_d, inv_d)
        if stop == 1:
            f = S.op("gpsimd", "dma_start", out=outs["ZF"][0, 0:64, :], in_=c.cosT[:], reads=[c.B_cos], dma="dbg")
            S.emit(final_waits=[f])
            return nc
        load_phase_a_weights(c, w_in_d, mixn_d, wqb_d, qan_d, wkvb_d, kvan_d)
        if stop == 2:
            f = S.op("gpsimd", "dma_start", out=outs["QN"][0, :, :], in_=c.win[:, 0, 0:2048], reads=[c.B_win], dma="dbg")
            S.emit(final_waits=[f])
            return nc
        x_ring = Ring(c, "xin", [128, D], F32, 3)
        c.dbg = stop

        def x_src(t):
            xt, B_xt, key = x_ring.next()
            S.op("sync", "dma_start", out=xt[:], in_=x_d[t * 128:(t + 1) * 128, :], writes=[B_xt], dma=key)
            return xt[:], B_xt
        final = phase_a(c, x_src, outs)
        S.emit(final_waits=final)
    return nc


NQT = SEQ // 128
MASK_NEG = -30000.0


def mixer_consts():
    cm = np.where(np.arange(128)[None, :] <= np.arange(128)[:, None], 0.0, MASK_NEG).astype(np.float32)
    hm = (np.arange(64)[:, None] <= np.arange(64)[None, :]).astype(np.float32)
    rs = np.ones((128, 512), np.float32)
    rs[:, ::64] = 0.0
    return {"cmask": cm.astype(NPBF), "hmask": hm, "resetm": rs}


IDX_QN, IDX_KN, IDX_VT, IDX_QP, IDX_KPE, IDX_HQ, IDX_HIT, IDX_HGT, IDX_ZF, IDX_MIX, IDX_N = 0, 4, 8, 12, 16, 20, 36, 52, 68, 84, 92


def attention(c, d, AT_d, nqt=NQT, pfx="a"):
    S = c.S
    nc = c.nc
    final = []
    with contextlib.ExitStack() as st2:
        c2 = Ctx(nc, S, st2)
        qn = c2.sb(pfx + "_qn", [128, SEQ], BF16)
        qp = c2.sb(pfx + "_qp", [64, SEQ], BF16)
        kn = c2.sb(pfx + "_kn", [128, SEQ], BF16)
        kpe = c2.sb(pfx + "_kpe", [64, SEQ], BF16)
        v = c2.sb(pfx + "_v", [128, NQT, 128], BF16)
        vt = c2.sb(pfx + "_vt", [128, SEQ], BF16)
        cmask = c2.sb(pfx + "_cmask", [128, 128], BF16)
        B_in = [Buf() for _ in range(4)]
        B_vt = [Buf() for _ in range(4)]
        B_cm = Buf()
        S.op("sync", "dma_start", out=cmask[:], in_=d["cmask"], writes=[B_cm], dma=c.key("a_cm"))
        CH = 2048
        pv = d["pa16v"]
        for i in range(SEQ // CH):
            sl = slice(i * CH, (i + 1) * CH)
            S.gather_rows(kn[:, sl], pv, c.idx[:, IDX_KN + i:IDX_KN + i + 1], reads=[c.B_idx], writes=[B_in[i]], dma=c.key("a_in"))
            S.gather_rows(kpe[:, sl], pv, c.idx[0:64, IDX_KPE + i:IDX_KPE + i + 1], reads=[c.B_idx], writes=[B_in[i]], dma=c.key("a_in"))
            S.gather_rows(qn[:, sl], pv, c.idx[:, IDX_QN + i:IDX_QN + i + 1], reads=[c.B_idx], writes=[B_in[i]], dma=c.key("a_in"))
            S.gather_rows(qp[:, sl], pv, c.idx[0:64, IDX_QP + i:IDX_QP + i + 1], reads=[c.B_idx], writes=[B_in[i]], dma=c.key("a_in"))
            S.gather_rows(vt[:, sl], pv, c.idx[:, IDX_VT + i:IDX_VT + i + 1], reads=[c.B_idx], writes=[B_vt[i]], dma=c.key("a_in"))
        ps_s = Ring.of("pss", c.psF.tiles[0:3])
        ps_o = Ring.of("pso", c.psF.tiles[3:5])
        ps_t = Ring.of("pst", c.psT.tiles[0:2])
        p_ring = Ring(c2, pfx + "_p", [128, 512], BF16, 3)
        pT_ring = Ring(c2, pfx + "_pT", [128, 512], BF16, 3)
        st_ring = Ring(c2, pfx + "_stat", [128, 40], F32, 3)
        o_ring = Ring(c2, pfx + "_o", [128, 128], BF16, 2)
        out_ring = Ring(c2, pfx + "_out", [128, 512], BF16, 2)
        for g in range(NQT // 8):
            pt, B_pt, _ = ps_t.next()
            for u in range(8):
                t = g * 8 + u
                S.op("tensor", "transpose", out=pt[:, u, :], in_=vt[:, t * 128:(t + 1) * 128], identity=c.ident[:],
                     reads=[B_vt[(t * 128) // CH], c.B_ident], writes=[B_pt])
            S.op("vector", "tensor_copy", out=v[:, g * 8:(g + 1) * 8, :], in_=pt[:], reads=[B_pt], writes=[B_in[(g * 8 * 128) // CH]])

        def scores(i, kb, w, diag):
            ps, B_ps, _ = ps_s.next()
            q0 = i * 128
            k0 = kb * 512
            rd = list({id(b): b for b in (B_in[q0 // CH], B_in[k0 // CH], B_in[(k0 + w - 1) // CH])}.values())
            S.op("tensor", "matmul", out=ps[:, 0:w], lhsT=qn[:, q0:q0 + 128], rhs=kn[:, k0:k0 + w], start=True, stop=False, reads=rd, writes=[B_ps])
            S.op("tensor", "matmul", out=ps[:, 0:w], lhsT=qp[:, q0:q0 + 128], rhs=kpe[:, k0:k0 + w], start=False, stop=(not diag), reads=rd, writes=[B_ps])
            if diag:
                S.op("tensor", "matmul", out=ps[:, w - 128:w], lhsT=c.ident[:], rhs=cmask[:], start=False, stop=True,
                     reads=[c.B_ident, B_cm], writes=[B_ps])
            return ps, B_ps

        outt = None
        for i in range(nqt):
            nk = i + 1
            nb = (nk + 3) // 4
            stt, B_st, _ = st_ring.next()
            S.op("vector", "memset", ap=stt[:, 16:32], constant=0.0, writes=[B_st])
            for kb in range(nb):
                w = min(4, nk - 4 * kb) * 128
                ps, B_ps = scores(i, kb, w, kb == nb - 1)
                S.op("vector", "reduce_max", out=stt[:, kb:kb + 1], in_=ps[:, 0:w], axis=AX.X, reads=[B_ps], writes=[B_st])
            S.op("vector", "reduce_max", out=stt[:, 32:33], in_=stt[:, 0:nb], axis=AX.X, reads=[B_st], writes=[B_st])
            S.op("vector", "tensor_scalar", out=stt[:, 33:34], in0=stt[:, 32:33], scalar1=-1.0, scalar2=None, op0=ALU.mult, reads=[B_st], writes=[B_st])
            po, B_po, _ = ps_o.next()
            for kb in range(nb):
                nsub = min(4, nk - 4 * kb)
                w = nsub * 128
                ps, B_ps = scores(i, kb, w, kb == nb - 1)
                p, B_p, _ = p_ring.next()
                S.op("scalar", "activation", out=p[:, 0:w], in_=ps[:, 0:w], func=AF.Exp, bias=stt[:, 33:34], accum_out=stt[:, 16 + kb:17 + kb],
                     reads=[B_ps, B_st], writes=[B_p, B_st])
                pt, B_pt, _ = ps_t.next()
                for j in range(nsub):
                    S.op("tensor", "transpose", out=pt[:, j, :], in_=p[:, j * 128:(j + 1) * 128], identity=c.ident[:], reads=[B_p, c.B_ident], writes=[B_pt])
                pT, B_pT, _ = pT_ring.next()
                S.op("vector", "tensor_copy", out=pT[:, 0:w].rearrange("p (j k) -> p j k", k=128), in_=pt[:, 0:nsub, :], reads=[B_pt], writes=[B_pT])
                for j in range(nsub):
                    kt = kb * 4 + j
                    S.op("tensor", "matmul", out=po[:, 0:128], lhsT=pT[:, j * 128:(j + 1) * 128], rhs=v[:, kt, :],
                         start=(kb == 0 and j == 0), stop=(kb == nb - 1 and j == nsub - 1), reads=[B_pT, B_in[(kt * 128) // CH]], writes=[B_po])
            S.op("vector", "reduce_sum", out=stt[:, 34:35], in_=stt[:, 16:16 + nb], axis=AX.X, reads=[B_st], writes=[B_st])
            S.op("vector", "reciprocal", out=stt[:, 35:36], in_=stt[:, 34:35], reads=[B_st], writes=[B_st])
            o, B_o, _ = o_ring.next()
            S.op("scalar", "activation", out=o[:], in_=po[:, 0:128], func=AF.Copy, scale=stt[:, 35:36], reads=[B_po, B_st], writes=[B_o])
            pt, B_pt, _ = ps_t.next()
            S.op("tensor", "transpose", out=pt[:, 0, :], in_=o[:], identity=c.ident[:], reads=[B_o, c.B_ident], writes=[B_pt])
            if i % 4 == 0:
                outt = out_ring.next()
            S.op("vector", "tensor_copy", out=outt[0][:, (i % 4) * 128:(i % 4 + 1) * 128], in_=pt[:, 0, :], reads=[B_pt], writes=[outt[1]])
            if i % 4 == 3 or i == nqt - 1:
                i0 = (i // 4) * 4
                wd = (i - i0 + 1) * 128
                final.append(S.op("sync", "dma_start", out=AT_d[:, i0 * 128:i0 * 128 + wd], in_=outt[0][:, 0:wd], reads=[outt[1]], dma=outt[2]))
        S.barrier()
    return final


def hgrn(c, d, RT_d, layer, nsb=SEQ // 512, pfx="h"):
    S = c.S
    nc = c.nc
    final = []
    with contextlib.ExitStack() as st2:
        c2 = Ctx(nc, S, st2)
        hmask = c2.sb(pfx + "_hmask", [64, 64], F32)
        resetm = c2.sb(pfx + "_resetm", [128, 512], F32)
        lbraw = c2.sb(pfx + "_lbraw", [128, 2], F32)
        gn = c2.sb(pfx + "_gn", [128, 1], F32)
        cst = c2.sb(pfx + "_cst", [128, 4], F32)
        B_c = Buf()
        kc = c.key("h_c")
        S.op("sync", "dma_start", out=hmask[:], in_=d["hmask"], writes=[B_c], dma=kc)
        S.op("sync", "dma_start", out=resetm[:], in_=d["resetm"], writes=[B_c], dma=kc)
        S.op("sync", "dma_start", out=lbraw[:], in_=d["lbraw"], writes=[B_c], dma=kc)
        S.op("sync", "dma_start", out=gn[:], in_=d["gn"], writes=[B_c], dma=kc)
        if layer == 0:
            S.op("vector", "memset", ap=cst[:, 0:1], constant=0.0, writes=[B_c])
        else:
            S.op("vector", "tensor_tensor", out=cst[:, 2:3], in0=lbraw[:, 1:2], in1=lbraw[:, 0:1], op=ALU.subtract, reads=[B_c], writes=[B_c])
            S.op("scalar", "activation", out=cst[:, 0:1], in_=cst[:, 2:3], func=AF.Sigmoid, reads=[B_c], writes=[B_c])
        S.op("vector", "tensor_scalar", out=cst[:, 1:2], in0=cst[:, 0:1], scalar1=-1.0, scalar2=1.0, op0=ALU.mult, op1=ALU.add, reads=[B_c], writes=[B_c])
        state = c2.sb(pfx + "_state", [128, 128], F32)
        state_bf = c2.sb(pfx + "_state_bf", [128, 128], BF16)
        B_state = Buf()
        B_sbf = Buf()
        S.op("vector", "memset", ap=state[:], constant=0.0, writes=[B_state])
        S.op("vector", "memset", ap=state_bf[:], constant=0.0, writes=[B_sbf])
        ps_at = Ring.of("psat", c.psF.tiles[0:2])
        ps_st = Ring.of("psst", c.psF.tiles[2:4])
        ps_o = Ring.of("pso", c.psF.tiles[4:6])
        ps_kd = Ring.of("pskd", c.psT.tiles[0:1])
        ps_rt = Ring.of("psrt", c.psT.tiles[1:2])
        in_hq = Ring(c2, pfx + "_hq", [128, 512], BF16, 2)
        in_zf = Ring(c2, pfx + "_zf", [128, 512], F32, 2)
        in_hiT = Ring(c2, pfx + "_hiT", [128, 512], BF16, 2)
        in_sgT = Ring(c2, pfx + "_sgT", [128, 512], BF16, 2)
        in_hi = Ring(c2, pfx + "_hi", [64, 8, 128], BF16, 2)
        in_sg = Ring(c2, pfx + "_sg", [64, 8, 128], BF16, 2)
        f32r = Ring(c2, pfx + "_f32", [128, 512], F32, 8)
        b_ring = Ring(c2, pfx + "_b", [128, 512], F32, 2)
        k_ring = Ring(c2, pfx + "_k", [128, 512], F32, 2)
        qe_ring = Ring(c2, pfx + "_qe", [128, 512], BF16, 2)
        qh_ring = Ring(c2, pfx + "_qh", [128, 512], BF16, 2)
        kh_ring = Ring(c2, pfx + "_kh", [128, 512], BF16, 2)
        kdT_ring = Ring(c2, pfx + "_kdT", [128, 512], BF16, 2)
        kd_ring = Ring(c2, pfx + "_kd", [64, 8, 128], BF16, 2)
        dec_ring = Ring(c2, pfx + "_dec", [128, 8], F32, 2)
        at_ring = Ring(c2, pfx + "_at", [64, 64], BF16, 3)
        sm_ring = Ring(c2, pfx + "_sm", [64, 4], F32, 4)
        junk_ring = Ring(c2, pfx + "_junk", [64, 128], BF16, 2)
        on_ring = Ring(c2, pfx + "_on", [64, 128], F32, 3)
        r_ring = Ring(c2, pfx + "_r", [64, 128], BF16, 3)
        rT_ring = Ring(c2, pfx + "_rT", [128, 512], BF16, 2)

        def issue_loads(sb):
            hq, B_hq, k1 = in_hq.next()
            zf, B_zf, k2 = in_zf.next()
            hiT, B_hiT, k3 = in_hiT.next()
            sgT, B_sgT, k4 = in_sgT.next()
            S.gather_rows(hq[:], d["pa16v4"], c.idx[:, IDX_HQ + sb:IDX_HQ + sb + 1], reads=[c.B_idx], writes=[B_hq], dma=k1)
            S.gather_rows(zf[:], d["pa32v4"], c.idx[:, IDX_ZF + sb:IDX_ZF + sb + 1], reads=[c.B_idx], writes=[B_zf], dma=k2)
            S.gather_rows(hiT[:], d["pa16v4"], c.idx[:, IDX_HIT + sb:IDX_HIT + sb + 1], reads=[c.B_idx], writes=[B_hiT], dma=k3)
            S.gather_rows(sgT[:], d["pa16v4"], c.idx[:, IDX_HGT + sb:IDX_HGT + sb + 1], reads=[c.B_idx], writes=[B_sgT], dma=k4)
            return hq, B_hq, zf, B_zf, hiT, B_hiT, sgT, B_sgT

        nxt = issue_loads(0)
        for sb in range(nsb):
            t0 = sb * 512
            hq, B_hq, zf, B_zf, hiT, B_hiT, sgT, B_sgT = nxt
            if sb + 1 < nsb:
                nxt = issue_loads(sb + 1)
            hi, B_hi, _ = in_hi.next()
            sg, B_sg, _ = in_sg.next()
            for (srcT, B_srcT, dst, B_dst) in ((hiT, B_hiT, hi, B_hi), (sgT, B_sgT, sg, B_sg)):
                ptr, B_ptr, _ = ps_kd.next()
                for cc in range(8):
                    S.op("tensor", "transpose", out=ptr[0:64, cc, :], in_=srcT[:, cc * 64:(cc + 1) * 64], identity=c.ident[:],
                         reads=[B_srcT, c.B_ident], writes=[B_ptr])
                S.op("scalar", "copy", out=dst[:], in_=ptr[0:64, :, :], reads=[B_ptr], writes=[B_dst])
            ez, B_ez, _ = f32r.next()
            S.op("scalar", "activation", out=ez[:], in_=zf[:], func=AF.Exp, scale=-1.0, reads=[B_zf], writes=[B_ez])
            S.op("vector", "tensor_scalar", out=ez[:], in0=ez[:], scalar1=1.0, scalar2=None, op0=ALU.add, reads=[B_ez], writes=[B_ez])
            S.op("vector", "reciprocal", out=ez[:], in_=ez[:], reads=[B_ez], writes=[B_ez])
            f, B_f, _ = f32r.next()
            S.op("vector", "tensor_scalar", out=f[:], in0=ez[:], scalar1=cst[:, 1:2], scalar2=cst[:, 0:1], op0=ALU.mult, op1=ALU.add,
                 reads=[B_ez, B_c], writes=[B_f])
            kk, B_kk, _ = k_ring.next()
            S.op("gpsimd", "tensor_scalar", out=kk[:], in0=f[:], scalar1=-1.0, scalar2=1.0, op0=ALU.mult, op1=ALU.add, reads=[B_f], writes=[B_kk])
            lf, B_lf, _ = f32r.next()
            S.op("vector", "tensor_scalar", out=lf[:], in0=f[:], scalar1=1e-30, scalar2=None, op0=ALU.max, reads=[B_f], writes=[B_lf])
            S.op("scalar", "activation", out=lf[:], in_=lf[:], func=AF.Ln, reads=[B_lf], writes=[B_lf])
            b, B_b, _ = b_ring.next()
            S.op("vector", "tensor_tensor_scan", out=b[:], data0=resetm[:], data1=lf[:], initial=0.0, op0=ALU.mult, op1=ALU.add,
                 reads=[B_c, B_lf], writes=[B_b])
            b3 = b[:].rearrange("p (c t) -> p c t", t=64)
            bmid = b3[:, :, 31:32].to_broadcast([128, 8, 64])
            blast = b3[:, :, 63:64].to_broadcast([128, 8, 64])
            e0, B_e0, _ = f32r.next()
            S.op("scalar", "activation", out=e0[:], in_=b[:], func=AF.Exp, reads=[B_b], writes=[B_e0])
            qe, B_qe, _ = qe_ring.next()
            S.op("gpsimd", "tensor_tensor", out=qe[:], in0=hq[:], in1=e0[:], op=ALU.mult, reads=[B_hq, B_e0], writes=[B_qe])
            d1, B_d1, _ = f32r.next()
            S.op("vector", "tensor_tensor", out=d1[:].rearrange("p (c t) -> p c t", t=64), in0=b3, in1=bmid, op=ALU.subtract, reads=[B_b], writes=[B_d1])
            e1, B_e1, _ = f32r.next()
            S.op("scalar", "activation", out=e1[:], in_=d1[:], func=AF.Exp, reads=[B_d1], writes=[B_e1])
            qh, B_qh, _ = qh_ring.next()
            S.op("vector", "tensor_tensor", out=qh[:], in0=hq[:], in1=e1[:], op=ALU.mult, reads=[B_hq, B_e1], writes=[B_qh])
            e2, B_e2, _ = f32r.next()
            S.op("scalar", "activation", out=e2[:], in_=d1[:], func=AF.Exp, scale=-1.0, reads=[B_d1], writes=[B_e2])
            kh, B_kh, _ = kh_ring.next()
            S.op("gpsimd", "tensor_tensor", out=kh[:], in0=kk[:], in1=e2[:], op=ALU.mult, reads=[B_kk, B_e2], writes=[B_kh])
            d3, B_d3, _ = f32r.next()
            S.op("vector", "tensor_tensor", out=d3[:].rearrange("p (c t) -> p c t", t=64), in0=blast, in1=b3, op=ALU.subtract, reads=[B_b], writes=[B_d3])
            S.op("scalar", "activation", out=d3[:], in_=d3[:], func=AF.Exp, reads=[B_d3], writes=[B_d3])
            kdT, B_kdT, _ = kdT_ring.next()
            S.op("vector", "tensor_tensor", out=kdT[:], in0=kk[:], in1=d3[:], op=ALU.mult, reads=[B_kk, B_d3], writes=[B_kdT])
            dec, B_dec, _ = dec_ring.next()
            S.op("scalar", "activation", out=dec[:].rearrange("p (c o) -> p c o", o=1), in_=b3[:, :, 63:64], func=AF.Exp, reads=[B_b], writes=[B_dec])
            pkd, B_pkd, _ = ps_kd.next()
            for cc in range(8):
                S.op("tensor", "transpose", out=pkd[0:64, cc, :], in_=kdT[:, cc * 64:(cc + 1) * 64], identity=c.ident[:], reads=[B_kdT, c.B_ident], writes=[B_pkd])
            kd, B_kd, _ = kd_ring.next()
            S.op("scalar", "copy", out=kd[:], in_=pkd[0:64, :, :], reads=[B_pkd], writes=[B_kd])
            prt, B_prt, _ = ps_rt.next()
            for cc in range(8):
                cs = slice(cc * 64, (cc + 1) * 64)
                pat, B_pat, _ = ps_at.next()
                S.op("tensor", "matmul", out=pat[0:64, 0:64], lhsT=kh[:, cs], rhs=qh[:, cs], start=True, stop=True, reads=[B_kh, B_qh], writes=[B_pat])
                at, B_at, _ = at_ring.next()
                S.op("vector", "tensor_tensor", out=at[:], in0=pat[0:64, 0:64], in1=hmask[:], op=ALU.mult, reads=[B_pat, B_c], writes=[B_at])
                po, B_po, _ = ps_o.next()
                S.op("tensor", "matmul", out=po[0:64, 0:128], lhsT=at[:], rhs=hi[:, cc, :], start=True, stop=False, reads=[B_at, B_hi], writes=[B_po])
                S.op("tensor", "matmul", out=po[0:64, 0:128], lhsT=qe[:, cs], rhs=state_bf[:], start=False, stop=True, reads=[B_qe, B_sbf], writes=[B_po])
                pst, B_pst, _ = ps_st.next()
                S.op("tensor", "matmul", out=pst[:, 0:128], lhsT=kd[:, cc, :], rhs=hi[:, cc, :], start=True, stop=True, reads=[B_kd, B_hi], writes=[B_pst])
                S.op("vector", "scalar_tensor_tensor", out=state[:], in0=state[:], scalar=dec[:, cc:cc + 1], in1=pst[:, 0:128], op0=ALU.mult, op1=ALU.add,
                     reads=[B_state, B_dec, B_pst], writes=[B_state])
                S.op("scalar", "copy", out=state_bf[:], in_=state[:], reads=[B_state], writes=[B_sbf])
                sm, B_sm, _ = sm_ring.next()
                jk, B_jk, _ = junk_ring.next()
                S.op("scalar", "activation", out=jk[:], in_=po[0:64, 0:128], func=AF.Square, accum_out=sm[:, 0:1], reads=[B_po], writes=[B_jk, B_sm])
                S.op("scalar", "activation", out=sm[:, 1:2], in_=sm[:, 0:1], func=AF.Sqrt, scale=1.0 / 128.0, bias=c.eps_t[0:64, 0:1],
                     reads=[B_sm, c.B_eps], writes=[B_sm])
                S.op("vector", "reciprocal", out=sm[:, 2:3], in_=sm[:, 1:2], reads=[B_sm], writes=[B_sm])
                on, B_on, _ = on_ring.next()
                S.op("scalar", "activation", out=on[:], in_=po[0:64, 0:128], func=AF.Copy, scale=sm[:, 2:3], reads=[B_po, B_sm], writes=[B_on])
                r, B_r, _ = r_ring.next()
                S.op("gpsimd", "tensor_tensor", out=r[:], in0=on[:], in1=sg[:, cc, :], op=ALU.mult, reads=[B_on, B_sg], writes=[B_r])
                S.op("tensor", "transpose", out=prt[:, cc, 0:64], in_=r[:], identity=c.ident[0:64, 0:64], reads=[B_r, c.B_ident], writes=[B_prt])
            rT, B_rT, key = rT_ring.next()
            S.op("scalar", "activation", out=rT[:].rearrange("p (c t) -> p c t", t=64), in_=prt[:, :, 0:64], func=AF.Copy, scale=gn[:, 0:1],
                 reads=[B_prt, B_c], writes=[B_rT])
            final.append(S.op("sync", "dma_start", out=RT_d[:, t0:t0 + 512], in_=rT[:], reads=[B_rT], dma=key))
        S.barrier()
    return final


NT = NTOK // 128


def load_x_resident(c, x_d, name="xres"):
    c.x = c.sb(name, [128, NT, D], F32)
    c.B_x = [Buf("x%d" % t) for t in range(NT)]
    for t in range(NT):
        c.S.op("sync", "dma_start", out=c.x[:, t, :], in_=x_d[t * 128:(t + 1) * 128, :], writes=[c.B_x[t]], dma="%s_%d" % (name, t))


def wout_step(c, mixv, w_out_d, fT, B_fT):
    S = c.S
    with contextlib.ExitStack() as st2:
        c2 = Ctx(c.nc, S, st2)
        wout = c2.sb(c.key("wout"), [128, 8, D], BF16)
        B_wout = Buf()
        kwo = c.key("woutl")
        for k in range(8):
            S.gather_rows(fT[:, k, :], mixv, c.idx[:, IDX_MIX + k:IDX_MIX + k + 1], reads=[c.B_idx], writes=[B_fT], dma=c.key("mixT"))
        for k in range(8):
            S.op("gpsimd", "dma_start", out=wout[:, k, :], in_=w_out_d[k * 128:(k + 1) * 128, :], writes=[B_wout], dma=kwo)
        for t in range(NT):
            for half in range(2):
                ps, B_ps, _ = c.psF.next()
                for k in range(8):
                    S.op("tensor", "matmul", out=ps[:], lhsT=fT[:, k, t * 128:(t + 1) * 128], rhs=wout[:, k, half * 512:(half + 1) * 512],
                         start=(k == 0), stop=(k == 7), reads=[B_fT, B_wout], writes=[B_ps])
                xs = c.x[:, t, half * 512:(half + 1) * 512]
                S.op("vector", "tensor_tensor", out=xs, in0=ps[:], in1=xs, op=ALU.add, reads=[B_ps, c.B_x[t]], writes=[c.B_x[t]])
        S.barrier()


def load_bcast_row(c, name, row_d, n):
    t = c.sb(name, [128, n], F32)
    B = Buf(name)
    c.S.op("sync", "dma_start", out=t[:], in_=row_d.partition_broadcast(128), writes=[B], dma=name)
    return t, B


def ffn_norm_step(c, gain_row_d, fT, B_fT, router_T_d=None):
    S = c.S
    G = None
    B_G = None
    if router_T_d is not None:
        G = c.sb(c.key("G"), [128, NT, 8], F32)
        B_G = Buf()
    with contextlib.ExitStack() as st2:
        c2 = Ctx(c.nc, S, st2)
        c2.dkey = c.dkey + 5000
        gain, B_gain = load_bcast_row(c2, c.key("fgain"), gain_row_d, D)
        junk = Ring(c2, c.key("fjunk"), [128, D], BF16, 2)
        small = Ring(c2, c.key("fsmall"), [128, 4], F32, 4)
        xn_r = Ring(c2, c.key("fxn"), [128, D], BF16, 2)
        if router_T_d is not None:
            wrg = c2.sb(c.key("wrg"), [128, 8, D], F32)
            B_wrg = Buf()
            kwr = c.key("wrgl")
            for e in range(8):
                S.op("sync", "dma_start", out=wrg[:, e, :], in_=router_T_d[e:e + 1, :].partition_broadcast(128), writes=[B_wrg], dma=kwr)
            for e in range(8):
                S.op("gpsimd", "tensor_tensor", out=wrg[:, e, :], in0=wrg[:, e, :], in1=gain[:], op=ALU.mult, reads=[B_wrg, B_gain], writes=[B_wrg])
            rj = Ring(c2, c.key("rjunk"), [128, D], F32, 2)
            lg_r = Ring(c2, c.key("lg"), [128, 32], F32, 3)
        for t in range(NT):
            jk, B_jk, _ = junk.next()
            sm, B_sm, _ = small.next()
            xn, B_xn, _ = xn_r.next()
            xt = c.x[:, t, :]
            S.op("scalar", "activation", out=jk[:], in_=xt, func=AF.Square, accum_out=sm[:, 0:1], reads=[c.B_x[t]], writes=[B_jk, B_sm])
            S.op("scalar", "activation", out=sm[:, 1:2], in_=sm[:, 0:1], func=AF.Sqrt, scale=1.0 / D, bias=c.eps_t[:, 0:1], reads=[B_sm, c.B_eps], writes=[B_sm])
            S.op("vector", "reciprocal", out=sm[:, 2:3], in_=sm[:, 1:2], reads=[B_sm], writes=[B_sm])
            S.op("vector", "scalar_tensor_tensor", out=xn[:], in0=xt, scalar=sm[:, 2:3], in1=gain[:], op0=ALU.mult, op1=ALU.mult,
                 reads=[c.B_x[t], B_sm, B_gain], writes=[B_xn])
            pT, B_pT, _ = c.psT.next()
            for k in range(8):
                S.op("tensor", "transpose", out=pT[:, k, :], in_=xn[:, k * 128:(k + 1) * 128], identity=c.ident[:], reads=[B_xn, c.B_ident], writes=[B_pT])
            S.op("scalar", "copy", out=fT[:, :, t * 128:(t + 1) * 128], in_=pT[:], reads=[B_pT], writes=[B_fT])
            if router_T_d is not None:
                lg, B_lg, _ = lg_r.next()
                S.op("vector", "memset", ap=lg[:], constant=0.0, writes=[B_lg])
                for e in range(8):
                    r, B_r, _ = rj.next()
                    S.op("vector", "scalar_tensor_tensor", out=r[:], in0=xt, scalar=sm[:, 2:3], in1=wrg[:, e, :], op0=ALU.mult, op1=ALU.mult,
                         accum_out=lg[:, e:e + 1], reads=[c.B_x[t], B_sm, B_wrg], writes=[B_r, B_lg])
                S.op("vector", "max", out=lg[:, 8:16], in_=lg[:, 0:8], reads=[B_lg], writes=[B_lg])
                S.op("vector", "tensor_tensor", out=lg[:, 16:17], in0=lg[:, 9:10], in1=lg[:, 8:9], op=ALU.subtract, reads=[B_lg], writes=[B_lg])
                S.op("scalar", "activation", out=lg[:, 16:17], in_=lg[:, 16:17], func=AF.Exp, reads=[B_lg], writes=[B_lg])
                S.op("vector", "tensor_scalar", out=lg[:, 17:18], in0=lg[:, 16:17], scalar1=1.0, scalar2=None, op0=ALU.add, reads=[B_lg], writes=[B_lg])
                S.op("vector", "reciprocal", out=lg[:, 17:18], in_=lg[:, 17:18], reads=[B_lg], writes=[B_lg])
                S.op("vector", "tensor_scalar", out=lg[:, 18:19], in0=lg[:, 17:18], scalar1=-1.0, scalar2=1.0, op0=ALU.mult, op1=ALU.add, reads=[B_lg], writes=[B_lg])
                S.op("vector", "tensor_scalar", out=lg[:, 20:28], in0=lg[:, 0:8], scalar1=lg[:, 8:9], scalar2=lg[:, 17:18], op0=ALU.is_equal, op1=ALU.mult,
                     reads=[B_lg], writes=[B_lg])
                S.op("vector", "tensor_scalar", out=G[:, t, :], in0=lg[:, 0:8], scalar1=lg[:, 9:10], scalar2=lg[:, 18:19], op0=ALU.is_equal, op1=ALU.mult,
                     reads=[B_lg], writes=[B_G])
                S.op("vector", "tensor_tensor", out=G[:, t, :], in0=G[:, t, :], in1=lg[:, 20:28], op=ALU.add, reads=[B_lg, B_G], writes=[B_G])
        S.barrier()
    return G, B_G


class FfnBufs:
    def __init__(self, c2):
        self.wg = Ring(c2, c2.key("wg"), [128, 8, 512], BF16, 2)
        self.wu = Ring(c2, c2.key("wu"), [128, 8, 512], BF16, 2)
        self.wd = Ring(c2, c2.key("wd"), [128, 4, D], BF16, 2)
        self.act = Ring(c2, c2.key("act"), [128, 4, TB], BF16, 2)
        self.sg = Ring(c2, c2.key("sgate"), [128, TB], F32, 3)


def swiglu_accumulate(c, fb, fT, B_fT, wg_d, wu_d, wd_d, FF, G=None, B_G=None, e=None):
    S = c.S
    g0 = 0
    while g0 < FF:
        gw = min(512, FF - g0)
        nch = gw // 128
        wg, B_wg, k1 = fb.wg.next()
        wu, B_wu, k2 = fb.wu.next()
        wd, B_wd, k3 = fb.wd.next()
        S.op("gpsimd", "dma_start", out=wg[:, :, 0:gw], in_=wg_d[:, g0:g0 + gw].rearrange("(k p) c -> p k c", p=128), writes=[B_wg], dma=k1)
        S.op("gpsimd", "dma_start", out=wu[:, :, 0:gw], in_=wu_d[:, g0:g0 + gw].rearrange("(k p) c -> p k c", p=128), writes=[B_wu], dma=k2)
        S.op("gpsimd", "dma_start", out=wd[:, 0:nch, :], in_=wd_d[g0:g0 + gw, :].rearrange("(c p) n -> p c n", p=128), writes=[B_wd], dma=k3)
        for tb in range(NTB):
            ts = slice(tb * TB, (tb + 1) * TB)
            act, B_act, _ = fb.act.next()
            for cch in range(nch):
                psg, B_psg, _ = c.psF.next()
                for k in range(8):
                    S.op("tensor", "matmul", out=psg[:], lhsT=wg[:, k, cch * 128:(cch + 1) * 128], rhs=fT[:, k, ts], start=(k == 0), stop=(k == 7),
                         reads=[B_wg, B_fT], writes=[B_psg])
                psu, B_psu, _ = c.psF.next()
                for k in range(8):
                    S.op("tensor", "matmul", out=psu[:], lhsT=wu[:, k, cch * 128:(cch + 1) * 128], rhs=fT[:, k, ts], start=(k == 0), stop=(k == 7),
                         reads=[B_wu, B_fT], writes=[B_psu])
                sg, B_sg, _ = fb.sg.next()
                S.op("scalar", "activation", out=sg[:], in_=psg[:], func=AF.Silu, reads=[B_psg], writes=[B_sg])
                S.op("vector", "tensor_tensor", out=act[:, cch, :], in0=psu[:], in1=sg[:], op=ALU.mult, reads=[B_psu, B_sg], writes=[B_act])
            for j in range(4):
                t = tb * 4 + j
                for half in range(2):
                    psy, B_psy, _ = c.psF.next()
                    for cch in range(nch):
                        S.op("tensor", "matmul", out=psy[:], lhsT=act[:, cch, j * 128:(j + 1) * 128], rhs=wd[:, cch, half * 512:(half + 1) * 512],
                             start=(cch == 0), stop=(cch == nch - 1), reads=[B_act, B_wd], writes=[B_psy])
                    xs = c.x[:, t, half * 512:(half + 1) * 512]
                    if G is None:
                        S.op("vector", "tensor_tensor", out=xs, in0=psy[:], in1=xs, op=ALU.add, reads=[B_psy, c.B_x[t]], writes=[c.B_x[t]])
                    else:
                        S.op("vector", "scalar_tensor_tensor", out=xs, in0=psy[:], scalar=G[:, t, e:e + 1], in1=xs, op0=ALU.mult, op1=ALU.add,
                             reads=[B_psy, c.B_x[t], B_G], writes=[c.B_x[t]])
        g0 += gw


def final_norm_store(c, gain_row_d, out_d):
    S = c.S
    final = []
    with contextlib.ExitStack() as st2:
        c2 = Ctx(c.nc, S, st2)
        gain, B_gain = load_bcast_row(c2, c.key("fin_gain"), gain_row_d, D)
        junk = Ring(c2, c.key("finjunk"), [128, D], BF16, 2)
        small = Ring(c2, c.key("finsmall"), [128, 4], F32, 4)
        o_r = Ring(c2, c.key("fino"), [128, D], F32, 3)
        for t in range(NT):
            jk, B_jk, _ = junk.next()
            sm, B_sm, _ = small.next()
            xt = c.x[:, t, :]
            S.op("scalar", "activation", out=jk[:], in_=xt, func=AF.Square, accum_out=sm[:, 0:1], reads=[c.B_x[t]], writes=[B_jk, B_sm])
            S.op("scalar", "activation", out=sm[:, 1:2], in_=sm[:, 0:1], func=AF.Sqrt, scale=1.0 / D, bias=c.eps_t[:, 0:1], reads=[B_sm, c.B_eps], writes=[B_sm])
            S.op("vector", "reciprocal", out=sm[:, 2:3], in_=sm[:, 1:2], reads=[B_sm], writes=[B_sm])
            o, B_o, key = o_r.next()
            S.op("vector", "scalar_tensor_tensor", out=o[:], in0=xt, scalar=sm[:, 2:3], in1=gain[:], op0=ALU.mult, op1=ALU.mult,
                 reads=[c.B_x[t], B_sm, B_gain], writes=[B_o])
            final.append(S.op("sync", "dma_start", out=out_d[t * 128:(t + 1) * 128, :], in_=o[:], reads=[B_o], dma=key))
        S.barrier()
    return final


def phase_b(c, d, layer):
    S = c.S
    fT = c.sb(c.key("fT"), [128, 8, NTOK], BF16)
    B_fT = Buf()
    wout_step(c, d["mixv"], d["w_out"], fT, B_fT)
    moe = (layer % 2 == 1)
    G, B_G = ffn_norm_step(c, d["ffn_norm"], fT, B_fT, d["router_T"] if moe else None)
    with contextlib.ExitStack() as st2:
        c2 = Ctx(c.nc, S, st2)
        c2.dkey = c.dkey + 7000
        fb = FfnBufs(c2)
        if not moe:
            swiglu_accumulate(c, fb, fT, B_fT, d["wg"], d["wu"], d["wd"], 2816)
        else:
            for e in range(8):
                swiglu_accumulate(c, fb, fT, B_fT, d["mwg"][e], d["mwu"][e], d["mwd"][e], 3584, G, B_G, e)
        S.barrier()


U32 = mybir.dt.uint32


def make_idx(core):
    b, hj = core // 4, core % 4
    p = np.arange(128, dtype=np.int64)
    idx = np.zeros((128, IDX_N), np.int64)
    for j in range(4):
        r = 4 * b + j
        idx[:, IDX_QN + j] = r * PA16_ROWS + PA16_BASE["QN"] + hj * 128 + p
        idx[:, IDX_KN + j] = r * PA16_ROWS + PA16_BASE["KN"] + hj * 128 + p
        idx[:, IDX_VT + j] = r * PA16_ROWS + PA16_BASE["VT"] + hj * 128 + p
        idx[:64, IDX_QP + j] = r * PA16_ROWS + PA16_BASE["QP"] + hj * 64 + p[:64]
        idx[:64, IDX_KPE + j] = r * PA16_ROWS + PA16_BASE["KPE"] + p[:64]
    for sb in range(16):
        r = 4 * b + sb // 4
        q = sb % 4
        for name, col in (("HQ", IDX_HQ), ("HIT", IDX_HIT), ("HGT", IDX_HGT)):
            idx[:, col + sb] = (r * PA16_ROWS + PA16_BASE[name] + hj * 128 + p) * 4 + q
        idx[:, IDX_ZF + sb] = (r * 512 + hj * 128 + p) * 4 + q
    for k in range(8):
        part, h2 = k // 4, k % 4
        idx[:, IDX_MIX + k] = ((4 * b + h2) * 256 + part * 128 + p) * 4 + hj
    return idx.astype(np.uint32)


def tagged_in(nc, name, rows, cols):
    return dram_in(nc, name, [rows + 1, cols], F32)[0:rows, :]


def tag_rows(a2d, core):
    return np.concatenate([a2d, np.full((1, a2d.shape[1]), float(core), np.float32)], axis=0)


TAGGED = ("w_in0", "w_in1", "w_q_b0", "w_q_b1", "w_kv_b0", "w_kv_b1", "w_out0", "w_out1", "wg", "wu", "wd", "mwg", "mwu", "mwd")


def build_fused():
    nc = bass.Bass("TRN2", target_bir_lowering=False)
    x_d = dram_in(nc, "x", [NTOK, D], F32)
    pos_d = dram_in(nc, "pos", [1, NTOK], I32)
    ident_d = dram_in(nc, "ident", [128, 128], BF16)
    ones_d = dram_in(nc, "ones", [128, 128], BF16)
    inv_d = dram_in(nc, "inv", [64, 1], F32)
    idx_d = dram_in(nc, "idx", [128, IDX_N], U32)
    md = {"cmask": dram_in(nc, "cmask", [128, 128], BF16), "hmask": dram_in(nc, "hmask", [64, 64], F32),
          "resetm": dram_in(nc, "resetm", [128, 512], F32), "lbraw": dram_in(nc, "lbraw", [128, 2], F32)}
    gn_d = [dram_in(nc, "gn%d" % l, [128, 1], F32) for l in range(2)]
    W = []
    for l in range(2):
        W.append({"w_in": tagged_in(nc, "w_in%d" % l, D, IN_COLS), "mix_norm": dram_in(nc, "mix_norm%d" % l, [D], F32),
                  "w_q_b": tagged_in(nc, "w_q_b%d" % l, 384, 768), "q_a_norm": dram_in(nc, "q_a_norm%d" % l, [384], F32),
                  "w_kv_b": tagged_in(nc, "w_kv_b%d" % l, 256, 1024), "kv_a_norm": dram_in(nc, "kv_a_norm%d" % l, [256], F32),
                  "w_out": tagged_in(nc, "w_out%d" % l, D, D), "ffn_norm": dram_in(nc, "ffn_norm%d" % l, [1, D], F32)})
    W[0].update({"wg": tagged_in(nc, "wg", D, 2816), "wu": tagged_in(nc, "wu", D, 2816), "wd": tagged_in(nc, "wd", 2816, D)})
    W[1].update({"router_T": dram_in(nc, "router_T", [8, D], F32),
                 "mwg": tagged_in(nc, "mwg", 8 * D, 3584).rearrange("(e r) c -> e r c", e=8),
                 "mwu": tagged_in(nc, "mwu", 8 * D, 3584).rearrange("(e r) c -> e r c", e=8),
                 "mwd": tagged_in(nc, "mwd", 8 * 3584, D).rearrange("(e r) c -> e r c", e=8)})
    fin_d = dram_in(nc, "final_norm", [1, D], F32)
    out_d = nc.dram_tensor("OUT", [NTOK, D], F32, kind="ExternalOutput").ap()
    pa16 = nc.dram_tensor("pa16", [PA16_ROWS, NTOK], BF16).ap()
    pa32 = nc.dram_tensor("pa32", [512, NTOK], F32).ap()
    pa16all = nc.dram_tensor("pa16all", [8 * PA16_ROWS, NTOK], BF16).ap()
    pa32all = nc.dram_tensor("pa32all", [8 * 512, NTOK], F32).ap()
    mix = nc.dram_tensor("mixloc", [256, SEQ], BF16).ap()
    mixall = nc.dram_tensor("mixall", [8 * 256, SEQ], BF16).ap()
    xo_d = nc.dram_tensor("xo", [NTOK, D], F32).ap()
    pav = pa_views(pa16, pa32)
    md["pa16v"] = pa16all
    md["pa16v4"] = pa16all.rearrange("r (q c) -> (r q) c", q=4)
    md["pa32v4"] = pa32all.rearrange("r (q c) -> (r q) c", q=4)
    mixv = mixall.rearrange("r (q c) -> (r q) c", q=4)

    S = Sched(nc)
    c = new_ctx(nc, S)
    final = []
    with c.st:
        load_consts(c, ident_d, ones_d)
        c.idx = c.sb("idx", [128, IDX_N], U32)
        c.B_idx = Buf()
        S.op("sync", "dma_start", out=c.idx[:], in_=idx_d, writes=[c.B_idx], dma="idx")
        rope_tables(c, pos_d, inv_d)
        for l in range(2):
            w = W[l]
            with contextlib.ExitStack() as st:
                ca = c.sub(st)
                load_phase_a_weights(ca, w["w_in"], w["mix_norm"], w["w_q_b"], w["q_a_norm"], w["w_kv_b"], w["kv_a_norm"])
                x_ring = Ring(ca, ca.key("xin"), [128, D], F32, 3)
                xsrc_d = x_d if l == 0 else xo_d

                def x_src(t, x_ring=x_ring, xsrc_d=xsrc_d):
                    xt, B_xt, key = x_ring.next()
                    S.op("sync", "dma_start", out=xt[:], in_=xsrc_d[t * 128:(t + 1) * 128, :], writes=[B_xt], dma=key)
                    return xt[:], B_xt
                phase_a(ca, x_src, pav)
                S.barrier()
            S.all_gather(pa16, pa16all, dma=c.key("ag"))
            S.all_gather(pa32, pa32all, dma=c.key("ag"))
            S.barrier()
            md["gn"] = gn_d[l]
            attention(c, md, mix[0:128, :], pfx="a%d" % l)
            hgrn(c, md, mix[128:256, :], l, pfx="h%d" % l)
            S.all_gather(mix, mixall, dma=c.key("ag"))
            S.barrier()
            with contextlib.ExitStack() as st:
                cb = c.sub(st)
                load_x_resident(cb, x_d if l == 0 else xo_d, "xr%d" % l)
                d = dict(w)
                d["mixv"] = mixv
                phase_b(cb, d, l)
                if l == 1:
                    final = final_norm_store(cb, fin_d, out_d)
                else:
                    for t in range(NT):
                        S.op("sync", "dma_start", out=xo_d[t * 128:(t + 1) * 128, :], in_=cb.x[:, t, :], reads=[cb.B_x[t]], dma="xo%d" % t)
                    S.barrier()
        S.emit(final_waits=final)
    return nc


_PROG = {}


def _c(a):
    return np.ascontiguousarray(a)


def kernel(x, positions, mix_norm, w_in, q_a_norm, w_q_b, kv_a_norm, w_kv_b, hg_lower_bounds, hg_out_norm, w_out, ffn_norm,
           dense_w_gate, dense_w_up, dense_w_down, moe_router, moe_w_gate, moe_w_up, moe_w_down, final_norm):
    f32 = lambda a: np.asarray(a, dtype=np.float32)
    consts = const_inputs()
    mc = mixer_consts()
    xf = f32(x).reshape(-1, D)
    posf = np.asarray(positions).reshape(-1).astype(np.int32)
    shared = {"ident": consts["ident"], "ones": consts["ones"], "inv": consts["inv"], "cmask": mc["cmask"], "hmask": mc["hmask"], "resetm": mc["resetm"]}
    for l in range(2):
        shared["w_in%d" % l] = _c(f32(w_in)[l])
        shared["mix_norm%d" % l] = _c(f32(mix_norm)[l])
        shared["w_q_b%d" % l] = _c(f32(w_q_b)[l])
        shared["q_a_norm%d" % l] = _c(f32(q_a_norm)[l])
        shared["w_kv_b%d" % l] = _c(f32(w_kv_b)[l])
        shared["kv_a_norm%d" % l] = _c(f32(kv_a_norm)[l])
        shared["w_out%d" % l] = _c(f32(w_out)[l])
        shared["ffn_norm%d" % l] = _c(f32(ffn_norm)[l]).reshape(1, D)
        shared["gn%d" % l] = _c(f32(hg_out_norm)[l]).reshape(128, 1)
    shared["wg"] = _c(f32(dense_w_gate)[0])
    shared["wu"] = _c(f32(dense_w_up)[0])
    shared["wd"] = _c(f32(dense_w_down)[0])
    shared["router_T"] = _c(f32(moe_router)[0].T)
    shared["mwg"] = _c(f32(moe_w_gate)[0])
    shared["mwu"] = _c(f32(moe_w_up)[0])
    shared["mwd"] = _c(f32(moe_w_down)[0])
    shared["final_norm"] = _c(f32(final_norm)).reshape(1, D)
    lb = f32(hg_lower_bounds)
    maps = []
    for core in range(8):
        m = dict(shared)
        for n in TAGGED:
            a = shared[n]
            m[n] = tag_rows(a.reshape(-1, a.shape[-1]), core)
        m["x"] = _c(xf[core * NTOK:(core + 1) * NTOK])
        m["pos"] = _c(posf[core * NTOK:(core + 1) * NTOK]).reshape(1, NTOK)
        m["idx"] = make_idx(core)
        h = core % 4
        m["lbraw"] = _c(lb[:, h * 128:(h + 1) * 128].T)
        maps.append(m)
    if "nc" not in _PROG:
        _PROG["nc"] = build_fused()
    res = run_bass_kernel_spmd(_PROG["nc"], maps, core_ids=list(range(8)))
    out = np.concatenate([np.asarray(res.results[c]["OUT"], dtype=np.float32) for c in range(8)], axis=0)
    return out.reshape(2, SEQ, D)
```

```python
import contextlib
import math
import numpy as np
import ml_dtypes
import concourse.bass as bass
import concourse.mybir as mybir
from concourse.bass_utils import run_bass_kernel_spmd

F32 = mybir.dt.float32
BF16 = mybir.dt.bfloat16
I32 = mybir.dt.int32
AF = mybir.ActivationFunctionType
ALU = mybir.AluOpType
AX = mybir.AxisListType
NPBF = ml_dtypes.bfloat16

D = 1024
NTOK = 2048
SEQ = 8192
TB = 512
NTB = NTOK // TB
EPS = 1e-6
SCALE = 192.0 ** -0.5
IN_COLS = 2752
SEM_CAP = 12000
DUMP = False
SCOPES = False
TWO_PI = 2.0 * math.pi
C1 = 6.28125
C2 = TWO_PI - C1


class Buf:
    __slots__ = ("name", "w", "readers", "psum")

    def __init__(self, name="", psum=False):
        self.name = name
        self.w = None
        self.readers = {}
        self.psum = psum


class Op:
    __slots__ = ("eng", "fn", "deps", "signal", "sem", "val", "dma", "idx", "gidx", "inc", "scope")


class Sched:
    ENGS = ("sync", "scalar", "vector", "gpsimd", "tensor")

    def __init__(self, nc):
        self.nc = nc
        self.q = {e: [] for e in self.ENGS}
        self.n = 0
        self.dmas_since_barrier = []
        self.barrier_marks = []
        self.scope = None

    def add(self, eng, fn, reads=(), writes=(), dma=None, after=(), inc=16):
        op = Op()
        op.inc = inc
        op.scope = self.scope
        op.eng = eng
        op.fn = fn
        op.signal = False
        op.sem = None
        op.val = None
        op.dma = dma
        op.gidx = self.n
        self.n += 1
        deps = {}

        def dep(p):
            if p is None:
                return
            if p.dma is None and p.eng == eng and eng == "tensor" and dma is None:
                return
            key = ("dma", id(p)) if p.dma is not None else p.eng
            o = deps.get(key)
            if o is None or o.gidx < p.gidx:
                deps[key] = p

        for r in reads:
            dep(r.w)
        for w in writes:
            dep(w.w)
            for p in w.readers.values():
                if p.dma is None and p.eng == eng and dma is None:
                    continue
                dep(p)
        for p in after:
            dep(p)
        for p in deps.values():
            p.signal = True
        op.deps = list(deps.values())
        for r in reads:
            key = ("dma", id(op)) if dma is not None else eng
            r.readers[key] = op
            assert not (r.psum and len(r.readers) > 1), "PSUM tile %s read by two engines" % r.name
        for w in writes:
            w.w = op
            w.readers = {}
        if dma is not None:
            op.signal = True
            self.dmas_since_barrier.append(op)
        op.idx = len(self.q[eng])
        self.q[eng].append(op)
        return op

    def op(self, eng, meth, reads=(), writes=(), dma=None, after=(), inc=16, **kw):
        if meth == "dma_nc":
            def fn(e, kw=kw):
                with self.nc.allow_non_contiguous_dma(reason="tiny strided vector"):
                    return e.dma_start(**kw)
        else:
            def fn(e, meth=meth, kw=kw):
                return getattr(e, meth)(**kw)
        return self.add(eng, fn, reads=reads, writes=writes, dma=dma, after=after, inc=inc)

    def gather_rows(self, out, src, idx_col, reads=(), writes=(), dma=None):
        def fn(e, out=out, src=src, idx_col=idx_col):
            return e.indirect_dma_start(out=out, out_offset=None, in_=src, in_offset=bass.IndirectOffsetOnAxis(ap=idx_col, axis=0))
        return self.add("gpsimd", fn, reads=reads, writes=writes, dma=dma)

    def all_gather(self, src, dst, reads=(), writes=(), dma=None):
        def fn(e, src=src, dst=dst):
            return e.collective_compute("AllGather", ALU.bypass, replica_groups=[list(range(8))], ins=[src], outs=[dst])
        return self.add("gpsimd", fn, reads=reads, writes=writes, dma=dma, inc=1)

    def barrier(self):
        lasts = [self.q[e][-1] for e in self.ENGS if self.q[e] and self.q[e][-1].dma is None]
        tails = []
        for e in ("scalar", "vector", "gpsimd", "tensor"):
            ops = [o for o in self.q[e] if o.dma is None]
            if ops:
                tails.append(ops[-1])
        dmas = list(self.dmas_since_barrier)
        self.dmas_since_barrier = []
        self.barrier_marks.append(self.n)
        for e in self.ENGS:
            self.add(e, lambda eng: eng.nop(), after=tails + dmas)

    def emit(self, final_waits=()):
        nc = self.nc
        sems = []
        stack = contextlib.ExitStack()
        with stack:
            def newsem(name):
                s = stack.enter_context(nc.semaphore(name))
                sems.append(s)
                return s
            dsem = {}
            free = []
            marks = list(self.barrier_marks)
            allops = sorted((op for e in self.ENGS for op in self.q[e] if op.dma is not None), key=lambda o: o.gidx)
            for op in allops:
                while marks and op.gidx >= marks[0]:
                    marks.pop(0)
                    free.extend(dsem.values())
                    dsem = {}
                ent = dsem.get(op.dma)
                if ent is None:
                    ent = free.pop() if free else [newsem("d%d" % len(sems)), 0]
                    dsem[op.dma] = ent
                ent[1] += op.inc
                op.sem = ent[0]
                op.val = ent[1]
            for e in self.ENGS:
                cur = None
                cnt = 0
                k = 0
                for op in self.q[e]:
                    if op.dma is None and op.signal:
                        if cur is None or cnt >= SEM_CAP:
                            cur = newsem("c_%s_%d" % (e, k))
                            k += 1
                            cnt = 0
                        cnt += 1
                        op.sem = cur
                        op.val = cnt
            self.nsems = len(sems)
            with nc.Block() as block:
                def run(e):
                    def body(eng):
                        known = {}
                        for op in self.q[e]:
                            best = {}
                            for p in op.deps:
                                o = best.get(id(p.sem))
                                if o is None or o.val < p.val:
                                    best[id(p.sem)] = p
                            for p in best.values():
                                kk = id(p.sem)
                                if known.get(kk, 0) < p.val:
                                    eng.wait_ge(p.sem, p.val)
                                    known[kk] = p.val
                                    if DUMP:
                                        print(e, "  wait", p.sem, p.val)
                            if SCOPES and op.scope is not None:
                                with nc.named_scope(op.scope):
                                    ins = op.fn(eng)
                            else:
                                ins = op.fn(eng)
                            if op.signal:
                                ins.then_inc(op.sem, op.inc if op.dma is not None else 1)
                            if DUMP:
                                print(e, "op", op.gidx, "signal" if op.signal else "", op.sem if op.signal else "", op.val if op.signal else "")
                        if e == "sync":
                            for p in final_waits:
                                eng.wait_ge(p.sem, p.val)
                    return body
                block.sync(run("sync"))
                block.scalar(run("scalar"))
                block.vector(run("vector"))
                block.gpsimd(run("gpsimd"))
                block.tensor(run("tensor"))


class Ring:
    def __init__(self, ctx, name, shape, dt, n, psum=False):
        self.tiles = []
        for i in range(n):
            if psum:
                t = ctx.st.enter_context(ctx.nc.psum_tensor("p_%s%d" % (name, i), shape, dt))
            else:
                t = ctx.st.enter_context(ctx.nc.sbuf_tensor("s_%s%d" % (name, i), shape, dt))
            self.tiles.append((t, Buf("%s%d" % (name, i), psum=psum)))
        self.i = 0
        self.name = name

    @classmethod
    def of(cls, name, tiles):
        r = cls.__new__(cls)
        r.tiles = list(tiles)
        r.i = 0
        r.name = name
        return r

    def next(self):
        t = self.tiles[self.i % len(self.tiles)]
        slot = self.i % len(self.tiles)
        self.i += 1
        return t[0], t[1], "%s_%d" % (self.name, slot)


class Ctx:
    gkey = 0
    SHARED = ("ident", "B_ident", "ones", "B_ones", "psT", "psF", "eps_t", "B_eps", "cosT", "sinT", "B_cos", "B_sin", "idx", "B_idx")

    def sub(self, st):
        c2 = Ctx(self.nc, self.S, st)
        for a in Ctx.SHARED:
            if hasattr(self, a):
                setattr(c2, a, getattr(self, a))
        return c2

    def __init__(self, nc, S, st):
        self.nc = nc
        self.S = S
        self.st = st
        self.dkey = 0

    def sb(self, name, shape, dt):
        return self.st.enter_context(self.nc.sbuf_tensor("s_" + name, shape, dt))

    def key(self, base):
        Ctx.gkey += 1
        return "%s_%d" % (base, Ctx.gkey)


def new_ctx(nc, S):
    st = contextlib.ExitStack()
    return Ctx(nc, S, st)


def load_consts(c, ident_d, ones_d):
    S = c.S
    c.ident = c.sb("ident", [128, 128], BF16)
    c.B_ident = Buf()
    c.ones = c.sb("ones", [128, 128], BF16)
    c.B_ones = Buf()
    S.op("sync", "dma_start", out=c.ident[:], in_=ident_d, writes=[c.B_ident], dma="ident")
    S.op("sync", "dma_start", out=c.ones[:], in_=ones_d, writes=[c.B_ones], dma="ones")
    c.psT = Ring(c, "psT", [128, 8, 128], BF16, 2, psum=True)
    c.psF = Ring(c, "psF", [128, 512], F32, 6, psum=True)
    c.eps_t = c.sb("eps_t", [128, 1], F32)
    c.B_eps = Buf()
    S.op("vector", "memset", ap=c.eps_t[:], constant=EPS, writes=[c.B_eps])


def load_gain_cols(c, name, vec_d, nk):
    t = c.sb(name, [128, nk], F32)
    B = Buf(name)
    c.S.op("sync", "dma_nc", out=t[:], in_=vec_d.rearrange("(k p) -> p k", p=128), writes=[B], dma=name)
    return t, B


class NormRings:
    def __init__(self, c):
        self.junk = Ring(c, c.key("junk"), [128, D], BF16, 2)
        self.small = Ring(c, c.key("small"), [128, 4], F32, 6)
        self.xn = Ring(c, c.key("xn"), [128, D], BF16, 2)


def norm_transpose_tile(c, x_ap, B_x, hT_ap, B_hT, col0, nr, xn_dt=BF16):
    S = c.S
    junk, B_junk, _ = nr.junk.next()
    sm, B_sm, _ = nr.small.next()
    xn, B_xn, _ = nr.xn.next()
    S.op("scalar", "activation", out=junk[:], in_=x_ap, func=AF.Square, accum_out=sm[:, 0:1], reads=[B_x], writes=[B_junk, B_sm])
    S.op("scalar", "activation", out=sm[:, 1:2], in_=sm[:, 0:1], func=AF.Sqrt, scale=1.0 / D, bias=c.eps_t[:, 0:1],
         reads=[B_sm, c.B_eps], writes=[B_sm])
    S.op("vector", "reciprocal", out=sm[:, 2:3], in_=sm[:, 1:2], reads=[B_sm], writes=[B_sm])
    S.op("vector", "tensor_scalar", out=xn[:], in0=x_ap, scalar1=sm[:, 2:3], scalar2=None, op0=ALU.mult, reads=[B_x, B_sm], writes=[B_xn])
    pT, B_pT, _ = c.psT.next()
    for k in range(8):
        S.op("tensor", "transpose", out=pT[:, k, :], in_=xn[:, k * 128:(k + 1) * 128], identity=c.ident[:], reads=[B_xn, c.B_ident], writes=[B_pT])
    S.op("scalar", "copy", out=hT_ap[:, :, col0:col0 + 128], in_=pT[:], reads=[B_pT], writes=[B_hT])
    return sm, B_sm


def rope_tables(c, pos_d, inv_d):
    S = c.S
    nc = c.nc
    c.cosT = c.sb("cosT", [64, NTOK], F32)
    c.sinT = c.sb("sinT", [64, NTOK], F32)
    c.B_cos = Buf()
    c.B_sin = Buf()
    with contextlib.ExitStack() as st2:
        def sb(name, shape, dt):
            return st2.enter_context(nc.sbuf_tensor("s_" + name, shape, dt))
        pos_i = sb("pos_i", [64, NTOK], I32)
        B_pi = Buf()
        inv = sb("inv", [64, 1], F32)
        B_inv = Buf()
        ang = sb("ang", [64, NTOK], F32)
        B_ang = Buf()
        ni = sb("ni", [64, NTOK], I32)
        B_ni = Buf()
        nf = sb("nf", [64, NTOK], F32)
        B_nf = Buf()
        r1 = sb("r1", [64, NTOK], F32)
        B_r1 = Buf()
        S.op("sync", "dma_start", out=pos_i[:], in_=pos_d.partition_broadcast(64), writes=[B_pi], dma="pos")
        S.op("sync", "dma_start", out=inv[:], in_=inv_d, writes=[B_inv], dma="inv")
        S.op("vector", "tensor_copy", out=ang[:], in_=pos_i[:], reads=[B_pi], writes=[B_ang])
        S.op("vector", "tensor_scalar", out=ang[:], in0=ang[:], scalar1=inv[:, 0:1], scalar2=None, op0=ALU.mult, reads=[B_ang, B_inv], writes=[B_ang])
        for which, (dst, B_dst) in enumerate(((c.sinT, c.B_sin), (c.cosT, c.B_cos))):
            off = 0.0 if which == 0 else math.pi / 2
            S.op("vector", "tensor_scalar", out=ni[:], in0=ang[:], scalar1=off, scalar2=1.0 / TWO_PI, op0=ALU.add, op1=ALU.mult, reads=[B_ang], writes=[B_ni])
            S.op("vector", "tensor_copy", out=nf[:], in_=ni[:], reads=[B_ni], writes=[B_nf])
            S.op("vector", "scalar_tensor_tensor", out=r1[:], in0=nf[:], scalar=-C1, in1=ang[:], op0=ALU.mult, op1=ALU.add, reads=[B_nf, B_ang], writes=[B_r1])
            S.op("vector", "scalar_tensor_tensor", out=r1[:], in0=nf[:], scalar=-C2, in1=r1[:], op0=ALU.mult, op1=ALU.add, reads=[B_nf, B_r1], writes=[B_r1])
            S.op("vector", "tensor_scalar", out=r1[:], in0=r1[:], scalar1=off, scalar2=math.pi, op0=ALU.add, op1=ALU.min, reads=[B_r1], writes=[B_r1])
            S.op("vector", "tensor_scalar", out=r1[:], in0=r1[:], scalar1=-math.pi, scalar2=None, op0=ALU.max, reads=[B_r1], writes=[B_r1])
            S.op("scalar", "activation", out=dst[:], in_=r1[:], func=AF.Sin, reads=[B_r1], writes=[B_dst])
        S.barrier()


def load_phase_a_weights(c, w_in_d, mix_norm_d, w_q_b_d, q_a_norm_d, w_kv_b_d, kv_a_norm_d):
    S = c.S
    c.win = c.sb(c.key("win"), [128, 8, IN_COLS + 64], BF16)
    c.B_win = Buf()
    c.wqb = c.sb(c.key("wqb"), [128, 3, 768 + 256], BF16)
    c.B_wqb = Buf()
    c.wk = c.sb(c.key("wk"), [128, 2, 512], BF16)
    c.wv = c.sb(c.key("wv"), [128, 2, 512], BF16)
    c.B_wkv = Buf()
    gmix, B_gmix = load_gain_cols(c, c.key("gmix"), mix_norm_d, 8)
    gq, B_gq = load_gain_cols(c, c.key("gq"), q_a_norm_d, 3)
    gkv, B_gkv = load_gain_cols(c, c.key("gkv"), kv_a_norm_d, 2)
    stage = Ring(c, c.key("wstage"), [128, IN_COLS], F32, 2)
    for k in range(8):
        st_t, B_st, key = stage.next()
        S.op("sync", "dma_start", out=st_t[:], in_=w_in_d[k * 128:(k + 1) * 128, :], writes=[B_st], dma=key)
        S.op("vector" if k % 2 == 0 else "gpsimd", "tensor_scalar", out=c.win[:, k, 0:IN_COLS], in0=st_t[:], scalar1=gmix[:, k:k + 1], scalar2=None,
             op0=ALU.mult, reads=[B_st, B_gmix], writes=[c.B_win])
    S.op("vector", "tensor_scalar", out=c.win[:, :, IN_COLS:IN_COLS + 32], in0=c.win[:, :, 672:704], scalar1=-1.0, scalar2=None, op0=ALU.mult,
         reads=[c.B_win], writes=[c.B_win])
    S.op("vector", "tensor_copy", out=c.win[:, :, IN_COLS + 32:IN_COLS + 64], in_=c.win[:, :, 640:672], reads=[c.B_win], writes=[c.B_win])
    for k in range(3):
        st_t, B_st, key = stage.next()
        S.op("sync", "dma_start", out=st_t[:, 0:768], in_=w_q_b_d[k * 128:(k + 1) * 128, :], writes=[B_st], dma=key)
        S.op("vector", "tensor_scalar", out=c.wqb[:, k, 0:768], in0=st_t[:, 0:768], scalar1=gq[:, k:k + 1], scalar2=None, op0=ALU.mult,
             reads=[B_st, B_gq], writes=[c.B_wqb])
    for h in range(4):
        b0 = h * 192 + 128
        S.op("vector", "tensor_scalar", out=c.wqb[:, :, 768 + h * 64:768 + h * 64 + 32], in0=c.wqb[:, :, b0 + 32:b0 + 64], scalar1=-1.0, scalar2=None,
             op0=ALU.mult, reads=[c.B_wqb], writes=[c.B_wqb])
        S.op("vector", "tensor_copy", out=c.wqb[:, :, 768 + h * 64 + 32:768 + h * 64 + 64], in_=c.wqb[:, :, b0:b0 + 32], reads=[c.B_wqb], writes=[c.B_wqb])
    for k in range(2):
        st_t, B_st, key = stage.next()
        S.op("sync", "dma_start", out=st_t[:, 0:1024], in_=w_kv_b_d[k * 128:(k + 1) * 128, :], writes=[B_st], dma=key)
        src = st_t[:, 0:1024].rearrange("p (h t c) -> p h t c", h=4, t=2)
        S.op("vector", "tensor_scalar", out=c.wk[:, k, :].rearrange("p (h c) -> p h c", h=4), in0=src[:, :, 0, :], scalar1=gkv[:, k:k + 1], scalar2=None,
             op0=ALU.mult, reads=[B_st, B_gkv], writes=[c.B_wkv])
        S.op("vector", "tensor_scalar", out=c.wv[:, k, :].rearrange("p (h c) -> p h c", h=4), in0=src[:, :, 1, :], scalar1=gkv[:, k:k + 1], scalar2=None,
             op0=ALU.mult, reads=[B_st, B_gkv], writes=[c.B_wkv])


def phase_a(c, x_src, outs):
    S = c.S
    hT_ring = Ring(c, c.key("hT"), [128, 8, TB], BF16, 2)
    nr = NormRings(c)
    cq_ring = Ring(c, c.key("cqbf"), [128, 3, TB], BF16, 2)
    cqs_ring = Ring(c, c.key("cqsq"), [128, 3, TB], BF16, 2)
    ckv_ring = Ring(c, c.key("ckvbf"), [128, 2, TB], BF16, 2)
    ckvs_ring = Ring(c, c.key("ckvsq"), [128, 2, TB], BF16, 2)
    rbc_ring = Ring(c, c.key("rbc"), [128, TB], F32, 3)
    o16_ring = Ring(c, c.key("o16"), [128, TB], BF16, 8)
    o32_ring = Ring(c, c.key("o32"), [128, TB], F32, 3)
    t32_ring = Ring(c, c.key("t32"), [64, TB], F32, 4)
    final = []

    def store(dst_ap, src_ap, B_src, key):
        final.append(S.op("gpsimd", "dma_start", out=dst_ap, in_=src_ap, reads=[B_src], dma=key))

    def proj_fm(hT, B_hT, c0, M):
        ps, B_ps, _ = c.psF.next()
        for k in range(8):
            S.op("tensor", "matmul", out=ps[0:M, :], lhsT=c.win[:, k, c0:c0 + M], rhs=hT[:, k, :], start=(k == 0), stop=(k == 7),
                 reads=[c.B_win, B_hT], writes=[B_ps])
        return ps, B_ps

    def rms_bc(sq, B_sq, nk, dim):
        ps, B_ps, _ = c.psF.next()
        for k in range(nk):
            S.op("tensor", "matmul", out=ps[:], lhsT=c.ones[:], rhs=sq[:, k, :], start=(k == 0), stop=(k == nk - 1), reads=[c.B_ones, B_sq], writes=[B_ps])
        r, B_r, _ = rbc_ring.next()
        S.op("scalar", "activation", out=r[:], in_=ps[:], func=AF.Sqrt, scale=1.0 / dim, bias=c.eps_t[:, 0:1], reads=[B_ps, c.B_eps], writes=[B_r])
        S.op("vector", "reciprocal", out=r[:], in_=r[:], reads=[B_r], writes=[B_r])
        return r, B_r

    def rope_out(ps_a, B_a, ps_b, B_b, t0, mul_ap, B_mul, dst_ap, scale):
        t1, B_t1, _ = t32_ring.next()
        t2, B_t2, _ = t32_ring.next()
        S.op("vector", "tensor_tensor", out=t1[:], in0=ps_a[0:64, :], in1=c.cosT[:, t0:t0 + TB], op=ALU.mult, reads=[B_a, c.B_cos], writes=[B_t1])
        S.op("vector", "tensor_tensor", out=t2[:], in0=ps_b[0:64, :], in1=c.sinT[:, t0:t0 + TB], op=ALU.mult, reads=[B_b, c.B_sin], writes=[B_t2])
        o, B_o, key = o16_ring.next()
        if mul_ap is None:
            S.op("vector", "tensor_tensor", out=o[0:64, :], in0=t1[:], in1=t2[:], op=ALU.add, reads=[B_t1, B_t2], writes=[B_o])
        else:
            S.op("vector", "tensor_tensor", out=t1[:], in0=t1[:], in1=t2[:], op=ALU.add, reads=[B_t1, B_t2], writes=[B_t1])
            S.op("vector", "scalar_tensor_tensor", out=o[0:64, :], in0=t1[:], scalar=scale, in1=mul_ap[0:64, :], op0=ALU.mult, op1=ALU.mult,
                 reads=[B_t1, B_mul], writes=[B_o])
        store(dst_ap, o[0:64, :], B_o, key)

    for tb in range(NTB):
        t0 = tb * TB
        hT, B_hT, _ = hT_ring.next()
        for j in range(4):
            x_ap, B_x = x_src(tb * 4 + j)
            norm_transpose_tile(c, x_ap, B_x, hT, B_hT, j * 128, nr)
        if getattr(c, "dbg", 0) == 3:
            store(outs["QN"][0, :, t0:t0 + TB], hT[:, 0, :], B_hT, "dbg")
            return final
        cq, B_cq, _ = cq_ring.next()
        cqs, B_cqs, _ = cqs_ring.next()
        for ch in range(3):
            ps, B_ps = proj_fm(hT, B_hT, ch * 128, 128)
            S.op("scalar", "copy", out=cq[:, ch, :], in_=ps[:], reads=[B_ps], writes=[B_cq])
            S.op("vector", "tensor_tensor", out=cqs[:, ch, :], in0=cq[:, ch, :], in1=cq[:, ch, :], op=ALU.mult, reads=[B_cq], writes=[B_cqs])
        if getattr(c, "dbg", 0) in (41, 411, 412):
            return final
        rq, B_rq = rms_bc(cqs, B_cqs, 3, 384.0)
        if getattr(c, "dbg", 0) == 42:
            return final
        for h in range(4):
            ps, B_ps, _ = c.psF.next()
            for k in range(3):
                S.op("tensor", "matmul", out=ps[:], lhsT=c.wqb[:, k, h * 192:h * 192 + 128], rhs=cq[:, k, :], start=(k == 0), stop=(k == 2),
                     reads=[c.B_wqb, B_cq], writes=[B_ps])
            o, B_o, key = o16_ring.next()
            S.op("vector", "scalar_tensor_tensor", out=o[:], in0=ps[:], scalar=SCALE, in1=rq[:], op0=ALU.mult, op1=ALU.mult, reads=[B_ps, B_rq], writes=[B_o])
            store(outs["QN"][h, :, t0:t0 + TB], o[:], B_o, key)
            if getattr(c, "dbg", 0) == 43:
                return final
            psa, B_psa, _ = c.psF.next()
            psb, B_psb, _ = c.psF.next()
            for k in range(3):
                S.op("tensor", "matmul", out=psa[0:64, :], lhsT=c.wqb[:, k, h * 192 + 128:h * 192 + 192], rhs=cq[:, k, :], start=(k == 0), stop=(k == 2),
                     reads=[c.B_wqb, B_cq], writes=[B_psa])
            for k in range(3):
                S.op("tensor", "matmul", out=psb[0:64, :], lhsT=c.wqb[:, k, 768 + h * 64:768 + h * 64 + 64], rhs=cq[:, k, :], start=(k == 0), stop=(k == 2),
                     reads=[c.B_wqb, B_cq], writes=[B_psb])
            if getattr(c, "dbg", 0) == 44:
                return final
            rope_out(psa, B_psa, psb, B_psb, t0, rq, B_rq, outs["QP"][h, :, t0:t0 + TB], SCALE)
        if getattr(c, "dbg", 0) == 4:
            return final
        ckv, B_ckv, _ = ckv_ring.next()
        ckvs, B_ckvs, _ = ckvs_ring.next()
        for ch in range(2):
            ps, B_ps = proj_fm(hT, B_hT, 384 + ch * 128, 128)
            S.op("scalar", "copy", out=ckv[:, ch, :], in_=ps[:], reads=[B_ps], writes=[B_ckv])
            S.op("vector", "tensor_tensor", out=ckvs[:, ch, :], in0=ckv[:, ch, :], in1=ckv[:, ch, :], op=ALU.mult, reads=[B_ckv], writes=[B_ckvs])
        rkv, B_rkv = rms_bc(ckvs, B_ckvs, 2, 256.0)
        for h in range(4):
            ps, B_ps, _ = c.psF.next()
            for k in range(2):
                S.op("tensor", "matmul", out=ps[:], lhsT=c.wk[:, k, h * 128:(h + 1) * 128], rhs=ckv[:, k, :], start=(k == 0), stop=(k == 1),
                     reads=[c.B_wkv, B_ckv], writes=[B_ps])
            o, B_o, key = o16_ring.next()
            S.op("vector", "tensor_tensor", out=o[:], in0=ps[:], in1=rkv[:], op=ALU.mult, reads=[B_ps, B_rkv], writes=[B_o])
            store(outs["KN"][h, :, t0:t0 + TB], o[:], B_o, key)
        for h in range(4):
            ps, B_ps, _ = c.psF.next()
            for k in range(2):
                S.op("tensor", "matmul", out=ps[:], lhsT=c.wv[:, k, h * 128:(h + 1) * 128], rhs=ckv[:, k, :], start=(k == 0), stop=(k == 1),
                     reads=[c.B_wkv, B_ckv], writes=[B_ps])
            o, B_o, key = o16_ring.next()
            S.op("vector", "tensor_tensor", out=o[:], in0=ps[:], in1=rkv[:], op=ALU.mult, reads=[B_ps, B_rkv], writes=[B_o])
            store(outs["VT"][h, :, t0:t0 + TB], o[:], B_o, key)
        if getattr(c, "dbg", 0) == 5:
            return final
        psa, B_psa = proj_fm(hT, B_hT, 640, 64)
        psb, B_psb = proj_fm(hT, B_hT, IN_COLS, 64)
        rope_out(psa, B_psa, psb, B_psb, t0, None, None, outs["KPE"][:, t0:t0 + TB], 1.0)
        if getattr(c, "dbg", 0) == 6:
            return final
        for h in range(4):
            ps, B_ps = proj_fm(hT, B_hT, 704 + h * 128, 128)
            o, B_o, key = o16_ring.next()
            S.op("scalar", "activation", out=o[:], in_=ps[:], func=AF.Silu, reads=[B_ps], writes=[B_o])
            store(outs["HQ"][h, :, t0:t0 + TB], o[:], B_o, key)
        for h in range(4):
            ps, B_ps = proj_fm(hT, B_hT, 1216 + h * 128, 128)
            o, B_o, key = o32_ring.next()
            S.op("vector", "tensor_copy", out=o[:], in_=ps[:], reads=[B_ps], writes=[B_o])
            store(outs["ZF"][h, :, t0:t0 + TB], o[:], B_o, key)
        if getattr(c, "dbg", 0) == 7:
            return final
        for h in range(4):
            ps, B_ps = proj_fm(hT, B_hT, 1728 + h * 128, 128)
            o, B_o, key = o16_ring.next()
            S.op("vector", "tensor_copy", out=o[:], in_=ps[:], reads=[B_ps], writes=[B_o])
            store(outs["HIT"][h, :, t0:t0 + TB], o[:], B_o, key)
        for h in range(4):
            ps, B_ps = proj_fm(hT, B_hT, 2240 + h * 128, 128)
            o, B_o, key = o16_ring.next()
            S.op("scalar", "activation", out=o[:], in_=ps[:], func=AF.Silu, reads=[B_ps], writes=[B_o])
            store(outs["HGT"][h, :, t0:t0 + TB], o[:], B_o, key)
    return final


PA16_ROWS = 3392
PA16_BASE = {"QN": 0, "KN": 512, "VT": 1024, "HQ": 1536, "HIT": 2048, "HGT": 2560, "QP": 3072, "KPE": 3328}


def pa_views(pa16, pa32):
    o = {}
    for n in ("QN", "KN", "VT", "HQ", "HIT", "HGT"):
        b0 = PA16_BASE[n]
        o[n] = pa16[b0:b0 + 512, :].rearrange("(h p) t -> h p t", p=128)
    o["QP"] = pa16[3072:3328, :].rearrange("(h p) t -> h p t", p=64)
    o["KPE"] = pa16[3328:3392, :]
    o["ZF"] = pa32.rearrange("(h p) t -> h p t", p=128)
    return o


def const_inputs():
    inv = (np.float32(10000.0) ** (-(np.arange(0, 64, 2, dtype=np.float32)) / np.float32(64))).astype(np.float32)
    return {
        "ident": np.eye(128, dtype=np.float32).astype(NPBF),
        "ones": np.ones((128, 128), dtype=np.float32).astype(NPBF),
        "inv": np.concatenate([inv, inv]).reshape(64, 1).astype(np.float32),
    }


def dram_in(nc, name, shape, dt):
    return nc.dram_tensor(name, shape, dt, kind="ExternalInput").ap()


def build_l1(stop=0):
    nc = bass.Bass("TRN2", target_bir_lowering=False)
    x_d = dram_in(nc, "x", [NTOK, D], F32)
    pos_d = dram_in(nc, "pos", [1, NTOK], I32)
    ident_d = dram_in(nc, "ident", [128, 128], BF16)
    ones_d = dram_in(nc, "ones", [128, 128], BF16)
    inv_d = dram_in(nc, "inv", [64, 1], F32)
    w_in_d = dram_in(nc, "w_in", [D, IN_COLS], F32)
    mixn_d = dram_in(nc, "mix_norm", [D], F32)
    wqb_d = dram_in(nc, "w_q_b", [384, 768], F32)
    qan_d = dram_in(nc, "q_a_norm", [384], F32)
    wkvb_d = dram_in(nc, "w_kv_b", [256, 1024], F32)
    kvan_d = dram_in(nc, "kv_a_norm", [256], F32)
    pa16 = nc.dram_tensor("PA16", [PA16_ROWS, NTOK], BF16, kind="ExternalOutput").ap()
    pa32 = nc.dram_tensor("PA32", [512, NTOK], F32, kind="ExternalOutput").ap()
    outs = pa_views(pa16, pa32)
    S = Sched(nc)
    c = new_ctx(nc, S)
    with c.st:
        load_consts(c, ident_d, ones_d)
        rope_tables(c, pos_d, inv_d)
        if stop == 1:
            f = S.op("gpsimd", "dma_start", out=outs["ZF"][0, 0:64, :], in_=c.cosT[:], reads=[c.B_cos], dma="dbg")
            S.emit(final_waits=[f])
            return nc
        load_phase_a_weights(c, w_in_d, mixn_d, wqb_d, qan_d, wkvb_d, kvan_d)
        if stop == 2:
            f = S.op("gpsimd", "dma_start", out=outs["QN"][0, :, :], in_=c.win[:, 0, 0:2048], reads=[c.B_win], dma="dbg")
            S.emit(final_waits=[f])
            return nc
        x_ring = Ring(c, "xin", [128, D], F32, 3)
        c.dbg = stop

        def x_src(t):
            xt, B_xt, key = x_ring.next()
            S.op("sync", "dma_start", out=xt[:], in_=x_d[t * 128:(t + 1) * 128, :], writes=[B_xt], dma=key)
            return xt[:], B_xt
        final = phase_a(c, x_src, outs)
        S.emit(final_waits=final)
    return nc


NQT = SEQ // 128
MASK_NEG = -30000.0


def mixer_consts():
    cm = np.where(np.arange(128)[None, :] <= np.arange(128)[:, None], 0.0, MASK_NEG).astype(np.float32)
    hm = (np.arange(64)[:, None] <= np.arange(64)[None, :]).astype(np.float32)
    rs = np.ones((128, 512), np.float32)
    rs[:, ::64] = 0.0
    return {"cmask": cm.astype(NPBF), "hmask": hm, "resetm": rs}


IDX_QN, IDX_KN, IDX_VT, IDX_QP, IDX_KPE, IDX_HQ, IDX_HIT, IDX_HGT, IDX_ZF, IDX_MIX, IDX_N = 0, 4, 8, 12, 16, 20, 36, 52, 68, 84, 92


def attention(c, d, AT_d, nqt=NQT, pfx="a"):
    S = c.S
    nc = c.nc
    final = []
    with contextlib.ExitStack() as st2:
        c2 = Ctx(nc, S, st2)
        qn = c2.sb(pfx + "_qn", [128, SEQ], BF16)
        qp = c2.sb(pfx + "_qp", [64, SEQ], BF16)
        kn = c2.sb(pfx + "_kn", [128, SEQ], BF16)
        kpe = c2.sb(pfx + "_kpe", [64, SEQ], BF16)
        v = c2.sb(pfx + "_v", [128, NQT, 128], BF16)
        vt = c2.sb(pfx + "_vt", [128, SEQ], BF16)
        cmask = c2.sb(pfx + "_cmask", [128, 128], BF16)
        B_in = [Buf() for _ in range(4)]
        B_vt = [Buf() for _ in range(4)]
        B_cm = Buf()
        S.op("sync", "dma_start", out=cmask[:], in_=d["cmask"], writes=[B_cm], dma=c.key("a_cm"))
        CH = 2048
        pv = d["pa16v"]
        for i in range(SEQ // CH):
            sl = slice(i * CH, (i + 1) * CH)
            S.gather_rows(kn[:, sl], pv, c.idx[:, IDX_KN + i:IDX_KN + i + 1], reads=[c.B_idx], writes=[B_in[i]], dma=c.key("a_in"))
            S.gather_rows(kpe[:, sl], pv, c.idx[0:64, IDX_KPE + i:IDX_KPE + i + 1], reads=[c.B_idx], writes=[B_in[i]], dma=c.key("a_in"))
            S.gather_rows(qn[:, sl], pv, c.idx[:, IDX_QN + i:IDX_QN + i + 1], reads=[c.B_idx], writes=[B_in[i]], dma=c.key("a_in"))
            S.gather_rows(qp[:, sl], pv, c.idx[0:64, IDX_QP + i:IDX_QP + i + 1], reads=[c.B_idx], writes=[B_in[i]], dma=c.key("a_in"))
            S.gather_rows(vt[:, sl], pv, c.idx[:, IDX_VT + i:IDX_VT + i + 1], reads=[c.B_idx], writes=[B_vt[i]], dma=c.key("a_in"))
        ps_s = Ring.of("pss", c.psF.tiles[0:3])
        ps_o = Ring.of("pso", c.psF.tiles[3:5])
        ps_t = Ring.of("pst", c.psT.tiles[0:2])
        p_ring = Ring(c2, pfx + "_p", [128, 512], BF16, 3)
        pT_ring = Ring(c2, pfx + "_pT", [128, 512], BF16, 3)
        st_ring = Ring(c2, pfx + "_stat", [128, 40], F32, 3)
        o_ring = Ring(c2, pfx + "_o", [128, 128], BF16, 2)
        out_ring = Ring(c2, pfx + "_out", [128, 512], BF16, 2)
        for g in range(NQT // 8):
            pt, B_pt, _ = ps_t.next()
            for u in range(8):
                t = g * 8 + u
                S.op("tensor", "transpose", out=pt[:, u, :], in_=vt[:, t * 128:(t + 1) * 128], identity=c.ident[:],
                     reads=[B_vt[(t * 128) // CH], c.B_ident], writes=[B_pt])
            S.op("vector", "tensor_copy", out=v[:, g * 8:(g + 1) * 8, :], in_=pt[:], reads=[B_pt], writes=[B_in[(g * 8 * 128) // CH]])

        def scores(i, kb, w, diag):
            ps, B_ps, _ = ps_s.next()
            q0 = i * 128
            k0 = kb * 512
            rd = list({id(b): b for b in (B_in[q0 // CH], B_in[k0 // CH], B_in[(k0 + w - 1) // CH])}.values())
            S.op("tensor", "matmul", out=ps[:, 0:w], lhsT=qn[:, q0:q0 + 128], rhs=kn[:, k0:k0 + w], start=True, stop=False, reads=rd, writes=[B_ps])
            S.op("tensor", "matmul", out=ps[:, 0:w], lhsT=qp[:, q0:q0 + 128], rhs=kpe[:, k0:k0 + w], start=False, stop=(not diag), reads=rd, writes=[B_ps])
            if diag:
                S.op("tensor", "matmul", out=ps[:, w - 128:w], lhsT=c.ident[:], rhs=cmask[:], start=False, stop=True,
                     reads=[c.B_ident, B_cm], writes=[B_ps])
            return ps, B_ps

        ps_s4 = Ring.of("pss4", c.psF.tiles[0:4])
        ps_o2 = Ring.of("pso2", c.psF.tiles[4:6])
        st_ring4 = Ring(c2, pfx + "_stat4", [128, 40], F32, 4)
        tiles = {}

        def nblocks(i):
            return (i + 1 + 3) // 4

        def width(i, kb):
            return min(4, i + 1 - 4 * kb) * 128

        def scores2(i, kb):
            w = width(i, kb)
            diag = (kb == nblocks(i) - 1)
            ps, B_ps, _ = ps_s4.next()
            q0 = i * 128
            k0 = kb * 512
            rd = list({id(b): b for b in (B_in[q0 // CH], B_in[k0 // CH], B_in[(k0 + w - 1) // CH])}.values())
            S.op("tensor", "matmul", out=ps[:, 0:w], lhsT=qn[:, q0:q0 + 128], rhs=kn[:, k0:k0 + w], start=True, stop=False, reads=rd, writes=[B_ps])
            S.op("tensor", "matmul", out=ps[:, 0:w], lhsT=qp[:, q0:q0 + 128], rhs=kpe[:, k0:k0 + w], start=False, stop=(not diag), reads=rd, writes=[B_ps])
            if diag:
                S.op("tensor", "matmul", out=ps[:, w - 128:w], lhsT=c.ident[:], rhs=cmask[:], start=False, stop=True,
                     reads=[c.B_ident, B_cm], writes=[B_ps])
            return ps, B_ps, w

        def tile_begin(i):
            stt, B_st, _ = st_ring4.next()
            S.op("vector", "memset", ap=stt[:, 16:32], constant=0.0, writes=[B_st])
            tiles[i] = {"stt": stt, "B_st": B_st, "p": {}, "pT": {}}

        def P1(i, kb):
            t = tiles[i]
            ps, B_ps, w = scores2(i, kb)
            S.op("vector", "reduce_max", out=t["stt"][:, kb:kb + 1], in_=ps[:, 0:w], axis=AX.X, reads=[B_ps], writes=[t["B_st"]])

        def P1_fin(i):
            t = tiles[i]
            stt, B_st = t["stt"], t["B_st"]
            S.op("vector", "reduce_max", out=stt[:, 32:33], in_=stt[:, 0:nblocks(i)], axis=AX.X, reads=[B_st], writes=[B_st])
            S.op("vector", "tensor_scalar", out=stt[:, 33:34], in0=stt[:, 32:33], scalar1=-1.0, scalar2=None, op0=ALU.mult, reads=[B_st], writes=[B_st])

        def S2(i, kb):
            t = tiles[i]
            ps, B_ps, w = scores2(i, kb)
            p, B_p, _ = p_ring.next()
            S.op("scalar", "activation", out=p[:, 0:w], in_=ps[:, 0:w], func=AF.Exp, bias=t["stt"][:, 33:34], accum_out=t["stt"][:, 16 + kb:17 + kb],
                 reads=[B_ps, t["B_st"]], writes=[B_p, t["B_st"]])
            t["p"][kb] = (p, B_p)

        def T(i, kb):
            t = tiles[i]
            p, B_p = t["p"].pop(kb)
            nsub = width(i, kb) // 128
            pt, B_pt, _ = ps_t.next()
            for j in range(nsub):
                S.op("tensor", "transpose", out=pt[:, j, :], in_=p[:, j * 128:(j + 1) * 128], identity=c.ident[:], reads=[B_p, c.B_ident], writes=[B_pt])
            pT, B_pT, _ = pT_ring.next()
            S.op("vector", "tensor_copy", out=pT[:, 0:nsub * 128].rearrange("p (j k) -> p j k", k=128), in_=pt[:, 0:nsub, :], reads=[B_pt], writes=[B_pT])
            t["pT"][kb] = (pT, B_pT)

        def V(i, kb):
            t = tiles[i]
            if kb == 0:
                t["po"] = ps_o2.next()
            po, B_po, _ = t["po"]
            pT, B_pT = t["pT"].pop(kb)
            nsub = width(i, kb) // 128
            nb = nblocks(i)
            for j in range(nsub):
                kt = kb * 4 + j
                S.op("tensor", "matmul", out=po[:, 0:128], lhsT=pT[:, j * 128:(j + 1) * 128], rhs=v[:, kt, :],
                     start=(kb == 0 and j == 0), stop=(kb == nb - 1 and j == nsub - 1), reads=[B_pT, B_in[(kt * 128) // CH]], writes=[B_po])

        def fin1(i):
            t = tiles[i]
            stt, B_st = t["stt"], t["B_st"]
            po, B_po, _ = t["po"]
            S.op("vector", "reduce_sum", out=stt[:, 34:35], in_=stt[:, 16:16 + nblocks(i)], axis=AX.X, reads=[B_st], writes=[B_st])
            S.op("vector", "reciprocal", out=stt[:, 35:36], in_=stt[:, 34:35], reads=[B_st], writes=[B_st])
            o, B_o, _ = o_ring.next()
            S.op("scalar", "activation", out=o[:], in_=po[:, 0:128], func=AF.Copy, scale=stt[:, 35:36], reads=[B_po, B_st], writes=[B_o])
            t["o"] = (o, B_o)

        outs_state = {"outt": None}

        def fin2(i):
            t = tiles.pop(i)
            o, B_o = t["o"]
            pt, B_pt, _ = ps_t.next()
            S.op("tensor", "transpose", out=pt[:, 0, :], in_=o[:], identity=c.ident[:], reads=[B_o, c.B_ident], writes=[B_pt])
            if i % 4 == 0:
                outs_state["outt"] = out_ring.next()
            outt = outs_state["outt"]
            S.op("vector", "tensor_copy", out=outt[0][:, (i % 4) * 128:(i % 4 + 1) * 128], in_=pt[:, 0, :], reads=[B_pt], writes=[outt[1]])
            if i % 4 == 3 or i == nqt - 1:
                i0 = (i // 4) * 4
                wd = (i - i0 + 1) * 128
                final.append(S.op("sync", "dma_start", out=AT_d[:, i0 * 128:i0 * 128 + wd], in_=outt[0][:, 0:wd], reads=[outt[1]], dma=outt[2]))

        tile_begin(0)
        for kb in range(nblocks(0)):
            P1(0, kb)
        P1_fin(0)
        for i in range(nqt):
            nb_i = nblocks(i)
            nb_n = nblocks(i + 1) if i + 1 < nqt else 0
            if i + 1 < nqt:
                tile_begin(i + 1)
            for s in range(max(nb_i + 2, nb_n)):
                if s < nb_n:
                    P1(i + 1, s)
                if s < nb_i:
                    S2(i, s)
                if 1 <= s <= nb_i:
                    T(i, s - 1)
                if 2 <= s <= nb_i + 1:
                    V(i, s - 2)
                if s == 1 and i > 0:
                    fin2(i - 1)
            if i + 1 < nqt:
                P1_fin(i + 1)
            fin1(i)
        fin2(nqt - 1)
        S.barrier()
    return final


def hgrn(c, d, RT_d, layer, nsb=SEQ // 512, pfx="h"):
    S = c.S
    nc = c.nc
    final = []
    with contextlib.ExitStack() as st2:
        c2 = Ctx(nc, S, st2)
        hmask = c2.sb(pfx + "_hmask", [64, 64], F32)
        resetm = c2.sb(pfx + "_resetm", [128, 512], F32)
        lbraw = c2.sb(pfx + "_lbraw", [128, 2], F32)
        gn = c2.sb(pfx + "_gn", [128, 1], F32)
        cst = c2.sb(pfx + "_cst", [128, 4], F32)
        B_c = Buf()
        kc = c.key("h_c")
        S.op("sync", "dma_start", out=hmask[:], in_=d["hmask"], writes=[B_c], dma=kc)
        S.op("sync", "dma_start", out=resetm[:], in_=d["resetm"], writes=[B_c], dma=kc)
        S.op("sync", "dma_start", out=lbraw[:], in_=d["lbraw"], writes=[B_c], dma=kc)
        S.op("sync", "dma_start", out=gn[:], in_=d["gn"], writes=[B_c], dma=kc)
        if layer == 0:
            S.op("vector", "memset", ap=cst[:, 0:1], constant=0.0, writes=[B_c])
        else:
            S.op("vector", "tensor_tensor", out=cst[:, 2:3], in0=lbraw[:, 1:2], in1=lbraw[:, 0:1], op=ALU.subtract, reads=[B_c], writes=[B_c])
            S.op("scalar", "activation", out=cst[:, 0:1], in_=cst[:, 2:3], func=AF.Sigmoid, reads=[B_c], writes=[B_c])
        S.op("vector", "tensor_scalar", out=cst[:, 1:2], in0=cst[:, 0:1], scalar1=-1.0, scalar2=1.0, op0=ALU.mult, op1=ALU.add, reads=[B_c], writes=[B_c])
        state = c2.sb(pfx + "_state", [128, 128], F32)
        state_bf = c2.sb(pfx + "_state_bf", [128, 128], BF16)
        B_state = Buf()
        B_sbf = Buf()
        S.op("vector", "memset", ap=state[:], constant=0.0, writes=[B_state])
        S.op("vector", "memset", ap=state_bf[:], constant=0.0, writes=[B_sbf])
        ps_at = Ring.of("psat", c.psF.tiles[0:2])
        ps_st = Ring.of("psst", c.psF.tiles[2:4])
        ps_o = Ring.of("pso", c.psF.tiles[4:6])
        ps_kd = Ring.of("pskd", c.psT.tiles[0:1])
        ps_rt = Ring.of("psrt", c.psT.tiles[1:2])
        in_hq = Ring(c2, pfx + "_hq", [128, 512], BF16, 2)
        in_zf = Ring(c2, pfx + "_zf", [128, 512], F32, 2)
        in_hiT = Ring(c2, pfx + "_hiT", [128, 512], BF16, 2)
        in_sgT = Ring(c2, pfx + "_sgT", [128, 512], BF16, 2)
        in_hi = Ring(c2, pfx + "_hi", [64, 8, 128], BF16, 2)
        in_sg = Ring(c2, pfx + "_sg", [64, 8, 128], BF16, 2)
        f32r = Ring(c2, pfx + "_f32", [128, 512], F32, 8)
        b_ring = Ring(c2, pfx + "_b", [128, 512], F32, 2)
        k_ring = Ring(c2, pfx + "_k", [128, 512], F32, 2)
        qe_ring = Ring(c2, pfx + "_qe", [128, 512], BF16, 2)
        qh_ring = Ring(c2, pfx + "_qh", [128, 512], BF16, 2)
        kh_ring = Ring(c2, pfx + "_kh", [128, 512], BF16, 2)
        kdT_ring = Ring(c2, pfx + "_kdT", [128, 512], BF16, 2)
        kd_ring = Ring(c2, pfx + "_kd", [64, 8, 128], BF16, 2)
        dec_ring = Ring(c2, pfx + "_dec", [128, 8], F32, 2)
        at_ring = Ring(c2, pfx + "_at", [64, 64], BF16, 3)
        sm_ring = Ring(c2, pfx + "_sm", [64, 4], F32, 4)
        junk_ring = Ring(c2, pfx + "_junk", [64, 128], BF16, 2)
        on_ring = Ring(c2, pfx + "_on", [64, 128], F32, 3)
        r_ring = Ring(c2, pfx + "_r", [64, 128], BF16, 3)
        rT_ring = Ring(c2, pfx + "_rT", [128, 512], BF16, 2)

        def issue_loads(sb):
            hq, B_hq, k1 = in_hq.next()
            zf, B_zf, k2 = in_zf.next()
            hiT, B_hiT, k3 = in_hiT.next()
            sgT, B_sgT, k4 = in_sgT.next()
            S.gather_rows(hq[:], d["pa16v4"], c.idx[:, IDX_HQ + sb:IDX_HQ + sb + 1], reads=[c.B_idx], writes=[B_hq], dma=k1)
            S.gather_rows(zf[:], d["pa32v4"], c.idx[:, IDX_ZF + sb:IDX_ZF + sb + 1], reads=[c.B_idx], writes=[B_zf], dma=k2)
            S.gather_rows(hiT[:], d["pa16v4"], c.idx[:, IDX_HIT + sb:IDX_HIT + sb + 1], reads=[c.B_idx], writes=[B_hiT], dma=k3)
            S.gather_rows(sgT[:], d["pa16v4"], c.idx[:, IDX_HGT + sb:IDX_HGT + sb + 1], reads=[c.B_idx], writes=[B_sgT], dma=k4)
            return hq, B_hq, zf, B_zf, hiT, B_hiT, sgT, B_sgT

        nxt = issue_loads(0)
        for sb in range(nsb):
            t0 = sb * 512
            hq, B_hq, zf, B_zf, hiT, B_hiT, sgT, B_sgT = nxt
            if sb + 1 < nsb:
                nxt = issue_loads(sb + 1)
            hi, B_hi, _ = in_hi.next()
            sg, B_sg, _ = in_sg.next()
            for (srcT, B_srcT, dst, B_dst) in ((hiT, B_hiT, hi, B_hi), (sgT, B_sgT, sg, B_sg)):
                ptr, B_ptr, _ = ps_kd.next()
                for cc in range(8):
                    S.op("tensor", "transpose", out=ptr[0:64, cc, :], in_=srcT[:, cc * 64:(cc + 1) * 64], identity=c.ident[:],
                         reads=[B_srcT, c.B_ident], writes=[B_ptr])
                S.op("scalar", "copy", out=dst[:], in_=ptr[0:64, :, :], reads=[B_ptr], writes=[B_dst])
            ez, B_ez, _ = f32r.next()
            S.op("scalar", "activation", out=ez[:], in_=zf[:], func=AF.Exp, scale=-1.0, reads=[B_zf], writes=[B_ez])
            S.op("vector", "tensor_scalar", out=ez[:], in0=ez[:], scalar1=1.0, scalar2=None, op0=ALU.add, reads=[B_ez], writes=[B_ez])
            S.op("vector", "reciprocal", out=ez[:], in_=ez[:], reads=[B_ez], writes=[B_ez])
            f, B_f, _ = f32r.next()
            S.op("vector", "tensor_scalar", out=f[:], in0=ez[:], scalar1=cst[:, 1:2], scalar2=cst[:, 0:1], op0=ALU.mult, op1=ALU.add,
                 reads=[B_ez, B_c], writes=[B_f])
            kk, B_kk, _ = k_ring.next()
            S.op("gpsimd", "tensor_scalar", out=kk[:], in0=f[:], scalar1=-1.0, scalar2=1.0, op0=ALU.mult, op1=ALU.add, reads=[B_f], writes=[B_kk])
            lf, B_lf, _ = f32r.next()
            S.op("vector", "tensor_scalar", out=lf[:], in0=f[:], scalar1=1e-30, scalar2=None, op0=ALU.max, reads=[B_f], writes=[B_lf])
            S.op("scalar", "activation", out=lf[:], in_=lf[:], func=AF.Ln, reads=[B_lf], writes=[B_lf])
            b, B_b, _ = b_ring.next()
            S.op("vector", "tensor_tensor_scan", out=b[:], data0=resetm[:], data1=lf[:], initial=0.0, op0=ALU.mult, op1=ALU.add,
                 reads=[B_c, B_lf], writes=[B_b])
            b3 = b[:].rearrange("p (c t) -> p c t", t=64)
            bmid = b3[:, :, 31:32].to_broadcast([128, 8, 64])
            blast = b3[:, :, 63:64].to_broadcast([128, 8, 64])
            e0, B_e0, _ = f32r.next()
            S.op("scalar", "activation", out=e0[:], in_=b[:], func=AF.Exp, reads=[B_b], writes=[B_e0])
            qe, B_qe, _ = qe_ring.next()
            S.op("gpsimd", "tensor_tensor", out=qe[:], in0=hq[:], in1=e0[:], op=ALU.mult, reads=[B_hq, B_e0], writes=[B_qe])
            d1, B_d1, _ = f32r.next()
            S.op("vector", "tensor_tensor", out=d1[:].rearrange("p (c t) -> p c t", t=64), in0=b3, in1=bmid, op=ALU.subtract, reads=[B_b], writes=[B_d1])
            e1, B_e1, _ = f32r.next()
            S.op("scalar", "activation", out=e1[:], in_=d1[:], func=AF.Exp, reads=[B_d1], writes=[B_e1])
            qh, B_qh, _ = qh_ring.next()
            S.op("vector", "tensor_tensor", out=qh[:], in0=hq[:], in1=e1[:], op=ALU.mult, reads=[B_hq, B_e1], writes=[B_qh])
            e2, B_e2, _ = f32r.next()
            S.op("scalar", "activation", out=e2[:], in_=d1[:], func=AF.Exp, scale=-1.0, reads=[B_d1], writes=[B_e2])
            kh, B_kh, _ = kh_ring.next()
            S.op("gpsimd", "tensor_tensor", out=kh[:], in0=kk[:], in1=e2[:], op=ALU.mult, reads=[B_kk, B_e2], writes=[B_kh])
            d3, B_d3, _ = f32r.next()
            S.op("vector", "tensor_tensor", out=d3[:].rearrange("p (c t) -> p c t", t=64), in0=blast, in1=b3, op=ALU.subtract, reads=[B_b], writes=[B_d3])
            S.op("scalar", "activation", out=d3[:], in_=d3[:], func=AF.Exp, reads=[B_d3], writes=[B_d3])
            kdT, B_kdT, _ = kdT_ring.next()
            S.op("vector", "tensor_tensor", out=kdT[:], in0=kk[:], in1=d3[:], op=ALU.mult, reads=[B_kk, B_d3], writes=[B_kdT])
            dec, B_dec, _ = dec_ring.next()
            S.op("scalar", "activation", out=dec[:].rearrange("p (c o) -> p c o", o=1), in_=b3[:, :, 63:64], func=AF.Exp, reads=[B_b], writes=[B_dec])
            pkd, B_pkd, _ = ps_kd.next()
            for cc in range(8):
                S.op("tensor", "transpose", out=pkd[0:64, cc, :], in_=kdT[:, cc * 64:(cc + 1) * 64], identity=c.ident[:], reads=[B_kdT, c.B_ident], writes=[B_pkd])
            kd, B_kd, _ = kd_ring.next()
            S.op("scalar", "copy", out=kd[:], in_=pkd[0:64, :, :], reads=[B_pkd], writes=[B_kd])
            prt, B_prt, _ = ps_rt.next()
            for cc in range(8):
                cs = slice(cc * 64, (cc + 1) * 64)
                pat, B_pat, _ = ps_at.next()
                S.op("tensor", "matmul", out=pat[0:64, 0:64], lhsT=kh[:, cs], rhs=qh[:, cs], start=True, stop=True, reads=[B_kh, B_qh], writes=[B_pat])
                at, B_at, _ = at_ring.next()
                S.op("vector", "tensor_tensor", out=at[:], in0=pat[0:64, 0:64], in1=hmask[:], op=ALU.mult, reads=[B_pat, B_c], writes=[B_at])
                po, B_po, _ = ps_o.next()
                S.op("tensor", "matmul", out=po[0:64, 0:128], lhsT=at[:], rhs=hi[:, cc, :], start=True, stop=False, reads=[B_at, B_hi], writes=[B_po])
                S.op("tensor", "matmul", out=po[0:64, 0:128], lhsT=qe[:, cs], rhs=state_bf[:], start=False, stop=True, reads=[B_qe, B_sbf], writes=[B_po])
                pst, B_pst, _ = ps_st.next()
                S.op("tensor", "matmul", out=pst[:, 0:128], lhsT=kd[:, cc, :], rhs=hi[:, cc, :], start=True, stop=True, reads=[B_kd, B_hi], writes=[B_pst])
                S.op("vector", "scalar_tensor_tensor", out=state[:], in0=state[:], scalar=dec[:, cc:cc + 1], in1=pst[:, 0:128], op0=ALU.mult, op1=ALU.add,
                     reads=[B_state, B_dec, B_pst], writes=[B_state])
                S.op("scalar", "copy", out=state_bf[:], in_=state[:], reads=[B_state], writes=[B_sbf])
                sm, B_sm, _ = sm_ring.next()
                jk, B_jk, _ = junk_ring.next()
                S.op("scalar", "activation", out=jk[:], in_=po[0:64, 0:128], func=AF.Square, accum_out=sm[:, 0:1], reads=[B_po], writes=[B_jk, B_sm])
                S.op("scalar", "activation", out=sm[:, 1:2], in_=sm[:, 0:1], func=AF.Sqrt, scale=1.0 / 128.0, bias=c.eps_t[0:64, 0:1],
                     reads=[B_sm, c.B_eps], writes=[B_sm])
                S.op("vector", "reciprocal", out=sm[:, 2:3], in_=sm[:, 1:2], reads=[B_sm], writes=[B_sm])
                on, B_on, _ = on_ring.next()
                S.op("scalar", "activation", out=on[:], in_=po[0:64, 0:128], func=AF.Copy, scale=sm[:, 2:3], reads=[B_po, B_sm], writes=[B_on])
                r, B_r, _ = r_ring.next()
                S.op("gpsimd", "tensor_tensor", out=r[:], in0=on[:], in1=sg[:, cc, :], op=ALU.mult, reads=[B_on, B_sg], writes=[B_r])
                S.op("tensor", "transpose", out=prt[:, cc, 0:64], in_=r[:], identity=c.ident[0:64, 0:64], reads=[B_r, c.B_ident], writes=[B_prt])
            rT, B_rT, key = rT_ring.next()
            S.op("scalar", "activation", out=rT[:].rearrange("p (c t) -> p c t", t=64), in_=prt[:, :, 0:64], func=AF.Copy, scale=gn[:, 0:1],
                 reads=[B_prt, B_c], writes=[B_rT])
            final.append(S.op("sync", "dma_start", out=RT_d[:, t0:t0 + 512], in_=rT[:], reads=[B_rT], dma=key))
        S.barrier()
    return final


NT = NTOK // 128


def load_x_resident(c, x_d, name="xres"):
    c.x = c.sb(name, [128, NT, D], F32)
    c.B_x = [Buf("x%d" % t) for t in range(NT)]
    for t in range(NT):
        c.S.op("sync", "dma_start", out=c.x[:, t, :], in_=x_d[t * 128:(t + 1) * 128, :], writes=[c.B_x[t]], dma="%s_%d" % (name, t))


def wout_step(c, mixv, w_out_d, fT, B_fT):
    S = c.S
    with contextlib.ExitStack() as st2:
        c2 = Ctx(c.nc, S, st2)
        wout = c2.sb(c.key("wout"), [128, 8, D], BF16)
        B_wout = Buf()
        kwo = c.key("woutl")
        for k in range(8):
            S.gather_rows(fT[:, k, :], mixv, c.idx[:, IDX_MIX + k:IDX_MIX + k + 1], reads=[c.B_idx], writes=[B_fT], dma=c.key("mixT"))
        for k in range(8):
            S.op("gpsimd", "dma_start", out=wout[:, k, :], in_=w_out_d[k * 128:(k + 1) * 128, :], writes=[B_wout], dma=kwo)
        for t in range(NT):
            for half in range(2):
                ps, B_ps, _ = c.psF.next()
                for k in range(8):
                    S.op("tensor", "matmul", out=ps[:], lhsT=fT[:, k, t * 128:(t + 1) * 128], rhs=wout[:, k, half * 512:(half + 1) * 512],
                         start=(k == 0), stop=(k == 7), reads=[B_fT, B_wout], writes=[B_ps])
                xs = c.x[:, t, half * 512:(half + 1) * 512]
                S.op("vector", "tensor_tensor", out=xs, in0=ps[:], in1=xs, op=ALU.add, reads=[B_ps, c.B_x[t]], writes=[c.B_x[t]])
        S.barrier()


def load_bcast_row(c, name, row_d, n):
    t = c.sb(name, [128, n], F32)
    B = Buf(name)
    c.S.op("sync", "dma_start", out=t[:], in_=row_d.partition_broadcast(128), writes=[B], dma=name)
    return t, B


def ffn_norm_step(c, gain_row_d, fT, B_fT, router_T_d=None):
    S = c.S
    G = None
    B_G = None
    if router_T_d is not None:
        G = c.sb(c.key("G"), [128, NT, 8], F32)
        B_G = Buf()
    with contextlib.ExitStack() as st2:
        c2 = Ctx(c.nc, S, st2)
        c2.dkey = c.dkey + 5000
        gain, B_gain = load_bcast_row(c2, c.key("fgain"), gain_row_d, D)
        junk = Ring(c2, c.key("fjunk"), [128, D], BF16, 2)
        small = Ring(c2, c.key("fsmall"), [128, 4], F32, 4)
        xn_r = Ring(c2, c.key("fxn"), [128, D], BF16, 2)
        if router_T_d is not None:
            wrg = c2.sb(c.key("wrg"), [128, 8, D], F32)
            B_wrg = Buf()
            kwr = c.key("wrgl")
            for e in range(8):
                S.op("sync", "dma_start", out=wrg[:, e, :], in_=router_T_d[e:e + 1, :].partition_broadcast(128), writes=[B_wrg], dma=kwr)
            for e in range(8):
                S.op("gpsimd", "tensor_tensor", out=wrg[:, e, :], in0=wrg[:, e, :], in1=gain[:], op=ALU.mult, reads=[B_wrg, B_gain], writes=[B_wrg])
            rj = Ring(c2, c.key("rjunk"), [128, D], F32, 2)
            lg_r = Ring(c2, c.key("lg"), [128, 32], F32, 3)
        for t in range(NT):
            jk, B_jk, _ = junk.next()
            sm, B_sm, _ = small.next()
            xn, B_xn, _ = xn_r.next()
            xt = c.x[:, t, :]
            S.op("scalar", "activation", out=jk[:], in_=xt, func=AF.Square, accum_out=sm[:, 0:1], reads=[c.B_x[t]], writes=[B_jk, B_sm])
            S.op("scalar", "activation", out=sm[:, 1:2], in_=sm[:, 0:1], func=AF.Sqrt, scale=1.0 / D, bias=c.eps_t[:, 0:1], reads=[B_sm, c.B_eps], writes=[B_sm])
            S.op("vector", "reciprocal", out=sm[:, 2:3], in_=sm[:, 1:2], reads=[B_sm], writes=[B_sm])
            S.op("vector", "scalar_tensor_tensor", out=xn[:], in0=xt, scalar=sm[:, 2:3], in1=gain[:], op0=ALU.mult, op1=ALU.mult,
                 reads=[c.B_x[t], B_sm, B_gain], writes=[B_xn])
            pT, B_pT, _ = c.psT.next()
            for k in range(8):
                S.op("tensor", "transpose", out=pT[:, k, :], in_=xn[:, k * 128:(k + 1) * 128], identity=c.ident[:], reads=[B_xn, c.B_ident], writes=[B_pT])
            S.op("scalar", "copy", out=fT[:, :, t * 128:(t + 1) * 128], in_=pT[:], reads=[B_pT], writes=[B_fT])
            if router_T_d is not None:
                lg, B_lg, _ = lg_r.next()
                S.op("vector", "memset", ap=lg[:], constant=0.0, writes=[B_lg])
                for e in range(8):
                    r, B_r, _ = rj.next()
                    S.op("vector", "scalar_tensor_tensor", out=r[:], in0=xt, scalar=sm[:, 2:3], in1=wrg[:, e, :], op0=ALU.mult, op1=ALU.mult,
                         accum_out=lg[:, e:e + 1], reads=[c.B_x[t], B_sm, B_wrg], writes=[B_r, B_lg])
                S.op("vector", "max", out=lg[:, 8:16], in_=lg[:, 0:8], reads=[B_lg], writes=[B_lg])
                S.op("vector", "tensor_tensor", out=lg[:, 16:17], in0=lg[:, 9:10], in1=lg[:, 8:9], op=ALU.subtract, reads=[B_lg], writes=[B_lg])
                S.op("scalar", "activation", out=lg[:, 16:17], in_=lg[:, 16:17], func=AF.Exp, reads=[B_lg], writes=[B_lg])
                S.op("vector", "tensor_scalar", out=lg[:, 17:18], in0=lg[:, 16:17], scalar1=1.0, scalar2=None, op0=ALU.add, reads=[B_lg], writes=[B_lg])
                S.op("vector", "reciprocal", out=lg[:, 17:18], in_=lg[:, 17:18], reads=[B_lg], writes=[B_lg])
                S.op("vector", "tensor_scalar", out=lg[:, 18:19], in0=lg[:, 17:18], scalar1=-1.0, scalar2=1.0, op0=ALU.mult, op1=ALU.add, reads=[B_lg], writes=[B_lg])
                S.op("vector", "tensor_scalar", out=lg[:, 20:28], in0=lg[:, 0:8], scalar1=lg[:, 8:9], scalar2=lg[:, 17:18], op0=ALU.is_equal, op1=ALU.mult,
                     reads=[B_lg], writes=[B_lg])
                S.op("vector", "tensor_scalar", out=G[:, t, :], in0=lg[:, 0:8], scalar1=lg[:, 9:10], scalar2=lg[:, 18:19], op0=ALU.is_equal, op1=ALU.mult,
                     reads=[B_lg], writes=[B_G])
                S.op("vector", "tensor_tensor", out=G[:, t, :], in0=G[:, t, :], in1=lg[:, 20:28], op=ALU.add, reads=[B_lg, B_G], writes=[B_G])
        S.barrier()
    return G, B_G


class FfnBufs:
    def __init__(self, c2):
        self.wg = Ring(c2, c2.key("wg"), [128, 8, 512], BF16, 2)
        self.wu = Ring(c2, c2.key("wu"), [128, 8, 512], BF16, 2)
        self.wd = Ring(c2, c2.key("wd"), [128, 4, D], BF16, 2)
        self.act = Ring(c2, c2.key("act"), [128, 4, TB], BF16, 2)
        self.sg = Ring(c2, c2.key("sgate"), [128, TB], F32, 3)


def swiglu_accumulate(c, fb, fT, B_fT, wg_d, wu_d, wd_d, FF, G=None, B_G=None, e=None):
    S = c.S
    g0 = 0
    while g0 < FF:
        gw = min(512, FF - g0)
        nch = gw // 128
        wg, B_wg, k1 = fb.wg.next()
        wu, B_wu, k2 = fb.wu.next()
        wd, B_wd, k3 = fb.wd.next()
        S.op("gpsimd", "dma_start", out=wg[:, :, 0:gw], in_=wg_d[:, g0:g0 + gw].rearrange("(k p) c -> p k c", p=128), writes=[B_wg], dma=k1)
        S.op("gpsimd", "dma_start", out=wu[:, :, 0:gw], in_=wu_d[:, g0:g0 + gw].rearrange("(k p) c -> p k c", p=128), writes=[B_wu], dma=k2)
        S.op("gpsimd", "dma_start", out=wd[:, 0:nch, :], in_=wd_d[g0:g0 + gw, :].rearrange("(c p) n -> p c n", p=128), writes=[B_wd], dma=k3)
        for tb in range(NTB):
            ts = slice(tb * TB, (tb + 1) * TB)
            act, B_act, _ = fb.act.next()
            for cch in range(nch):
                psg, B_psg, _ = c.psF.next()
                for k in range(8):
                    S.op("tensor", "matmul", out=psg[:], lhsT=wg[:, k, cch * 128:(cch + 1) * 128], rhs=fT[:, k, ts], start=(k == 0), stop=(k == 7),
                         reads=[B_wg, B_fT], writes=[B_psg])
                psu, B_psu, _ = c.psF.next()
                for k in range(8):
                    S.op("tensor", "matmul", out=psu[:], lhsT=wu[:, k, cch * 128:(cch + 1) * 128], rhs=fT[:, k, ts], start=(k == 0), stop=(k == 7),
                         reads=[B_wu, B_fT], writes=[B_psu])
                sg, B_sg, _ = fb.sg.next()
                S.op("scalar", "activation", out=sg[:], in_=psg[:], func=AF.Silu, reads=[B_psg], writes=[B_sg])
                S.op("vector", "tensor_tensor", out=act[:, cch, :], in0=psu[:], in1=sg[:], op=ALU.mult, reads=[B_psu, B_sg], writes=[B_act])
            for j in range(4):
                t = tb * 4 + j
                for half in range(2):
                    psy, B_psy, _ = c.psF.next()
                    for cch in range(nch):
                        S.op("tensor", "matmul", out=psy[:], lhsT=act[:, cch, j * 128:(j + 1) * 128], rhs=wd[:, cch, half * 512:(half + 1) * 512],
                             start=(cch == 0), stop=(cch == nch - 1), reads=[B_act, B_wd], writes=[B_psy])
                    xs = c.x[:, t, half * 512:(half + 1) * 512]
                    if G is None:
                        S.op("vector", "tensor_tensor", out=xs, in0=psy[:], in1=xs, op=ALU.add, reads=[B_psy, c.B_x[t]], writes=[c.B_x[t]])
                    else:
                        S.op("vector", "scalar_tensor_tensor", out=xs, in0=psy[:], scalar=G[:, t, e:e + 1], in1=xs, op0=ALU.mult, op1=ALU.add,
                             reads=[B_psy, c.B_x[t], B_G], writes=[c.B_x[t]])
        g0 += gw


def final_norm_store(c, gain_row_d, out_d):
    S = c.S
    final = []
    with contextlib.ExitStack() as st2:
        c2 = Ctx(c.nc, S, st2)
        gain, B_gain = load_bcast_row(c2, c.key("fin_gain"), gain_row_d, D)
        junk = Ring(c2, c.key("finjunk"), [128, D], BF16, 2)
        small = Ring(c2, c.key("finsmall"), [128, 4], F32, 4)
        o_r = Ring(c2, c.key("fino"), [128, D], F32, 3)
        for t in range(NT):
            jk, B_jk, _ = junk.next()
            sm, B_sm, _ = small.next()
            xt = c.x[:, t, :]
            S.op("scalar", "activation", out=jk[:], in_=xt, func=AF.Square, accum_out=sm[:, 0:1], reads=[c.B_x[t]], writes=[B_jk, B_sm])
            S.op("scalar", "activation", out=sm[:, 1:2], in_=sm[:, 0:1], func=AF.Sqrt, scale=1.0 / D, bias=c.eps_t[:, 0:1], reads=[B_sm, c.B_eps], writes=[B_sm])
            S.op("vector", "reciprocal", out=sm[:, 2:3], in_=sm[:, 1:2], reads=[B_sm], writes=[B_sm])
            o, B_o, key = o_r.next()
            S.op("vector", "scalar_tensor_tensor", out=o[:], in0=xt, scalar=sm[:, 2:3], in1=gain[:], op0=ALU.mult, op1=ALU.mult,
                 reads=[c.B_x[t], B_sm, B_gain], writes=[B_o])
            final.append(S.op("sync", "dma_start", out=out_d[t * 128:(t + 1) * 128, :], in_=o[:], reads=[B_o], dma=key))
        S.barrier()
    return final


def phase_b(c, d, layer):
    S = c.S
    fT = c.sb(c.key("fT"), [128, 8, NTOK], BF16)
    B_fT = Buf()
    wout_step(c, d["mixv"], d["w_out"], fT, B_fT)
    moe = (layer % 2 == 1)
    G, B_G = ffn_norm_step(c, d["ffn_norm"], fT, B_fT, d["router_T"] if moe else None)
    with contextlib.ExitStack() as st2:
        c2 = Ctx(c.nc, S, st2)
        c2.dkey = c.dkey + 7000
        fb = FfnBufs(c2)
        if not moe:
            swiglu_accumulate(c, fb, fT, B_fT, d["wg"], d["wu"], d["wd"], 2816)
        else:
            for e in range(8):
                swiglu_accumulate(c, fb, fT, B_fT, d["mwg"][e], d["mwu"][e], d["mwd"][e], 3584, G, B_G, e)
        S.barrier()


U32 = mybir.dt.uint32


def make_idx(core):
    b, hj = core // 4, core % 4
    p = np.arange(128, dtype=np.int64)
    idx = np.zeros((128, IDX_N), np.int64)
    for j in range(4):
        r = 4 * b + j
        idx[:, IDX_QN + j] = r * PA16_ROWS + PA16_BASE["QN"] + hj * 128 + p
        idx[:, IDX_KN + j] = r * PA16_ROWS + PA16_BASE["KN"] + hj * 128 + p
        idx[:, IDX_VT + j] = r * PA16_ROWS + PA16_BASE["VT"] + hj * 128 + p
        idx[:64, IDX_QP + j] = r * PA16_ROWS + PA16_BASE["QP"] + hj * 64 + p[:64]
        idx[:64, IDX_KPE + j] = r * PA16_ROWS + PA16_BASE["KPE"] + p[:64]
    for sb in range(16):
        r = 4 * b + sb // 4
        q = sb % 4
        for name, col in (("HQ", IDX_HQ), ("HIT", IDX_HIT), ("HGT", IDX_HGT)):
            idx[:, col + sb] = (r * PA16_ROWS + PA16_BASE[name] + hj * 128 + p) * 4 + q
        idx[:, IDX_ZF + sb] = (r * 512 + hj * 128 + p) * 4 + q
    for k in range(8):
        part, h2 = k // 4, k % 4
        idx[:, IDX_MIX + k] = ((4 * b + h2) * 256 + part * 128 + p) * 4 + hj
    return idx.astype(np.uint32)


def tagged_in(nc, name, rows, cols):
    return dram_in(nc, name, [rows + 1, cols], F32)[0:rows, :]


def tag_rows(a2d, core):
    return np.concatenate([a2d, np.full((1, a2d.shape[1]), float(core), np.float32)], axis=0)


TAGGED = ("w_in0", "w_in1", "w_q_b0", "w_q_b1", "w_kv_b0", "w_kv_b1", "w_out0", "w_out1", "wg", "wu", "wd", "mwg", "mwu", "mwd")


def build_fused():
    nc = bass.Bass("TRN2", target_bir_lowering=False)
    x_d = dram_in(nc, "x", [NTOK, D], F32)
    pos_d = dram_in(nc, "pos", [1, NTOK], I32)
    ident_d = dram_in(nc, "ident", [128, 128], BF16)
    ones_d = dram_in(nc, "ones", [128, 128], BF16)
    inv_d = dram_in(nc, "inv", [64, 1], F32)
    idx_d = dram_in(nc, "idx", [128, IDX_N], U32)
    md = {"cmask": dram_in(nc, "cmask", [128, 128], BF16), "hmask": dram_in(nc, "hmask", [64, 64], F32),
          "resetm": dram_in(nc, "resetm", [128, 512], F32), "lbraw": dram_in(nc, "lbraw", [128, 2], F32)}
    gn_d = [dram_in(nc, "gn%d" % l, [128, 1], F32) for l in range(2)]
    W = []
    for l in range(2):
        W.append({"w_in": tagged_in(nc, "w_in%d" % l, D, IN_COLS), "mix_norm": dram_in(nc, "mix_norm%d" % l, [D], F32),
                  "w_q_b": tagged_in(nc, "w_q_b%d" % l, 384, 768), "q_a_norm": dram_in(nc, "q_a_norm%d" % l, [384], F32),
                  "w_kv_b": tagged_in(nc, "w_kv_b%d" % l, 256, 1024), "kv_a_norm": dram_in(nc, "kv_a_norm%d" % l, [256], F32),
                  "w_out": tagged_in(nc, "w_out%d" % l, D, D), "ffn_norm": dram_in(nc, "ffn_norm%d" % l, [1, D], F32)})
    W[0].update({"wg": tagged_in(nc, "wg", D, 2816), "wu": tagged_in(nc, "wu", D, 2816), "wd": tagged_in(nc, "wd", 2816, D)})
    W[1].update({"router_T": dram_in(nc, "router_T", [8, D], F32),
                 "mwg": tagged_in(nc, "mwg", 8 * D, 3584).rearrange("(e r) c -> e r c", e=8),
                 "mwu": tagged_in(nc, "mwu", 8 * D, 3584).rearrange("(e r) c -> e r c", e=8),
                 "mwd": tagged_in(nc, "mwd", 8 * 3584, D).rearrange("(e r) c -> e r c", e=8)})
    fin_d = dram_in(nc, "final_norm", [1, D], F32)
    out_d = nc.dram_tensor("OUT", [NTOK, D], F32, kind="ExternalOutput").ap()
    pa16 = nc.dram_tensor("pa16", [PA16_ROWS, NTOK], BF16).ap()
    pa32 = nc.dram_tensor("pa32", [512, NTOK], F32).ap()
    pa16all = nc.dram_tensor("pa16all", [8 * PA16_ROWS, NTOK], BF16).ap()
    pa32all = nc.dram_tensor("pa32all", [8 * 512, NTOK], F32).ap()
    mix = nc.dram_tensor("mixloc", [256, SEQ], BF16).ap()
    mixall = nc.dram_tensor("mixall", [8 * 256, SEQ], BF16).ap()
    xo_d = nc.dram_tensor("xo", [NTOK, D], F32).ap()
    pav = pa_views(pa16, pa32)
    md["pa16v"] = pa16all
    md["pa16v4"] = pa16all.rearrange("r (q c) -> (r q) c", q=4)
    md["pa32v4"] = pa32all.rearrange("r (q c) -> (r q) c", q=4)
    mixv = mixall.rearrange("r (q c) -> (r q) c", q=4)

    S = Sched(nc)
    c = new_ctx(nc, S)
    final = []
    with c.st:
        load_consts(c, ident_d, ones_d)
        c.idx = c.sb("idx", [128, IDX_N], U32)
        c.B_idx = Buf()
        S.op("sync", "dma_start", out=c.idx[:], in_=idx_d, writes=[c.B_idx], dma="idx")
        rope_tables(c, pos_d, inv_d)
        for l in range(2):
            w = W[l]
            S.scope = "L%d_phaseA" % l
            with contextlib.ExitStack() as st:
                ca = c.sub(st)
                load_phase_a_weights(ca, w["w_in"], w["mix_norm"], w["w_q_b"], w["q_a_norm"], w["w_kv_b"], w["kv_a_norm"])
                x_ring = Ring(ca, ca.key("xin"), [128, D], F32, 3)
                xsrc_d = x_d if l == 0 else xo_d

                def x_src(t, x_ring=x_ring, xsrc_d=xsrc_d):
                    xt, B_xt, key = x_ring.next()
                    S.op("sync", "dma_start", out=xt[:], in_=xsrc_d[t * 128:(t + 1) * 128, :], writes=[B_xt], dma=key)
                    return xt[:], B_xt
                phase_a(ca, x_src, pav)
                S.barrier()
            S.scope = "L%d_gatherA" % l
            S.all_gather(pa16, pa16all, dma=c.key("ag"))
            S.all_gather(pa32, pa32all, dma=c.key("ag"))
            S.barrier()
            md["gn"] = gn_d[l]
            S.scope = "L%d_attn" % l
            attention(c, md, mix[0:128, :], pfx="a%d" % l)
            S.scope = "L%d_hgrn" % l
            hgrn(c, md, mix[128:256, :], l, pfx="h%d" % l)
            S.scope = "L%d_gatherM" % l
            S.all_gather(mix, mixall, dma=c.key("ag"))
            S.barrier()
            S.scope = "L%d_phaseB" % l
            with contextlib.ExitStack() as st:
                cb = c.sub(st)
                load_x_resident(cb, x_d if l == 0 else xo_d, "xr%d" % l)
                d = dict(w)
                d["mixv"] = mixv
                phase_b(cb, d, l)
                if l == 1:
                    final = final_norm_store(cb, fin_d, out_d)
                else:
                    for t in range(NT):
                        S.op("sync", "dma_start", out=xo_d[t * 128:(t + 1) * 128, :], in_=cb.x[:, t, :], reads=[cb.B_x[t]], dma="xo%d" % t)
                    S.barrier()
        S.emit(final_waits=final)
    return nc


_PROG = {}


def _c(a):
    return np.ascontiguousarray(a)


def kernel(x, positions, mix_norm, w_in, q_a_norm, w_q_b, kv_a_norm, w_kv_b, hg_lower_bounds, hg_out_norm, w_out, ffn_norm,
           dense_w_gate, dense_w_up, dense_w_down, moe_router, moe_w_gate, moe_w_up, moe_w_down, final_norm):
    f32 = lambda a: np.asarray(a, dtype=np.float32)
    consts = const_inputs()
    mc = mixer_consts()
    xf = f32(x).reshape(-1, D)
    posf = np.asarray(positions).reshape(-1).astype(np.int32)
    shared = {"ident": consts["ident"], "ones": consts["ones"], "inv": consts["inv"], "cmask": mc["cmask"], "hmask": mc["hmask"], "resetm": mc["resetm"]}
    for l in range(2):
        shared["w_in%d" % l] = _c(f32(w_in)[l])
        shared["mix_norm%d" % l] = _c(f32(mix_norm)[l])
        shared["w_q_b%d" % l] = _c(f32(w_q_b)[l])
        shared["q_a_norm%d" % l] = _c(f32(q_a_norm)[l])
        shared["w_kv_b%d" % l] = _c(f32(w_kv_b)[l])
        shared["kv_a_norm%d" % l] = _c(f32(kv_a_norm)[l])
        shared["w_out%d" % l] = _c(f32(w_out)[l])
        shared["ffn_norm%d" % l] = _c(f32(ffn_norm)[l]).reshape(1, D)
        shared["gn%d" % l] = _c(f32(hg_out_norm)[l]).reshape(128, 1)
    shared["wg"] = _c(f32(dense_w_gate)[0])
    shared["wu"] = _c(f32(dense_w_up)[0])
    shared["wd"] = _c(f32(dense_w_down)[0])
    shared["router_T"] = _c(f32(moe_router)[0].T)
    shared["mwg"] = _c(f32(moe_w_gate)[0])
    shared["mwu"] = _c(f32(moe_w_up)[0])
    shared["mwd"] = _c(f32(moe_w_down)[0])
    shared["final_norm"] = _c(f32(final_norm)).reshape(1, D)
    lb = f32(hg_lower_bounds)
    maps = []
    for core in range(8):
        m = dict(shared)
        for n in TAGGED:
            a = shared[n]
            m[n] = tag_rows(a.reshape(-1, a.shape[-1]), core)
        m["x"] = _c(xf[core * NTOK:(core + 1) * NTOK])
        m["pos"] = _c(posf[core * NTOK:(core + 1) * NTOK]).reshape(1, NTOK)
        m["idx"] = make_idx(core)
        h = core % 4
        m["lbraw"] = _c(lb[:, h * 128:(h + 1) * 128].T)
        maps.append(m)
    if "nc" not in _PROG:
        _PROG["nc"] = build_fused()
    res = run_bass_kernel_spmd(_PROG["nc"], maps, core_ids=list(range(8)))
    out = np.concatenate([np.asarray(res.results[c]["OUT"], dtype=np.float32) for c in range(8)], axis=0)
    return out.reshape(2, SEQ, D)
```

```python
import contextlib
import math
import numpy as np
import ml_dtypes
import concourse.bass as bass
import concourse.mybir as mybir
from concourse.bass_utils import run_bass_kernel_spmd

F32 = mybir.dt.float32
BF16 = mybir.dt.bfloat16
I32 = mybir.dt.int32
AF = mybir.ActivationFunctionType
ALU = mybir.AluOpType
AX = mybir.AxisListType
NPBF = ml_dtypes.bfloat16

D = 1024
NTOK = 2048
SEQ = 8192
TB = 512
NTB = NTOK // TB
EPS = 1e-6
SCALE = 192.0 ** -0.5
IN_COLS = 2752
SEM_CAP = 12000
DUMP = False
SCOPES = False
TWO_PI = 2.0 * math.pi
C1 = 6.28125
C2 = TWO_PI - C1


class Buf:
    __slots__ = ("name", "w", "readers", "psum")

    def __init__(self, name="", psum=False):
        self.name = name
        self.w = None
        self.readers = {}
        self.psum = psum


class Op:
    __slots__ = ("eng", "fn", "deps", "signal", "sem", "val", "dma", "idx", "gidx", "inc", "scope")


class Sched:
    ENGS = ("sync", "scalar", "vector", "gpsimd", "tensor")

    def __init__(self, nc):
        self.nc = nc
        self.q = {e: [] for e in self.ENGS}
        self.n = 0
        self.dmas_since_barrier = []
        self.barrier_marks = []
        self.scope = None

    def add(self, eng, fn, reads=(), writes=(), dma=None, after=(), inc=16):
        op = Op()
        op.inc = inc
        op.scope = self.scope
        op.eng = eng
        op.fn = fn
        op.signal = False
        op.sem = None
        op.val = None
        op.dma = dma
        op.gidx = self.n
        self.n += 1
        deps = {}

        def dep(p):
            if p is None:
                return
            if p.dma is None and p.eng == eng and eng == "tensor" and dma is None:
                return
            key = ("dma", id(p)) if p.dma is not None else p.eng
            o = deps.get(key)
            if o is None or o.gidx < p.gidx:
                deps[key] = p

        for r in reads:
            dep(r.w)
        for w in writes:
            dep(w.w)
            for p in w.readers.values():
                if p.dma is None and p.eng == eng and dma is None:
                    continue
                dep(p)
        for p in after:
            dep(p)
        for p in deps.values():
            p.signal = True
        op.deps = list(deps.values())
        for r in reads:
            key = ("dma", id(op)) if dma is not None else eng
            r.readers[key] = op
            assert not (r.psum and len(r.readers) > 1), "PSUM tile %s read by two engines" % r.name
        for w in writes:
            w.w = op
            w.readers = {}
        if dma is not None:
            op.signal = True
            self.dmas_since_barrier.append(op)
        op.idx = len(self.q[eng])
        self.q[eng].append(op)
        return op

    def op(self, eng, meth, reads=(), writes=(), dma=None, after=(), inc=16, **kw):
        if meth == "dma_nc":
            def fn(e, kw=kw):
                with self.nc.allow_non_contiguous_dma(reason="tiny strided vector"):
                    return e.dma_start(**kw)
        else:
            def fn(e, meth=meth, kw=kw):
                return getattr(e, meth)(**kw)
        return self.add(eng, fn, reads=reads, writes=writes, dma=dma, after=after, inc=inc)

    def gather_rows(self, out, src, idx_col, reads=(), writes=(), dma=None):
        def fn(e, out=out, src=src, idx_col=idx_col):
            return e.indirect_dma_start(out=out, out_offset=None, in_=src, in_offset=bass.IndirectOffsetOnAxis(ap=idx_col, axis=0))
        return self.add("gpsimd", fn, reads=reads, writes=writes, dma=dma)

    def all_gather(self, src, dst, reads=(), writes=(), dma=None):
        def fn(e, src=src, dst=dst):
            return e.collective_compute("AllGather", ALU.bypass, replica_groups=[list(range(8))], ins=[src], outs=[dst])
        return self.add("gpsimd", fn, reads=reads, writes=writes, dma=dma, inc=1)

    def barrier(self):
        lasts = [self.q[e][-1] for e in self.ENGS if self.q[e] and self.q[e][-1].dma is None]
        tails = []
        for e in ("scalar", "vector", "gpsimd", "tensor"):
            ops = [o for o in self.q[e] if o.dma is None]
            if ops:
                tails.append(ops[-1])
        dmas = list(self.dmas_since_barrier)
        self.dmas_since_barrier = []
        self.barrier_marks.append(self.n)
        for e in self.ENGS:
            self.add(e, lambda eng: eng.nop(), after=tails + dmas)

    def emit(self, final_waits=()):
        nc = self.nc
        sems = []
        stack = contextlib.ExitStack()
        with stack:
            def newsem(name):
                s = stack.enter_context(nc.semaphore(name))
                sems.append(s)
                return s
            dsem = {}
            free = []
            marks = list(self.barrier_marks)
            allops = sorted((op for e in self.ENGS for op in self.q[e] if op.dma is not None), key=lambda o: o.gidx)
            for op in allops:
                while marks and op.gidx >= marks[0]:
                    marks.pop(0)
                    free.extend(dsem.values())
                    dsem = {}
                ent = dsem.get(op.dma)
                if ent is None:
                    ent = free.pop() if free else [newsem("d%d" % len(sems)), 0]
                    dsem[op.dma] = ent
                ent[1] += op.inc
                op.sem = ent[0]
                op.val = ent[1]
            for e in self.ENGS:
                cur = None
                cnt = 0
                k = 0
                for op in self.q[e]:
                    if op.dma is None and op.signal:
                        if cur is None or cnt >= SEM_CAP:
                            cur = newsem("c_%s_%d" % (e, k))
                            k += 1
                            cnt = 0
                        cnt += 1
                        op.sem = cur
                        op.val = cnt
            self.nsems = len(sems)
            with nc.Block() as block:
                def run(e):
                    def body(eng):
                        known = {}
                        for op in self.q[e]:
                            best = {}
                            for p in op.deps:
                                o = best.get(id(p.sem))
                                if o is None or o.val < p.val:
                                    best[id(p.sem)] = p
                            for p in best.values():
                                kk = id(p.sem)
                                if known.get(kk, 0) < p.val:
                                    eng.wait_ge(p.sem, p.val)
                                    known[kk] = p.val
                                    if DUMP:
                                        print(e, "  wait", p.sem, p.val)
                            if SCOPES and op.scope is not None:
                                with nc.named_scope(op.scope):
                                    ins = op.fn(eng)
                            else:
                                ins = op.fn(eng)
                            if op.signal:
                                ins.then_inc(op.sem, op.inc if op.dma is not None else 1)
                            if DUMP:
                                print(e, "op", op.gidx, "signal" if op.signal else "", op.sem if op.signal else "", op.val if op.signal else "")
                        if e == "sync":
                            for p in final_waits:
                                eng.wait_ge(p.sem, p.val)
                    return body
                block.sync(run("sync"))
                block.scalar(run("scalar"))
                block.vector(run("vector"))
                block.gpsimd(run("gpsimd"))
                block.tensor(run("tensor"))


class Ring:
    def __init__(self, ctx, name, shape, dt, n, psum=False):
        self.tiles = []
        for i in range(n):
            if psum:
                t = ctx.st.enter_context(ctx.nc.psum_tensor("p_%s%d" % (name, i), shape, dt))
            else:
                t = ctx.st.enter_context(ctx.nc.sbuf_tensor("s_%s%d" % (name, i), shape, dt))
            self.tiles.append((t, Buf("%s%d" % (name, i), psum=psum)))
        self.i = 0
        self.name = name

    @classmethod
    def of(cls, name, tiles):
        r = cls.__new__(cls)
        r.tiles = list(tiles)
        r.i = 0
        r.name = name
        return r

    def next(self):
        t = self.tiles[self.i % len(self.tiles)]
        slot = self.i % len(self.tiles)
        self.i += 1
        return t[0], t[1], "%s_%d" % (self.name, slot)


class Ctx:
    gkey = 0
    SHARED = ("ident", "B_ident", "ones", "B_ones", "psT", "psF", "eps_t", "B_eps", "cosT", "sinT", "B_cos", "B_sin", "idx", "B_idx")

    def sub(self, st):
        c2 = Ctx(self.nc, self.S, st)
        for a in Ctx.SHARED:
            if hasattr(self, a):
                setattr(c2, a, getattr(self, a))
        return c2

    def __init__(self, nc, S, st):
        self.nc = nc
        self.S = S
        self.st = st
        self.dkey = 0

    def sb(self, name, shape, dt):
        return self.st.enter_context(self.nc.sbuf_tensor("s_" + name, shape, dt))

    def key(self, base):
        Ctx.gkey += 1
        return "%s_%d" % (base, Ctx.gkey)


def new_ctx(nc, S):
    st = contextlib.ExitStack()
    return Ctx(nc, S, st)


def load_consts(c, ident_d, ones_d):
    S = c.S
    c.ident = c.sb("ident", [128, 128], BF16)
    c.B_ident = Buf()
    c.ones = c.sb("ones", [128, 128], BF16)
    c.B_ones = Buf()
    S.op("sync", "dma_start", out=c.ident[:], in_=ident_d, writes=[c.B_ident], dma="ident")
    S.op("sync", "dma_start", out=c.ones[:], in_=ones_d, writes=[c.B_ones], dma="ones")
    c.psT = Ring(c, "psT", [128, 8, 128], BF16, 2, psum=True)
    c.psF = Ring(c, "psF", [128, 512], F32, 6, psum=True)
    c.eps_t = c.sb("eps_t", [128, 1], F32)
    c.B_eps = Buf()
    S.op("vector", "memset", ap=c.eps_t[:], constant=EPS, writes=[c.B_eps])


def load_gain_cols(c, name, vec_d, nk):
    t = c.sb(name, [128, nk], F32)
    B = Buf(name)
    c.S.op("sync", "dma_nc", out=t[:], in_=vec_d.rearrange("(k p) -> p k", p=128), writes=[B], dma=name)
    return t, B


class NormRings:
    def __init__(self, c):
        self.junk = Ring(c, c.key("junk"), [128, D], BF16, 2)
        self.small = Ring(c, c.key("small"), [128, 4], F32, 6)
        self.xn = Ring(c, c.key("xn"), [128, D], BF16, 2)


def norm_transpose_tile(c, x_ap, B_x, hT_ap, B_hT, col0, nr, xn_dt=BF16):
    S = c.S
    junk, B_junk, _ = nr.junk.next()
    sm, B_sm, _ = nr.small.next()
    xn, B_xn, _ = nr.xn.next()
    S.op("scalar", "activation", out=junk[:], in_=x_ap, func=AF.Square, accum_out=sm[:, 0:1], reads=[B_x], writes=[B_junk, B_sm])
    S.op("scalar", "activation", out=sm[:, 1:2], in_=sm[:, 0:1], func=AF.Sqrt, scale=1.0 / D, bias=c.eps_t[:, 0:1],
         reads=[B_sm, c.B_eps], writes=[B_sm])
    S.op("vector", "reciprocal", out=sm[:, 2:3], in_=sm[:, 1:2], reads=[B_sm], writes=[B_sm])
    S.op("vector", "tensor_scalar", out=xn[:], in0=x_ap, scalar1=sm[:, 2:3], scalar2=None, op0=ALU.mult, reads=[B_x, B_sm], writes=[B_xn])
    pT, B_pT, _ = c.psT.next()
    for k in range(8):
        S.op("tensor", "transpose", out=pT[:, k, :], in_=xn[:, k * 128:(k + 1) * 128], identity=c.ident[:], reads=[B_xn, c.B_ident], writes=[B_pT])
    S.op("scalar", "copy", out=hT_ap[:, :, col0:col0 + 128], in_=pT[:], reads=[B_pT], writes=[B_hT])
    return sm, B_sm


def rope_tables(c, pos_d, inv_d):
    S = c.S
    nc = c.nc
    c.cosT = c.sb("cosT", [64, NTOK], F32)
    c.sinT = c.sb("sinT", [64, NTOK], F32)
    c.B_cos = Buf()
    c.B_sin = Buf()
    with contextlib.ExitStack() as st2:
        def sb(name, shape, dt):
            return st2.enter_context(nc.sbuf_tensor("s_" + name, shape, dt))
        pos_i = sb("pos_i", [64, NTOK], I32)
        B_pi = Buf()
        inv = sb("inv", [64, 1], F32)
        B_inv = Buf()
        ang = sb("ang", [64, NTOK], F32)
        B_ang = Buf()
        ni = sb("ni", [64, NTOK], I32)
        B_ni = Buf()
        nf = sb("nf", [64, NTOK], F32)
        B_nf = Buf()
        r1 = sb("r1", [64, NTOK], F32)
        B_r1 = Buf()
        S.op("sync", "dma_start", out=pos_i[:], in_=pos_d.partition_broadcast(64), writes=[B_pi], dma="pos")
        S.op("sync", "dma_start", out=inv[:], in_=inv_d, writes=[B_inv], dma="inv")
        S.op("vector", "tensor_copy", out=ang[:], in_=pos_i[:], reads=[B_pi], writes=[B_ang])
        S.op("vector", "tensor_scalar", out=ang[:], in0=ang[:], scalar1=inv[:, 0:1], scalar2=None, op0=ALU.mult, reads=[B_ang, B_inv], writes=[B_ang])
        for which, (dst, B_dst) in enumerate(((c.sinT, c.B_sin), (c.cosT, c.B_cos))):
            off = 0.0 if which == 0 else math.pi / 2
            S.op("vector", "tensor_scalar", out=ni[:], in0=ang[:], scalar1=off, scalar2=1.0 / TWO_PI, op0=ALU.add, op1=ALU.mult, reads=[B_ang], writes=[B_ni])
            S.op("vector", "tensor_copy", out=nf[:], in_=ni[:], reads=[B_ni], writes=[B_nf])
            S.op("vector", "scalar_tensor_tensor", out=r1[:], in0=nf[:], scalar=-C1, in1=ang[:], op0=ALU.mult, op1=ALU.add, reads=[B_nf, B_ang], writes=[B_r1])
            S.op("vector", "scalar_tensor_tensor", out=r1[:], in0=nf[:], scalar=-C2, in1=r1[:], op0=ALU.mult, op1=ALU.add, reads=[B_nf, B_r1], writes=[B_r1])
            S.op("vector", "tensor_scalar", out=r1[:], in0=r1[:], scalar1=off, scalar2=math.pi, op0=ALU.add, op1=ALU.min, reads=[B_r1], writes=[B_r1])
            S.op("vector", "tensor_scalar", out=r1[:], in0=r1[:], scalar1=-math.pi, scalar2=None, op0=ALU.max, reads=[B_r1], writes=[B_r1])
            S.op("scalar", "activation", out=dst[:], in_=r1[:], func=AF.Sin, reads=[B_r1], writes=[B_dst])
        S.barrier()


def load_phase_a_weights(c, w_in_d, mix_norm_d, w_q_b_d, q_a_norm_d, w_kv_b_d, kv_a_norm_d):
    S = c.S
    c.win = c.sb(c.key("win"), [128, 8, IN_COLS + 64], BF16)
    c.B_win = Buf()
    c.wqb = c.sb(c.key("wqb"), [128, 3, 768 + 256], BF16)
    c.B_wqb = Buf()
    c.wk = c.sb(c.key("wk"), [128, 2, 512], BF16)
    c.wv = c.sb(c.key("wv"), [128, 2, 512], BF16)
    c.B_wkv = Buf()
    gmix, B_gmix = load_gain_cols(c, c.key("gmix"), mix_norm_d, 8)
    gq, B_gq = load_gain_cols(c, c.key("gq"), q_a_norm_d, 3)
    gkv, B_gkv = load_gain_cols(c, c.key("gkv"), kv_a_norm_d, 2)
    stage = Ring(c, c.key("wstage"), [128, IN_COLS], F32, 2)
    for k in range(8):
        st_t, B_st, key = stage.next()
        S.op("sync", "dma_start", out=st_t[:], in_=w_in_d[k * 128:(k + 1) * 128, :], writes=[B_st], dma=key)
        S.op("vector" if k % 2 == 0 else "gpsimd", "tensor_scalar", out=c.win[:, k, 0:IN_COLS], in0=st_t[:], scalar1=gmix[:, k:k + 1], scalar2=None,
             op0=ALU.mult, reads=[B_st, B_gmix], writes=[c.B_win])
    S.op("vector", "tensor_scalar", out=c.win[:, :, IN_COLS:IN_COLS + 32], in0=c.win[:, :, 672:704], scalar1=-1.0, scalar2=None, op0=ALU.mult,
         reads=[c.B_win], writes=[c.B_win])
    S.op("vector", "tensor_copy", out=c.win[:, :, IN_COLS + 32:IN_COLS + 64], in_=c.win[:, :, 640:672], reads=[c.B_win], writes=[c.B_win])
    for k in range(3):
        st_t, B_st, key = stage.next()
        S.op("sync", "dma_start", out=st_t[:, 0:768], in_=w_q_b_d[k * 128:(k + 1) * 128, :], writes=[B_st], dma=key)
        S.op("vector", "tensor_scalar", out=c.wqb[:, k, 0:768], in0=st_t[:, 0:768], scalar1=gq[:, k:k + 1], scalar2=None, op0=ALU.mult,
             reads=[B_st, B_gq], writes=[c.B_wqb])
    for h in range(4):
        b0 = h * 192 + 128
        S.op("vector", "tensor_scalar", out=c.wqb[:, :, 768 + h * 64:768 + h * 64 + 32], in0=c.wqb[:, :, b0 + 32:b0 + 64], scalar1=-1.0, scalar2=None,
             op0=ALU.mult, reads=[c.B_wqb], writes=[c.B_wqb])
        S.op("vector", "tensor_copy", out=c.wqb[:, :, 768 + h * 64 + 32:768 + h * 64 + 64], in_=c.wqb[:, :, b0:b0 + 32], reads=[c.B_wqb], writes=[c.B_wqb])
    for k in range(2):
        st_t, B_st, key = stage.next()
        S.op("sync", "dma_start", out=st_t[:, 0:1024], in_=w_kv_b_d[k * 128:(k + 1) * 128, :], writes=[B_st], dma=key)
        src = st_t[:, 0:1024].rearrange("p (h t c) -> p h t c", h=4, t=2)
        S.op("vector", "tensor_scalar", out=c.wk[:, k, :].rearrange("p (h c) -> p h c", h=4), in0=src[:, :, 0, :], scalar1=gkv[:, k:k + 1], scalar2=None,
             op0=ALU.mult, reads=[B_st, B_gkv], writes=[c.B_wkv])
        S.op("vector", "tensor_scalar", out=c.wv[:, k, :].rearrange("p (h c) -> p h c", h=4), in0=src[:, :, 1, :], scalar1=gkv[:, k:k + 1], scalar2=None,
             op0=ALU.mult, reads=[B_st, B_gkv], writes=[c.B_wkv])


def phase_a(c, x_src, outs):
    S = c.S
    hT_ring = Ring(c, c.key("hT"), [128, 8, TB], BF16, 2)
    nr = NormRings(c)
    cq_ring = Ring(c, c.key("cqbf"), [128, 3, TB], BF16, 2)
    cqs_ring = Ring(c, c.key("cqsq"), [128, 3, TB], BF16, 2)
    ckv_ring = Ring(c, c.key("ckvbf"), [128, 2, TB], BF16, 2)
    ckvs_ring = Ring(c, c.key("ckvsq"), [128, 2, TB], BF16, 2)
    rbc_ring = Ring(c, c.key("rbc"), [128, TB], F32, 3)
    o16_ring = Ring(c, c.key("o16"), [128, TB], BF16, 8)
    o32_ring = Ring(c, c.key("o32"), [128, TB], F32, 3)
    t32_ring = Ring(c, c.key("t32"), [64, TB], F32, 4)
    final = []

    def store(dst_ap, src_ap, B_src, key):
        final.append(S.op("gpsimd", "dma_start", out=dst_ap, in_=src_ap, reads=[B_src], dma=key))

    def proj_fm(hT, B_hT, c0, M):
        ps, B_ps, _ = c.psF.next()
        for k in range(8):
            S.op("tensor", "matmul", out=ps[0:M, :], lhsT=c.win[:, k, c0:c0 + M], rhs=hT[:, k, :], start=(k == 0), stop=(k == 7),
                 reads=[c.B_win, B_hT], writes=[B_ps])
        return ps, B_ps

    def rms_bc(sq, B_sq, nk, dim):
        ps, B_ps, _ = c.psF.next()
        for k in range(nk):
            S.op("tensor", "matmul", out=ps[:], lhsT=c.ones[:], rhs=sq[:, k, :], start=(k == 0), stop=(k == nk - 1), reads=[c.B_ones, B_sq], writes=[B_ps])
        r, B_r, _ = rbc_ring.next()
        S.op("scalar", "activation", out=r[:], in_=ps[:], func=AF.Sqrt, scale=1.0 / dim, bias=c.eps_t[:, 0:1], reads=[B_ps, c.B_eps], writes=[B_r])
        S.op("vector", "reciprocal", out=r[:], in_=r[:], reads=[B_r], writes=[B_r])
        return r, B_r

    def rope_out(ps_a, B_a, ps_b, B_b, t0, mul_ap, B_mul, dst_ap, scale):
        t1, B_t1, _ = t32_ring.next()
        t2, B_t2, _ = t32_ring.next()
        S.op("vector", "tensor_tensor", out=t1[:], in0=ps_a[0:64, :], in1=c.cosT[:, t0:t0 + TB], op=ALU.mult, reads=[B_a, c.B_cos], writes=[B_t1])
        S.op("vector", "tensor_tensor", out=t2[:], in0=ps_b[0:64, :], in1=c.sinT[:, t0:t0 + TB], op=ALU.mult, reads=[B_b, c.B_sin], writes=[B_t2])
        o, B_o, key = o16_ring.next()
        if mul_ap is None:
            S.op("vector", "tensor_tensor", out=o[0:64, :], in0=t1[:], in1=t2[:], op=ALU.add, reads=[B_t1, B_t2], writes=[B_o])
        else:
            S.op("vector", "tensor_tensor", out=t1[:], in0=t1[:], in1=t2[:], op=ALU.add, reads=[B_t1, B_t2], writes=[B_t1])
            S.op("vector", "scalar_tensor_tensor", out=o[0:64, :], in0=t1[:], scalar=scale, in1=mul_ap[0:64, :], op0=ALU.mult, op1=ALU.mult,
                 reads=[B_t1, B_mul], writes=[B_o])
        store(dst_ap, o[0:64, :], B_o, key)

    for tb in range(NTB):
        t0 = tb * TB
        hT, B_hT, _ = hT_ring.next()
        for j in range(4):
            x_ap, B_x = x_src(tb * 4 + j)
            norm_transpose_tile(c, x_ap, B_x, hT, B_hT, j * 128, nr)
        if getattr(c, "dbg", 0) == 3:
            store(outs["QN"][0, :, t0:t0 + TB], hT[:, 0, :], B_hT, "dbg")
            return final
        cq, B_cq, _ = cq_ring.next()
        cqs, B_cqs, _ = cqs_ring.next()
        for ch in range(3):
            ps, B_ps = proj_fm(hT, B_hT, ch * 128, 128)
            S.op("scalar", "copy", out=cq[:, ch, :], in_=ps[:], reads=[B_ps], writes=[B_cq])
            S.op("vector", "tensor_tensor", out=cqs[:, ch, :], in0=cq[:, ch, :], in1=cq[:, ch, :], op=ALU.mult, reads=[B_cq], writes=[B_cqs])
        if getattr(c, "dbg", 0) in (41, 411, 412):
            return final
        rq, B_rq = rms_bc(cqs, B_cqs, 3, 384.0)
        if getattr(c, "dbg", 0) == 42:
            return final
        for h in range(4):
            ps, B_ps, _ = c.psF.next()
            for k in range(3):
                S.op("tensor", "matmul", out=ps[:], lhsT=c.wqb[:, k, h * 192:h * 192 + 128], rhs=cq[:, k, :], start=(k == 0), stop=(k == 2),
                     reads=[c.B_wqb, B_cq], writes=[B_ps])
            o, B_o, key = o16_ring.next()
            S.op("vector", "scalar_tensor_tensor", out=o[:], in0=ps[:], scalar=SCALE, in1=rq[:], op0=ALU.mult, op1=ALU.mult, reads=[B_ps, B_rq], writes=[B_o])
            store(outs["QN"][h, :, t0:t0 + TB], o[:], B_o, key)
            if getattr(c, "dbg", 0) == 43:
                return final
            psa, B_psa, _ = c.psF.next()
            psb, B_psb, _ = c.psF.next()
            for k in range(3):
                S.op("tensor", "matmul", out=psa[0:64, :], lhsT=c.wqb[:, k, h * 192 + 128:h * 192 + 192], rhs=cq[:, k, :], start=(k == 0), stop=(k == 2),
                     reads=[c.B_wqb, B_cq], writes=[B_psa])
            for k in range(3):
                S.op("tensor", "matmul", out=psb[0:64, :], lhsT=c.wqb[:, k, 768 + h * 64:768 + h * 64 + 64], rhs=cq[:, k, :], start=(k == 0), stop=(k == 2),
                     reads=[c.B_wqb, B_cq], writes=[B_psb])
            if getattr(c, "dbg", 0) == 44:
                return final
            rope_out(psa, B_psa, psb, B_psb, t0, rq, B_rq, outs["QP"][h, :, t0:t0 + TB], SCALE)
        if getattr(c, "dbg", 0) == 4:
            return final
        ckv, B_ckv, _ = ckv_ring.next()
        ckvs, B_ckvs, _ = ckvs_ring.next()
        for ch in range(2):
            ps, B_ps = proj_fm(hT, B_hT, 384 + ch * 128, 128)
            S.op("scalar", "copy", out=ckv[:, ch, :], in_=ps[:], reads=[B_ps], writes=[B_ckv])
            S.op("vector", "tensor_tensor", out=ckvs[:, ch, :], in0=ckv[:, ch, :], in1=ckv[:, ch, :], op=ALU.mult, reads=[B_ckv], writes=[B_ckvs])
        rkv, B_rkv = rms_bc(ckvs, B_ckvs, 2, 256.0)
        for h in range(4):
            ps, B_ps, _ = c.psF.next()
            for k in range(2):
                S.op("tensor", "matmul", out=ps[:], lhsT=c.wk[:, k, h * 128:(h + 1) * 128], rhs=ckv[:, k, :], start=(k == 0), stop=(k == 1),
                     reads=[c.B_wkv, B_ckv], writes=[B_ps])
            o, B_o, key = o16_ring.next()
            S.op("vector", "tensor_tensor", out=o[:], in0=ps[:], in1=rkv[:], op=ALU.mult, reads=[B_ps, B_rkv], writes=[B_o])
            store(outs["KN"][h, :, t0:t0 + TB], o[:], B_o, key)
        for h in range(4):
            ps, B_ps, _ = c.psF.next()
            for k in range(2):
                S.op("tensor", "matmul", out=ps[:], lhsT=c.wv[:, k, h * 128:(h + 1) * 128], rhs=ckv[:, k, :], start=(k == 0), stop=(k == 1),
                     reads=[c.B_wkv, B_ckv], writes=[B_ps])
            o, B_o, key = o16_ring.next()
            S.op("vector", "tensor_tensor", out=o[:], in0=ps[:], in1=rkv[:], op=ALU.mult, reads=[B_ps, B_rkv], writes=[B_o])
            store(outs["VT"][h, :, t0:t0 + TB], o[:], B_o, key)
        if getattr(c, "dbg", 0) == 5:
            return final
        psa, B_psa = proj_fm(hT, B_hT, 640, 64)
        psb, B_psb = proj_fm(hT, B_hT, IN_COLS, 64)
        rope_out(psa, B_psa, psb, B_psb, t0, None, None, outs["KPE"][:, t0:t0 + TB], 1.0)
        if getattr(c, "dbg", 0) == 6:
            return final
        for h in range(4):
            ps, B_ps = proj_fm(hT, B_hT, 704 + h * 128, 128)
            o, B_o, key = o16_ring.next()
            S.op("scalar", "activation", out=o[:], in_=ps[:], func=AF.Silu, reads=[B_ps], writes=[B_o])
            store(outs["HQ"][h, :, t0:t0 + TB], o[:], B_o, key)
        for h in range(4):
            ps, B_ps = proj_fm(hT, B_hT, 1216 + h * 128, 128)
            o, B_o, key = o32_ring.next()
            S.op("vector", "tensor_copy", out=o[:], in_=ps[:], reads=[B_ps], writes=[B_o])
            store(outs["ZF"][h, :, t0:t0 + TB], o[:], B_o, key)
        if getattr(c, "dbg", 0) == 7:
            return final
        for h in range(4):
            ps, B_ps = proj_fm(hT, B_hT, 1728 + h * 128, 128)
            o, B_o, key = o16_ring.next()
            S.op("vector", "tensor_copy", out=o[:], in_=ps[:], reads=[B_ps], writes=[B_o])
            store(outs["HIT"][h, :, t0:t0 + TB], o[:], B_o, key)
        for h in range(4):
            ps, B_ps = proj_fm(hT, B_hT, 2240 + h * 128, 128)
            o, B_o, key = o16_ring.next()
            S.op("scalar", "activation", out=o[:], in_=ps[:], func=AF.Silu, reads=[B_ps], writes=[B_o])
            store(outs["HGT"][h, :, t0:t0 + TB], o[:], B_o, key)
    return final


PA16A_ROWS = 1856
PA16H_ROWS = 1536
PA16A_BASE = {"QN": 0, "KN": 512, "VT": 1024, "QP": 1536, "KPE": 1792}
PA16H_BASE = {"HQ": 0, "HIT": 512, "HGT": 1024}


def pa_views(pa16a, pa16h, pa32):
    o = {}
    for n in ("QN", "KN", "VT"):
        b0 = PA16A_BASE[n]
        o[n] = pa16a[b0:b0 + 512, :].rearrange("(h p) t -> h p t", p=128)
    o["QP"] = pa16a[1536:1792, :].rearrange("(h p) t -> h p t", p=64)
    o["KPE"] = pa16a[1792:1856, :]
    for n in ("HQ", "HIT", "HGT"):
        b0 = PA16H_BASE[n]
        o[n] = pa16h[b0:b0 + 512, :].rearrange("(h p) t -> h p t", p=128)
    o["ZF"] = pa32.rearrange("(h p) t -> h p t", p=128)
    return o


def const_inputs():
    inv = (np.float32(10000.0) ** (-(np.arange(0, 64, 2, dtype=np.float32)) / np.float32(64))).astype(np.float32)
    return {
        "ident": np.eye(128, dtype=np.float32).astype(NPBF),
        "ones": np.ones((128, 128), dtype=np.float32).astype(NPBF),
        "inv": np.concatenate([inv, inv]).reshape(64, 1).astype(np.float32),
    }


def dram_in(nc, name, shape, dt):
    return nc.dram_tensor(name, shape, dt, kind="ExternalInput").ap()


NQT = SEQ // 128
MASK_NEG = -30000.0


def mixer_consts():
    cm = np.where(np.arange(128)[None, :] <= np.arange(128)[:, None], 0.0, MASK_NEG).astype(np.float32)
    hm = (np.arange(64)[:, None] <= np.arange(64)[None, :]).astype(np.float32)
    rs = np.ones((128, 512), np.float32)
    rs[:, ::64] = 0.0
    return {"cmask": cm.astype(NPBF), "hmask": hm, "resetm": rs}


IDX_QN, IDX_KN, IDX_VT, IDX_QP, IDX_KPE, IDX_HQ, IDX_HIT, IDX_HGT, IDX_ZF, IDX_MIX, IDX_N = 0, 4, 8, 12, 16, 20, 36, 52, 68, 84, 92


def attention(c, d, AT_d, nqt=NQT, pfx="a"):
    S = c.S
    nc = c.nc
    final = []
    with contextlib.ExitStack() as st2:
        c2 = Ctx(nc, S, st2)
        qn = c2.sb(pfx + "_qn", [128, SEQ], BF16)
        qp = c2.sb(pfx + "_qp", [64, SEQ], BF16)
        kn = c2.sb(pfx + "_kn", [128, SEQ], BF16)
        kpe = c2.sb(pfx + "_kpe", [64, SEQ], BF16)
        v = c2.sb(pfx + "_v", [128, NQT, 128], BF16)
        vt = c2.sb(pfx + "_vt", [128, SEQ], BF16)
        cmask = c2.sb(pfx + "_cmask", [128, 128], BF16)
        B_in = [Buf() for _ in range(4)]
        B_vt = [Buf() for _ in range(4)]
        B_cm = Buf()
        S.op("sync", "dma_start", out=cmask[:], in_=d["cmask"], writes=[B_cm], dma=c.key("a_cm"))
        CH = 2048
        pv = d["pa16v"]
        for i in range(SEQ // CH):
            sl = slice(i * CH, (i + 1) * CH)
            S.gather_rows(kn[:, sl], pv, c.idx[:, IDX_KN + i:IDX_KN + i + 1], reads=[c.B_idx, d["B_pa_a"]], writes=[B_in[i]], dma=c.key("a_in"))
            S.gather_rows(kpe[:, sl], pv, c.idx[0:64, IDX_KPE + i:IDX_KPE + i + 1], reads=[c.B_idx, d["B_pa_a"]], writes=[B_in[i]], dma=c.key("a_in"))
            S.gather_rows(qn[:, sl], pv, c.idx[:, IDX_QN + i:IDX_QN + i + 1], reads=[c.B_idx, d["B_pa_a"]], writes=[B_in[i]], dma=c.key("a_in"))
            S.gather_rows(qp[:, sl], pv, c.idx[0:64, IDX_QP + i:IDX_QP + i + 1], reads=[c.B_idx, d["B_pa_a"]], writes=[B_in[i]], dma=c.key("a_in"))
            S.gather_rows(vt[:, sl], pv, c.idx[:, IDX_VT + i:IDX_VT + i + 1], reads=[c.B_idx, d["B_pa_a"]], writes=[B_vt[i]], dma=c.key("a_in"))
        ps_s = Ring.of("pss", c.psF.tiles[0:3])
        ps_o = Ring.of("pso", c.psF.tiles[3:5])
        ps_t = Ring.of("pst", c.psT.tiles[0:2])
        p_ring = Ring(c2, pfx + "_p", [128, 512], BF16, 3)
        pT_ring = Ring(c2, pfx + "_pT", [128, 512], BF16, 3)
        st_ring = Ring(c2, pfx + "_stat", [128, 40], F32, 3)
        o_ring = Ring(c2, pfx + "_o", [128, 128], BF16, 2)
        out_ring = Ring(c2, pfx + "_out", [128, 512], BF16, 2)
        for g in range(NQT // 8):
            pt, B_pt, _ = ps_t.next()
            for u in range(8):
                t = g * 8 + u
                S.op("tensor", "transpose", out=pt[:, u, :], in_=vt[:, t * 128:(t + 1) * 128], identity=c.ident[:],
                     reads=[B_vt[(t * 128) // CH], c.B_ident], writes=[B_pt])
            S.op("vector", "tensor_copy", out=v[:, g * 8:(g + 1) * 8, :], in_=pt[:], reads=[B_pt], writes=[B_in[(g * 8 * 128) // CH]])

        def scores(i, kb, w, diag):
            ps, B_ps, _ = ps_s.next()
            q0 = i * 128
            k0 = kb * 512
            rd = list({id(b): b for b in (B_in[q0 // CH], B_in[k0 // CH], B_in[(k0 + w - 1) // CH])}.values())
            S.op("tensor", "matmul", out=ps[:, 0:w], lhsT=qn[:, q0:q0 + 128], rhs=kn[:, k0:k0 + w], start=True, stop=False, reads=rd, writes=[B_ps])
            S.op("tensor", "matmul", out=ps[:, 0:w], lhsT=qp[:, q0:q0 + 128], rhs=kpe[:, k0:k0 + w], start=False, stop=(not diag), reads=rd, writes=[B_ps])
            if diag:
                S.op("tensor", "matmul", out=ps[:, w - 128:w], lhsT=c.ident[:], rhs=cmask[:], start=False, stop=True,
                     reads=[c.B_ident, B_cm], writes=[B_ps])
            return ps, B_ps

        ps_s4 = Ring.of("pss4", c.psF.tiles[0:4])
        ps_o2 = Ring.of("pso2", c.psF.tiles[4:6])
        st_ring4 = Ring(c2, pfx + "_stat4", [128, 40], F32, 4)
        tiles = {}

        def nblocks(i):
            return (i + 1 + 3) // 4

        def width(i, kb):
            return min(4, i + 1 - 4 * kb) * 128

        def scores2(i, kb):
            w = width(i, kb)
            diag = (kb == nblocks(i) - 1)
            ps, B_ps, _ = ps_s4.next()
            q0 = i * 128
            k0 = kb * 512
            rd = list({id(b): b for b in (B_in[q0 // CH], B_in[k0 // CH], B_in[(k0 + w - 1) // CH])}.values())
            S.op("tensor", "matmul", out=ps[:, 0:w], lhsT=qn[:, q0:q0 + 128], rhs=kn[:, k0:k0 + w], start=True, stop=False, reads=rd, writes=[B_ps])
            S.op("tensor", "matmul", out=ps[:, 0:w], lhsT=qp[:, q0:q0 + 128], rhs=kpe[:, k0:k0 + w], start=False, stop=(not diag), reads=rd, writes=[B_ps])
            if diag:
                S.op("tensor", "matmul", out=ps[:, w - 128:w], lhsT=c.ident[:], rhs=cmask[:], start=False, stop=True,
                     reads=[c.B_ident, B_cm], writes=[B_ps])
            return ps, B_ps, w

        def tile_begin(i):
            stt, B_st, _ = st_ring4.next()
            S.op("vector", "memset", ap=stt[:, 16:32], constant=0.0, writes=[B_st])
            tiles[i] = {"stt": stt, "B_st": B_st, "p": {}, "pT": {}}

        def P1(i, kb):
            t = tiles[i]
            ps, B_ps, w = scores2(i, kb)
            S.op("vector", "reduce_max", out=t["stt"][:, kb:kb + 1], in_=ps[:, 0:w], axis=AX.X, reads=[B_ps], writes=[t["B_st"]])

        def P1_fin(i):
            t = tiles[i]
            stt, B_st = t["stt"], t["B_st"]
            S.op("vector", "reduce_max", out=stt[:, 32:33], in_=stt[:, 0:nblocks(i)], axis=AX.X, reads=[B_st], writes=[B_st])
            S.op("vector", "tensor_scalar", out=stt[:, 33:34], in0=stt[:, 32:33], scalar1=-1.0, scalar2=None, op0=ALU.mult, reads=[B_st], writes=[B_st])

        def S2(i, kb):
            t = tiles[i]
            ps, B_ps, w = scores2(i, kb)
            p, B_p, _ = p_ring.next()
            S.op("scalar", "activation", out=p[:, 0:w], in_=ps[:, 0:w], func=AF.Exp, bias=t["stt"][:, 33:34], accum_out=t["stt"][:, 16 + kb:17 + kb],
                 reads=[B_ps, t["B_st"]], writes=[B_p, t["B_st"]])
            t["p"][kb] = (p, B_p)

        def T(i, kb):
            t = tiles[i]
            p, B_p = t["p"].pop(kb)
            nsub = width(i, kb) // 128
            pt, B_pt, _ = ps_t.next()
            for j in range(nsub):
                S.op("tensor", "transpose", out=pt[:, j, :], in_=p[:, j * 128:(j + 1) * 128], identity=c.ident[:], reads=[B_p, c.B_ident], writes=[B_pt])
            pT, B_pT, _ = pT_ring.next()
            S.op("vector", "tensor_copy", out=pT[:, 0:nsub * 128].rearrange("p (j k) -> p j k", k=128), in_=pt[:, 0:nsub, :], reads=[B_pt], writes=[B_pT])
            t["pT"][kb] = (pT, B_pT)

        def V(i, kb):
            t = tiles[i]
            if kb == 0:
                t["po"] = ps_o2.next()
            po, B_po, _ = t["po"]
            pT, B_pT = t["pT"].pop(kb)
            nsub = width(i, kb) // 128
            nb = nblocks(i)
            for j in range(nsub):
                kt = kb * 4 + j
                S.op("tensor", "matmul", out=po[:, 0:128], lhsT=pT[:, j * 128:(j + 1) * 128], rhs=v[:, kt, :],
                     start=(kb == 0 and j == 0), stop=(kb == nb - 1 and j == nsub - 1), reads=[B_pT, B_in[(kt * 128) // CH]], writes=[B_po])

        def fin1(i):
            t = tiles[i]
            stt, B_st = t["stt"], t["B_st"]
            po, B_po, _ = t["po"]
            S.op("vector", "reduce_sum", out=stt[:, 34:35], in_=stt[:, 16:16 + nblocks(i)], axis=AX.X, reads=[B_st], writes=[B_st])
            S.op("vector", "reciprocal", out=stt[:, 35:36], in_=stt[:, 34:35], reads=[B_st], writes=[B_st])
            o, B_o, _ = o_ring.next()
            S.op("scalar", "activation", out=o[:], in_=po[:, 0:128], func=AF.Copy, scale=stt[:, 35:36], reads=[B_po, B_st], writes=[B_o])
            t["o"] = (o, B_o)

        outs_state = {"outt": None}

        def fin2(i):
            t = tiles.pop(i)
            o, B_o = t["o"]
            pt, B_pt, _ = ps_t.next()
            S.op("tensor", "transpose", out=pt[:, 0, :], in_=o[:], identity=c.ident[:], reads=[B_o, c.B_ident], writes=[B_pt])
            if i % 4 == 0:
                outs_state["outt"] = out_ring.next()
            outt = outs_state["outt"]
            S.op("vector", "tensor_copy", out=outt[0][:, (i % 4) * 128:(i % 4 + 1) * 128], in_=pt[:, 0, :], reads=[B_pt], writes=[outt[1]])
            if i % 4 == 3 or i == nqt - 1:
                i0 = (i // 4) * 4
                wd = (i - i0 + 1) * 128
                final.append(S.op("sync", "dma_start", out=AT_d[:, i0 * 128:i0 * 128 + wd], in_=outt[0][:, 0:wd], reads=[outt[1]], dma=outt[2]))

        tile_begin(0)
        for kb in range(nblocks(0)):
            P1(0, kb)
        P1_fin(0)
        for i in range(nqt):
            nb_i = nblocks(i)
            nb_n = nblocks(i + 1) if i + 1 < nqt else 0
            if i + 1 < nqt:
                tile_begin(i + 1)
            for s in range(max(nb_i + 2, nb_n)):
                if s < nb_n:
                    P1(i + 1, s)
                if s < nb_i:
                    S2(i, s)
                if 1 <= s <= nb_i:
                    T(i, s - 1)
                if 2 <= s <= nb_i + 1:
                    V(i, s - 2)
                if s == 1 and i > 0:
                    fin2(i - 1)
            if i + 1 < nqt:
                P1_fin(i + 1)
            fin1(i)
        fin2(nqt - 1)
        S.barrier()
    return final


def hgrn(c, d, RT_d, layer, nsb=SEQ // 512, pfx="h"):
    S = c.S
    nc = c.nc
    final = []
    with contextlib.ExitStack() as st2:
        c2 = Ctx(nc, S, st2)
        hmask = c2.sb(pfx + "_hmask", [64, 64], F32)
        resetm = c2.sb(pfx + "_resetm", [128, 512], F32)
        lbraw = c2.sb(pfx + "_lbraw", [128, 2], F32)
        gn = c2.sb(pfx + "_gn", [128, 1], F32)
        cst = c2.sb(pfx + "_cst", [128, 4], F32)
        B_c = Buf()
        kc = c.key("h_c")
        S.op("sync", "dma_start", out=hmask[:], in_=d["hmask"], writes=[B_c], dma=kc)
        S.op("sync", "dma_start", out=resetm[:], in_=d["resetm"], writes=[B_c], dma=kc)
        S.op("sync", "dma_start", out=lbraw[:], in_=d["lbraw"], writes=[B_c], dma=kc)
        S.op("sync", "dma_start", out=gn[:], in_=d["gn"], writes=[B_c], dma=kc)
        if layer == 0:
            S.op("vector", "memset", ap=cst[:, 0:1], constant=0.0, writes=[B_c])
        else:
            S.op("vector", "tensor_tensor", out=cst[:, 2:3], in0=lbraw[:, 1:2], in1=lbraw[:, 0:1], op=ALU.subtract, reads=[B_c], writes=[B_c])
            S.op("scalar", "activation", out=cst[:, 0:1], in_=cst[:, 2:3], func=AF.Sigmoid, reads=[B_c], writes=[B_c])
        S.op("vector", "tensor_scalar", out=cst[:, 1:2], in0=cst[:, 0:1], scalar1=-1.0, scalar2=1.0, op0=ALU.mult, op1=ALU.add, reads=[B_c], writes=[B_c])
        state = c2.sb(pfx + "_state", [128, 128], F32)
        state_bf = c2.sb(pfx + "_state_bf", [128, 128], BF16)
        B_state = Buf()
        B_sbf = Buf()
        S.op("vector", "memset", ap=state[:], constant=0.0, writes=[B_state])
        S.op("vector", "memset", ap=state_bf[:], constant=0.0, writes=[B_sbf])
        ps_at = Ring.of("psat", c.psF.tiles[0:2])
        ps_st = Ring.of("psst", c.psF.tiles[2:4])
        ps_o = Ring.of("pso", c.psF.tiles[4:6])
        ps_kd = Ring.of("pskd", c.psT.tiles[0:1])
        ps_rt = Ring.of("psrt", c.psT.tiles[1:2])
        in_hq = Ring(c2, pfx + "_hq", [128, 512], BF16, 2)
        in_zf = Ring(c2, pfx + "_zf", [128, 512], F32, 2)
        in_hiT = Ring(c2, pfx + "_hiT", [128, 512], BF16, 2)
        in_sgT = Ring(c2, pfx + "_sgT", [128, 512], BF16, 2)
        in_hi = Ring(c2, pfx + "_hi", [64, 8, 128], BF16, 2)
        in_sg = Ring(c2, pfx + "_sg", [64, 8, 128], BF16, 2)
        f32r = Ring(c2, pfx + "_f32", [128, 512], F32, 8)
        b_ring = Ring(c2, pfx + "_b", [128, 512], F32, 2)
        k_ring = Ring(c2, pfx + "_k", [128, 512], F32, 2)
        qe_ring = Ring(c2, pfx + "_qe", [128, 512], BF16, 2)
        qh_ring = Ring(c2, pfx + "_qh", [128, 512], BF16, 2)
        kh_ring = Ring(c2, pfx + "_kh", [128, 512], BF16, 2)
        kdT_ring = Ring(c2, pfx + "_kdT", [128, 512], BF16, 2)
        kd_ring = Ring(c2, pfx + "_kd", [64, 8, 128], BF16, 2)
        dec_ring = Ring(c2, pfx + "_dec", [128, 8], F32, 2)
        at_ring = Ring(c2, pfx + "_at", [64, 64], BF16, 3)
        sm_ring = Ring(c2, pfx + "_sm", [64, 4], F32, 4)
        junk_ring = Ring(c2, pfx + "_junk", [64, 128], BF16, 2)
        on_ring = Ring(c2, pfx + "_on", [64, 128], F32, 3)
        r_ring = Ring(c2, pfx + "_r", [64, 128], BF16, 3)
        rT_ring = Ring(c2, pfx + "_rT", [128, 512], BF16, 2)

        def issue_loads(sb):
            hq, B_hq, k1 = in_hq.next()
            zf, B_zf, k2 = in_zf.next()
            hiT, B_hiT, k3 = in_hiT.next()
            sgT, B_sgT, k4 = in_sgT.next()
            S.gather_rows(hq[:], d["pa16v4"], c.idx[:, IDX_HQ + sb:IDX_HQ + sb + 1], reads=[c.B_idx, d["B_pa_h"]], writes=[B_hq], dma=k1)
            S.gather_rows(zf[:], d["pa32v4"], c.idx[:, IDX_ZF + sb:IDX_ZF + sb + 1], reads=[c.B_idx, d["B_pa_z"]], writes=[B_zf], dma=k2)
            S.gather_rows(hiT[:], d["pa16v4"], c.idx[:, IDX_HIT + sb:IDX_HIT + sb + 1], reads=[c.B_idx, d["B_pa_h"]], writes=[B_hiT], dma=k3)
            S.gather_rows(sgT[:], d["pa16v4"], c.idx[:, IDX_HGT + sb:IDX_HGT + sb + 1], reads=[c.B_idx, d["B_pa_h"]], writes=[B_sgT], dma=k4)
            return hq, B_hq, zf, B_zf, hiT, B_hiT, sgT, B_sgT

        hmask3 = c2.sb(pfx + "_hmask3", [64, 1, 64], F32)
        S.op("vector", "tensor_copy", out=hmask3[:, 0, :], in_=hmask[:], reads=[B_c], writes=[B_c])
        atl_ring = Ring(c2, pfx + "_atl", [64, 8, 64], BF16, 2)
        state_ring = Ring(c2, pfx + "_stf", [128, 128], F32, 4)
        statebf_ring = Ring(c2, pfx + "_stb", [128, 128], BF16, 12)
        osb_ring = Ring(c2, pfx + "_osb", [64, 512], F32, 2)
        sq_ring = Ring(c2, pfx + "_sq", [64, 512], F32, 2)
        sm12_ring = Ring(c2, pfx + "_sm12", [64, 12], F32, 4)
        r4_ring = Ring(c2, pfx + "_r4", [64, 4, 128], BF16, 4)
        carry = {"f32": (state, B_state), "bf": (state_bf, B_sbf)}
        pending = []
        prt_state = {}

        def flush_pending():
            while pending:
                r, B_r, half, tt0 = pending.pop(0)
                if half == 0:
                    prt_state["t"] = ps_rt.next()
                prt, B_prt, _ = prt_state["t"]
                for j in range(4):
                    S.op("tensor", "transpose", out=prt[:, half * 4 + j, 0:64], in_=r[:, j, :], identity=c.ident[0:64, 0:64], reads=[B_r, c.B_ident], writes=[B_prt])
                if half == 1:
                    rT, B_rT, key = rT_ring.next()
                    S.op("scalar", "activation", out=rT[:].rearrange("p (c t) -> p c t", t=64), in_=prt[:, :, 0:64], func=AF.Copy, scale=gn[:, 0:1],
                         reads=[B_prt, B_c], writes=[B_rT])
                    final.append(S.op("sync", "dma_start", out=RT_d[:, tt0:tt0 + 512], in_=rT[:], reads=[B_rT], dma=key))

        nxt = issue_loads(0)
        for sb in range(nsb):
            t0 = sb * 512
            hq, B_hq, zf, B_zf, hiT, B_hiT, sgT, B_sgT = nxt
            if sb + 1 < nsb:
                nxt = issue_loads(sb + 1)
            hi, B_hi, _ = in_hi.next()
            sg, B_sg, _ = in_sg.next()
            for (srcT, B_srcT, dst, B_dst) in ((hiT, B_hiT, hi, B_hi), (sgT, B_sgT, sg, B_sg)):
                ptr, B_ptr, _ = ps_kd.next()
                for cc in range(8):
                    S.op("tensor", "transpose", out=ptr[0:64, cc, :], in_=srcT[:, cc * 64:(cc + 1) * 64], identity=c.ident[:],
                         reads=[B_srcT, c.B_ident], writes=[B_ptr])
                S.op("scalar", "copy", out=dst[:], in_=ptr[0:64, :, :], reads=[B_ptr], writes=[B_dst])
            ez, B_ez, _ = f32r.next()
            S.op("scalar", "activation", out=ez[:], in_=zf[:], func=AF.Exp, scale=-1.0, reads=[B_zf], writes=[B_ez])
            S.op("vector", "tensor_scalar", out=ez[:], in0=ez[:], scalar1=1.0, scalar2=None, op0=ALU.add, reads=[B_ez], writes=[B_ez])
            S.op("vector", "reciprocal", out=ez[:], in_=ez[:], reads=[B_ez], writes=[B_ez])
            f, B_f, _ = f32r.next()
            S.op("vector", "tensor_scalar", out=f[:], in0=ez[:], scalar1=cst[:, 1:2], scalar2=cst[:, 0:1], op0=ALU.mult, op1=ALU.add,
                 reads=[B_ez, B_c], writes=[B_f])
            kk, B_kk, _ = k_ring.next()
            S.op("gpsimd", "tensor_scalar", out=kk[:], in0=f[:], scalar1=-1.0, scalar2=1.0, op0=ALU.mult, op1=ALU.add, reads=[B_f], writes=[B_kk])
            lf, B_lf, _ = f32r.next()
            S.op("vector", "tensor_scalar", out=lf[:], in0=f[:], scalar1=1e-30, scalar2=None, op0=ALU.max, reads=[B_f], writes=[B_lf])
            S.op("scalar", "activation", out=lf[:], in_=lf[:], func=AF.Ln, reads=[B_lf], writes=[B_lf])
            b, B_b, _ = b_ring.next()
            S.op("vector", "tensor_tensor_scan", out=b[:], data0=resetm[:], data1=lf[:], initial=0.0, op0=ALU.mult, op1=ALU.add,
                 reads=[B_c, B_lf], writes=[B_b])
            b3 = b[:].rearrange("p (c t) -> p c t", t=64)
            bmid = b3[:, :, 31:32].to_broadcast([128, 8, 64])
            blast = b3[:, :, 63:64].to_broadcast([128, 8, 64])
            e0, B_e0, _ = f32r.next()
            S.op("scalar", "activation", out=e0[:], in_=b[:], func=AF.Exp, reads=[B_b], writes=[B_e0])
            qe, B_qe, _ = qe_ring.next()
            S.op("gpsimd", "tensor_tensor", out=qe[:], in0=hq[:], in1=e0[:], op=ALU.mult, reads=[B_hq, B_e0], writes=[B_qe])
            d1, B_d1, _ = f32r.next()
            S.op("vector", "tensor_tensor", out=d1[:].rearrange("p (c t) -> p c t", t=64), in0=b3, in1=bmid, op=ALU.subtract, reads=[B_b], writes=[B_d1])
            e1, B_e1, _ = f32r.next()
            S.op("scalar", "activation", out=e1[:], in_=d1[:], func=AF.Exp, reads=[B_d1], writes=[B_e1])
            qh, B_qh, _ = qh_ring.next()
            S.op("vector", "tensor_tensor", out=qh[:], in0=hq[:], in1=e1[:], op=ALU.mult, reads=[B_hq, B_e1], writes=[B_qh])
            e2, B_e2, _ = f32r.next()
            S.op("scalar", "activation", out=e2[:], in_=d1[:], func=AF.Exp, scale=-1.0, reads=[B_d1], writes=[B_e2])
            kh, B_kh, _ = kh_ring.next()
            S.op("gpsimd", "tensor_tensor", out=kh[:], in0=kk[:], in1=e2[:], op=ALU.mult, reads=[B_kk, B_e2], writes=[B_kh])
            d3, B_d3, _ = f32r.next()
            S.op("vector", "tensor_tensor", out=d3[:].rearrange("p (c t) -> p c t", t=64), in0=blast, in1=b3, op=ALU.subtract, reads=[B_b], writes=[B_d3])
            S.op("scalar", "activation", out=d3[:], in_=d3[:], func=AF.Exp, reads=[B_d3], writes=[B_d3])
            kdT, B_kdT, _ = kdT_ring.next()
            S.op("vector", "tensor_tensor", out=kdT[:], in0=kk[:], in1=d3[:], op=ALU.mult, reads=[B_kk, B_d3], writes=[B_kdT])
            dec, B_dec, _ = dec_ring.next()
            S.op("scalar", "activation", out=dec[:].rearrange("p (c o) -> p c o", o=1), in_=b3[:, :, 63:64], func=AF.Exp, reads=[B_b], writes=[B_dec])
            pkd, B_pkd, _ = ps_kd.next()
            for cc in range(8):
                S.op("tensor", "transpose", out=pkd[0:64, cc, :], in_=kdT[:, cc * 64:(cc + 1) * 64], identity=c.ident[:], reads=[B_kdT, c.B_ident], writes=[B_pkd])
            kd, B_kd, _ = kd_ring.next()
            S.op("scalar", "copy", out=kd[:], in_=pkd[0:64, :, :], reads=[B_pkd], writes=[B_kd])
            pat, B_pat, _ = ps_at.next()
            for cc in range(8):
                cs = slice(cc * 64, (cc + 1) * 64)
                S.op("tensor", "matmul", out=pat[0:64, cs], lhsT=kh[:, cs], rhs=qh[:, cs], start=True, stop=True, reads=[B_kh, B_qh], writes=[B_pat])
            atl, B_atl, _ = atl_ring.next()
            S.op("vector", "tensor_tensor", out=atl[:], in0=pat[0:64, :].rearrange("p (c t) -> p c t", t=64), in1=hmask3[:].to_broadcast([64, 8, 64]),
                 op=ALU.mult, reads=[B_pat, B_c], writes=[B_atl])
            pst2 = [ps_st.next(), ps_st.next()]
            for cc in range(8):
                bank, B_bank, _ = pst2[cc // 4]
                S.op("tensor", "matmul", out=bank[:, (cc % 4) * 128:(cc % 4 + 1) * 128], lhsT=kd[:, cc, :], rhs=hi[:, cc, :], start=True, stop=True,
                     reads=[B_kd, B_hi], writes=[B_bank])
            flush_pending()
            sprev = []
            for cc in range(8):
                bank, B_bank, _ = pst2[cc // 4]
                sprev.append(carry["bf"])
                stn, B_stn, _ = state_ring.next()
                S.op("vector", "scalar_tensor_tensor", out=stn[:], in0=carry["f32"][0][:], scalar=dec[:, cc:cc + 1], in1=bank[:, (cc % 4) * 128:(cc % 4 + 1) * 128],
                     op0=ALU.mult, op1=ALU.add, reads=[carry["f32"][1], B_dec, B_bank], writes=[B_stn])
                sbn, B_sbn, _ = statebf_ring.next()
                S.op("scalar", "copy", out=sbn[:], in_=stn[:], reads=[B_stn], writes=[B_sbn])
                carry["f32"] = (stn, B_stn)
                carry["bf"] = (sbn, B_sbn)
            po2 = [ps_o.next(), ps_o.next()]
            for cc in range(8):
                cs = slice(cc * 64, (cc + 1) * 64)
                bank, B_bank, _ = po2[cc // 4]
                ob = bank[0:64, (cc % 4) * 128:(cc % 4 + 1) * 128]
                S.op("tensor", "matmul", out=ob, lhsT=atl[:, cc, :], rhs=hi[:, cc, :], start=True, stop=False, reads=[B_atl, B_hi], writes=[B_bank])
                S.op("tensor", "matmul", out=ob, lhsT=qe[:, cs], rhs=sprev[cc][0][:], start=False, stop=True, reads=[B_qe, sprev[cc][1]], writes=[B_bank])
            for half in range(2):
                bank, B_bank, _ = po2[half]
                osb, B_osb, _ = osb_ring.next()
                S.op("scalar", "copy", out=osb[:], in_=bank[0:64, :], reads=[B_bank], writes=[B_osb])
                sq, B_sq, _ = sq_ring.next()
                S.op("gpsimd", "tensor_tensor", out=sq[:], in0=osb[:], in1=osb[:], op=ALU.mult, reads=[B_osb], writes=[B_sq])
                sm, B_sm, _ = sm12_ring.next()
                S.op("vector", "reduce_sum", out=sm[:, 0:4], in_=sq[:].rearrange("p (c d) -> p c d", d=128), axis=AX.X, reads=[B_sq], writes=[B_sm])
                S.op("scalar", "activation", out=sm[:, 4:8], in_=sm[:, 0:4], func=AF.Sqrt, scale=1.0 / 128.0, bias=c.eps_t[0:64, 0:1],
                     reads=[B_sm, c.B_eps], writes=[B_sm])
                S.op("vector", "reciprocal", out=sm[:, 8:12], in_=sm[:, 4:8], reads=[B_sm], writes=[B_sm])
                S.op("gpsimd", "tensor_tensor", out=sq[:].rearrange("p (c d) -> p c d", d=128), in0=osb[:].rearrange("p (c d) -> p c d", d=128),
                     in1=sm[:, 8:12].rearrange("p (c o) -> p c o", o=1).to_broadcast([64, 4, 128]), op=ALU.mult, reads=[B_osb, B_sm], writes=[B_sq])
                r, B_r, _ = r4_ring.next()
                S.op("gpsimd", "tensor_tensor", out=r[:], in0=sq[:].rearrange("p (c d) -> p c d", d=128), in1=sg[:, half * 4:(half + 1) * 4, :], op=ALU.mult,
                     reads=[B_sq, B_sg], writes=[B_r])
                pending.append((r, B_r, half, t0))
        flush_pending()
        S.barrier()
    return final


NT = NTOK // 128


def load_x_resident(c, x_d, name="xres"):
    c.x = c.sb(name, [128, NT, D], F32)
    c.B_x = [Buf("x%d" % t) for t in range(NT)]
    for t in range(NT):
        c.S.op("sync", "dma_start", out=c.x[:, t, :], in_=x_d[t * 128:(t + 1) * 128, :], writes=[c.B_x[t]], dma="%s_%d" % (name, t))


def wout_step(c, mixv, w_out_d, fT, B_fT):
    S = c.S
    with contextlib.ExitStack() as st2:
        c2 = Ctx(c.nc, S, st2)
        wout = c2.sb(c.key("wout"), [128, 8, D], BF16)
        B_wout = Buf()
        kwo = c.key("woutl")
        for k in range(8):
            S.gather_rows(fT[:, k, :], mixv, c.idx[:, IDX_MIX + k:IDX_MIX + k + 1], reads=[c.B_idx], writes=[B_fT], dma=c.key("mixT"))
        for k in range(8):
            S.op("gpsimd", "dma_start", out=wout[:, k, :], in_=w_out_d[k * 128:(k + 1) * 128, :], writes=[B_wout], dma=kwo)
        for t in range(NT):
            for half in range(2):
                ps, B_ps, _ = c.psF.next()
                for k in range(8):
                    S.op("tensor", "matmul", out=ps[:], lhsT=fT[:, k, t * 128:(t + 1) * 128], rhs=wout[:, k, half * 512:(half + 1) * 512],
                         start=(k == 0), stop=(k == 7), reads=[B_fT, B_wout], writes=[B_ps])
                xs = c.x[:, t, half * 512:(half + 1) * 512]
                S.op("vector", "tensor_tensor", out=xs, in0=ps[:], in1=xs, op=ALU.add, reads=[B_ps, c.B_x[t]], writes=[c.B_x[t]])
        S.barrier()


def load_bcast_row(c, name, row_d, n):
    t = c.sb(name, [128, n], F32)
    B = Buf(name)
    c.S.op("sync", "dma_start", out=t[:], in_=row_d.partition_broadcast(128), writes=[B], dma=name)
    return t, B


def ffn_norm_step(c, gain_row_d, fT, B_fT, router_T_d=None):
    S = c.S
    G = None
    B_G = None
    if router_T_d is not None:
        G = c.sb(c.key("G"), [128, NT, 8], F32)
        B_G = Buf()
    with contextlib.ExitStack() as st2:
        c2 = Ctx(c.nc, S, st2)
        c2.dkey = c.dkey + 5000
        gain, B_gain = load_bcast_row(c2, c.key("fgain"), gain_row_d, D)
        junk = Ring(c2, c.key("fjunk"), [128, D], BF16, 2)
        small = Ring(c2, c.key("fsmall"), [128, 4], F32, 4)
        xn_r = Ring(c2, c.key("fxn"), [128, D], BF16, 2)
        if router_T_d is not None:
            wrg = c2.sb(c.key("wrg"), [128, 8, D], F32)
            B_wrg = Buf()
            kwr = c.key("wrgl")
            for e in range(8):
                S.op("sync", "dma_start", out=wrg[:, e, :], in_=router_T_d[e:e + 1, :].partition_broadcast(128), writes=[B_wrg], dma=kwr)
            for e in range(8):
                S.op("gpsimd", "tensor_tensor", out=wrg[:, e, :], in0=wrg[:, e, :], in1=gain[:], op=ALU.mult, reads=[B_wrg, B_gain], writes=[B_wrg])
            rj = Ring(c2, c.key("rjunk"), [128, D], F32, 2)
            lg_r = Ring(c2, c.key("lg"), [128, 32], F32, 3)
        for t in range(NT):
            jk, B_jk, _ = junk.next()
            sm, B_sm, _ = small.next()
            xn, B_xn, _ = xn_r.next()
            xt = c.x[:, t, :]
            S.op("scalar", "activation", out=jk[:], in_=xt, func=AF.Square, accum_out=sm[:, 0:1], reads=[c.B_x[t]], writes=[B_jk, B_sm])
            S.op("scalar", "activation", out=sm[:, 1:2], in_=sm[:, 0:1], func=AF.Sqrt, scale=1.0 / D, bias=c.eps_t[:, 0:1], reads=[B_sm, c.B_eps], writes=[B_sm])
            S.op("vector", "reciprocal", out=sm[:, 2:3], in_=sm[:, 1:2], reads=[B_sm], writes=[B_sm])
            S.op("vector", "scalar_tensor_tensor", out=xn[:], in0=xt, scalar=sm[:, 2:3], in1=gain[:], op0=ALU.mult, op1=ALU.mult,
                 reads=[c.B_x[t], B_sm, B_gain], writes=[B_xn])
            pT, B_pT, _ = c.psT.next()
            for k in range(8):
                S.op("tensor", "transpose", out=pT[:, k, :], in_=xn[:, k * 128:(k + 1) * 128], identity=c.ident[:], reads=[B_xn, c.B_ident], writes=[B_pT])
            S.op("scalar", "copy", out=fT[:, :, t * 128:(t + 1) * 128], in_=pT[:], reads=[B_pT], writes=[B_fT])
            if router_T_d is not None:
                lg, B_lg, _ = lg_r.next()
                S.op("vector", "memset", ap=lg[:], constant=0.0, writes=[B_lg])
                for e in range(8):
                    r, B_r, _ = rj.next()
                    S.op("vector", "scalar_tensor_tensor", out=r[:], in0=xt, scalar=sm[:, 2:3], in1=wrg[:, e, :], op0=ALU.mult, op1=ALU.mult,
                         accum_out=lg[:, e:e + 1], reads=[c.B_x[t], B_sm, B_wrg], writes=[B_r, B_lg])
                S.op("vector", "max", out=lg[:, 8:16], in_=lg[:, 0:8], reads=[B_lg], writes=[B_lg])
                S.op("vector", "tensor_tensor", out=lg[:, 16:17], in0=lg[:, 9:10], in1=lg[:, 8:9], op=ALU.subtract, reads=[B_lg], writes=[B_lg])
                S.op("scalar", "activation", out=lg[:, 16:17], in_=lg[:, 16:17], func=AF.Exp, reads=[B_lg], writes=[B_lg])
                S.op("vector", "tensor_scalar", out=lg[:, 17:18], in0=lg[:, 16:17], scalar1=1.0, scalar2=None, op0=ALU.add, reads=[B_lg], writes=[B_lg])
                S.op("vector", "reciprocal", out=lg[:, 17:18], in_=lg[:, 17:18], reads=[B_lg], writes=[B_lg])
                S.op("vector", "tensor_scalar", out=lg[:, 18:19], in0=lg[:, 17:18], scalar1=-1.0, scalar2=1.0, op0=ALU.mult, op1=ALU.add, reads=[B_lg], writes=[B_lg])
                S.op("vector", "tensor_scalar", out=lg[:, 20:28], in0=lg[:, 0:8], scalar1=lg[:, 8:9], scalar2=lg[:, 17:18], op0=ALU.is_equal, op1=ALU.mult,
                     reads=[B_lg], writes=[B_lg])
                S.op("vector", "tensor_scalar", out=G[:, t, :], in0=lg[:, 0:8], scalar1=lg[:, 9:10], scalar2=lg[:, 18:19], op0=ALU.is_equal, op1=ALU.mult,
                     reads=[B_lg], writes=[B_G])
                S.op("vector", "tensor_tensor", out=G[:, t, :], in0=G[:, t, :], in1=lg[:, 20:28], op=ALU.add, reads=[B_lg, B_G], writes=[B_G])
        S.barrier()
    return G, B_G


class FfnBufs:
    def __init__(self, c2):
        self.wg = Ring(c2, c2.key("wg"), [128, 8, 512], BF16, 2)
        self.wu = Ring(c2, c2.key("wu"), [128, 8, 512], BF16, 2)
        self.wd = Ring(c2, c2.key("wd"), [128, 4, D], BF16, 2)
        self.act = Ring(c2, c2.key("act"), [128, 4, TB], BF16, 2)
        self.sg = Ring(c2, c2.key("sgate"), [128, TB], F32, 3)


def swiglu_accumulate(c, fb, fT, B_fT, wg_d, wu_d, wd_d, FF, G=None, B_G=None, e=None):
    S = c.S
    g0 = 0
    while g0 < FF:
        gw = min(512, FF - g0)
        nch = gw // 128
        wg, B_wg, k1 = fb.wg.next()
        wu, B_wu, k2 = fb.wu.next()
        wd, B_wd, k3 = fb.wd.next()
        S.op("gpsimd", "dma_start", out=wg[:, :, 0:gw], in_=wg_d[:, g0:g0 + gw].rearrange("(k p) c -> p k c", p=128), writes=[B_wg], dma=k1)
        S.op("gpsimd", "dma_start", out=wu[:, :, 0:gw], in_=wu_d[:, g0:g0 + gw].rearrange("(k p) c -> p k c", p=128), writes=[B_wu], dma=k2)
        S.op("gpsimd", "dma_start", out=wd[:, 0:nch, :], in_=wd_d[g0:g0 + gw, :].rearrange("(c p) n -> p c n", p=128), writes=[B_wd], dma=k3)
        for tb in range(NTB):
            ts = slice(tb * TB, (tb + 1) * TB)
            act, B_act, _ = fb.act.next()
            for cch in range(nch):
                psg, B_psg, _ = c.psF.next()
                for k in range(8):
                    S.op("tensor", "matmul", out=psg[:], lhsT=wg[:, k, cch * 128:(cch + 1) * 128], rhs=fT[:, k, ts], start=(k == 0), stop=(k == 7),
                         reads=[B_wg, B_fT], writes=[B_psg])
                psu, B_psu, _ = c.psF.next()
                for k in range(8):
                    S.op("tensor", "matmul", out=psu[:], lhsT=wu[:, k, cch * 128:(cch + 1) * 128], rhs=fT[:, k, ts], start=(k == 0), stop=(k == 7),
                         reads=[B_wu, B_fT], writes=[B_psu])
                sg, B_sg, _ = fb.sg.next()
                S.op("scalar", "activation", out=sg[:], in_=psg[:], func=AF.Silu, reads=[B_psg], writes=[B_sg])
                S.op("vector", "tensor_tensor", out=act[:, cch, :], in0=psu[:], in1=sg[:], op=ALU.mult, reads=[B_psu, B_sg], writes=[B_act])
            for j in range(4):
                t = tb * 4 + j
                for half in range(2):
                    psy, B_psy, _ = c.psF.next()
                    for cch in range(nch):
                        S.op("tensor", "matmul", out=psy[:], lhsT=act[:, cch, j * 128:(j + 1) * 128], rhs=wd[:, cch, half * 512:(half + 1) * 512],
                             start=(cch == 0), stop=(cch == nch - 1), reads=[B_act, B_wd], writes=[B_psy])
                    xs = c.x[:, t, half * 512:(half + 1) * 512]
                    if G is None:
                        S.op("vector", "tensor_tensor", out=xs, in0=psy[:], in1=xs, op=ALU.add, reads=[B_psy, c.B_x[t]], writes=[c.B_x[t]])
                    else:
                        S.op("vector", "scalar_tensor_tensor", out=xs, in0=psy[:], scalar=G[:, t, e:e + 1], in1=xs, op0=ALU.mult, op1=ALU.add,
                             reads=[B_psy, c.B_x[t], B_G], writes=[c.B_x[t]])
        g0 += gw


def final_norm_store(c, gain_row_d, out_d):
    S = c.S
    final = []
    with contextlib.ExitStack() as st2:
        c2 = Ctx(c.nc, S, st2)
        gain, B_gain = load_bcast_row(c2, c.key("fin_gain"), gain_row_d, D)
        junk = Ring(c2, c.key("finjunk"), [128, D], BF16, 2)
        small = Ring(c2, c.key("finsmall"), [128, 4], F32, 4)
        o_r = Ring(c2, c.key("fino"), [128, D], F32, 3)
        for t in range(NT):
            jk, B_jk, _ = junk.next()
            sm, B_sm, _ = small.next()
            xt = c.x[:, t, :]
            S.op("scalar", "activation", out=jk[:], in_=xt, func=AF.Square, accum_out=sm[:, 0:1], reads=[c.B_x[t]], writes=[B_jk, B_sm])
            S.op("scalar", "activation", out=sm[:, 1:2], in_=sm[:, 0:1], func=AF.Sqrt, scale=1.0 / D, bias=c.eps_t[:, 0:1], reads=[B_sm, c.B_eps], writes=[B_sm])
            S.op("vector", "reciprocal", out=sm[:, 2:3], in_=sm[:, 1:2], reads=[B_sm], writes=[B_sm])
            o, B_o, key = o_r.next()
            S.op("vector", "scalar_tensor_tensor", out=o[:], in0=xt, scalar=sm[:, 2:3], in1=gain[:], op0=ALU.mult, op1=ALU.mult,
                 reads=[c.B_x[t], B_sm, B_gain], writes=[B_o])
            final.append(S.op("sync", "dma_start", out=out_d[t * 128:(t + 1) * 128, :], in_=o[:], reads=[B_o], dma=key))
        S.barrier()
    return final


def phase_b(c, d, layer):
    S = c.S
    fT = c.sb(c.key("fT"), [128, 8, NTOK], BF16)
    B_fT = Buf()
    wout_step(c, d["mixv"], d["w_out"], fT, B_fT)
    moe = (layer % 2 == 1)
    G, B_G = ffn_norm_step(c, d["ffn_norm"], fT, B_fT, d["router_T"] if moe else None)
    with contextlib.ExitStack() as st2:
        c2 = Ctx(c.nc, S, st2)
        c2.dkey = c.dkey + 7000
        fb = FfnBufs(c2)
        if not moe:
            swiglu_accumulate(c, fb, fT, B_fT, d["wg"], d["wu"], d["wd"], 2816)
        else:
            for e in range(8):
                swiglu_accumulate(c, fb, fT, B_fT, d["mwg"][e], d["mwu"][e], d["mwd"][e], 3584, G, B_G, e)
        S.barrier()


U32 = mybir.dt.uint32


def make_idx(core):
    b, hj = core // 4, core % 4
    p = np.arange(128, dtype=np.int64)
    idx = np.zeros((128, IDX_N), np.int64)
    for j in range(4):
        r = 4 * b + j
        idx[:, IDX_QN + j] = r * PA16A_ROWS + PA16A_BASE["QN"] + hj * 128 + p
        idx[:, IDX_KN + j] = r * PA16A_ROWS + PA16A_BASE["KN"] + hj * 128 + p
        idx[:, IDX_VT + j] = r * PA16A_ROWS + PA16A_BASE["VT"] + hj * 128 + p
        idx[:64, IDX_QP + j] = r * PA16A_ROWS + PA16A_BASE["QP"] + hj * 64 + p[:64]
        idx[:64, IDX_KPE + j] = r * PA16A_ROWS + PA16A_BASE["KPE"] + p[:64]
    for sb in range(16):
        r = 4 * b + sb // 4
        q = sb % 4
        for name, col in (("HQ", IDX_HQ), ("HIT", IDX_HIT), ("HGT", IDX_HGT)):
            idx[:, col + sb] = (r * PA16H_ROWS + PA16H_BASE[name] + hj * 128 + p) * 4 + q
        idx[:, IDX_ZF + sb] = (r * 512 + hj * 128 + p) * 4 + q
    for k in range(8):
        part, h2 = k // 4, k % 4
        idx[:, IDX_MIX + k] = ((4 * b + h2) * 256 + part * 128 + p) * 4 + hj
    return idx.astype(np.uint32)


def tagged_in(nc, name, rows, cols):
    return dram_in(nc, name, [rows + 1, cols], F32)[0:rows, :]


def tag_rows(a2d, core):
    return np.concatenate([a2d, np.full((1, a2d.shape[1]), float(core), np.float32)], axis=0)


TAGGED = ("w_in0", "w_in1", "w_q_b0", "w_q_b1", "w_kv_b0", "w_kv_b1", "w_out0", "w_out1", "wg", "wu", "wd", "mwg", "mwu", "mwd")


def build_fused():
    nc = bass.Bass("TRN2", target_bir_lowering=False)
    x_d = dram_in(nc, "x", [NTOK, D], F32)
    pos_d = dram_in(nc, "pos", [1, NTOK], I32)
    ident_d = dram_in(nc, "ident", [128, 128], BF16)
    ones_d = dram_in(nc, "ones", [128, 128], BF16)
    inv_d = dram_in(nc, "inv", [64, 1], F32)
    idx_d = dram_in(nc, "idx", [128, IDX_N], U32)
    md = {"cmask": dram_in(nc, "cmask", [128, 128], BF16), "hmask": dram_in(nc, "hmask", [64, 64], F32),
          "resetm": dram_in(nc, "resetm", [128, 512], F32), "lbraw": dram_in(nc, "lbraw", [128, 2], F32)}
    gn_d = [dram_in(nc, "gn%d" % l, [128, 1], F32) for l in range(2)]
    W = []
    for l in range(2):
        W.append({"w_in": tagged_in(nc, "w_in%d" % l, D, IN_COLS), "mix_norm": dram_in(nc, "mix_norm%d" % l, [D], F32),
                  "w_q_b": tagged_in(nc, "w_q_b%d" % l, 384, 768), "q_a_norm": dram_in(nc, "q_a_norm%d" % l, [384], F32),
                  "w_kv_b": tagged_in(nc, "w_kv_b%d" % l, 256, 1024), "kv_a_norm": dram_in(nc, "kv_a_norm%d" % l, [256], F32),
                  "w_out": tagged_in(nc, "w_out%d" % l, D, D), "ffn_norm": dram_in(nc, "ffn_norm%d" % l, [1, D], F32)})
    W[0].update({"wg": tagged_in(nc, "wg", D, 2816), "wu": tagged_in(nc, "wu", D, 2816), "wd": tagged_in(nc, "wd", 2816, D)})
    W[1].update({"router_T": dram_in(nc, "router_T", [8, D], F32),
                 "mwg": tagged_in(nc, "mwg", 8 * D, 3584).rearrange("(e r) c -> e r c", e=8),
                 "mwu": tagged_in(nc, "mwu", 8 * D, 3584).rearrange("(e r) c -> e r c", e=8),
                 "mwd": tagged_in(nc, "mwd", 8 * 3584, D).rearrange("(e r) c -> e r c", e=8)})
    fin_d = dram_in(nc, "final_norm", [1, D], F32)
    out_d = nc.dram_tensor("OUT", [NTOK, D], F32, kind="ExternalOutput").ap()
    pa16a = nc.dram_tensor("pa16a", [PA16A_ROWS, NTOK], BF16).ap()
    pa16h = nc.dram_tensor("pa16h", [PA16H_ROWS, NTOK], BF16).ap()
    pa32 = nc.dram_tensor("pa32", [512, NTOK], F32).ap()
    pa16a_all = nc.dram_tensor("pa16a_all", [8 * PA16A_ROWS, NTOK], BF16).ap()
    pa16h_all = nc.dram_tensor("pa16h_all", [8 * PA16H_ROWS, NTOK], BF16).ap()
    pa32all = nc.dram_tensor("pa32all", [8 * 512, NTOK], F32).ap()
    mix = nc.dram_tensor("mixloc", [256, SEQ], BF16).ap()
    mixall = nc.dram_tensor("mixall", [8 * 256, SEQ], BF16).ap()
    xo_d = nc.dram_tensor("xo", [NTOK, D], F32).ap()
    pav = pa_views(pa16a, pa16h, pa32)
    md["pa16v"] = pa16a_all
    md["pa16v4"] = pa16h_all.rearrange("r (q c) -> (r q) c", q=4)
    md["pa32v4"] = pa32all.rearrange("r (q c) -> (r q) c", q=4)
    md["B_pa_a"] = Buf()
    md["B_pa_h"] = Buf()
    md["B_pa_z"] = Buf()
    mixv = mixall.rearrange("r (q c) -> (r q) c", q=4)

    S = Sched(nc)
    c = new_ctx(nc, S)
    final = []
    with c.st:
        load_consts(c, ident_d, ones_d)
        c.idx = c.sb("idx", [128, IDX_N], U32)
        c.B_idx = Buf()
        S.op("sync", "dma_start", out=c.idx[:], in_=idx_d, writes=[c.B_idx], dma="idx")
        rope_tables(c, pos_d, inv_d)
        for l in range(2):
            w = W[l]
            S.scope = "L%d_phaseA" % l
            with contextlib.ExitStack() as st:
                ca = c.sub(st)
                load_phase_a_weights(ca, w["w_in"], w["mix_norm"], w["w_q_b"], w["q_a_norm"], w["w_kv_b"], w["kv_a_norm"])
                x_ring = Ring(ca, ca.key("xin"), [128, D], F32, 3)
                xsrc_d = x_d if l == 0 else xo_d

                def x_src(t, x_ring=x_ring, xsrc_d=xsrc_d):
                    xt, B_xt, key = x_ring.next()
                    S.op("sync", "dma_start", out=xt[:], in_=xsrc_d[t * 128:(t + 1) * 128, :], writes=[B_xt], dma=key)
                    return xt[:], B_xt
                phase_a(ca, x_src, pav)
                S.barrier()
            S.scope = "L%d_gatherA" % l
            S.all_gather(pa16a, pa16a_all, writes=[md["B_pa_a"]], dma=c.key("ag"))
            S.all_gather(pa16h, pa16h_all, writes=[md["B_pa_h"]], dma=c.key("ag"))
            S.all_gather(pa32, pa32all, writes=[md["B_pa_z"]], dma=c.key("ag"))
            md["gn"] = gn_d[l]
            S.scope = "L%d_attn" % l
            attention(c, md, mix[0:128, :], pfx="a%d" % l)
            S.scope = "L%d_hgrn" % l
            hgrn(c, md, mix[128:256, :], l, pfx="h%d" % l)
            S.scope = "L%d_gatherM" % l
            S.all_gather(mix, mixall, dma=c.key("ag"))
            S.barrier()
            S.scope = "L%d_phaseB" % l
            with contextlib.ExitStack() as st:
                cb = c.sub(st)
                load_x_resident(cb, x_d if l == 0 else xo_d, "xr%d" % l)
                d = dict(w)
                d["mixv"] = mixv
                phase_b(cb, d, l)
                if l == 1:
                    final = final_norm_store(cb, fin_d, out_d)
                else:
                    for t in range(NT):
                        S.op("sync", "dma_start", out=xo_d[t * 128:(t + 1) * 128, :], in_=cb.x[:, t, :], reads=[cb.B_x[t]], dma="xo%d" % t)
                    S.barrier()
        S.emit(final_waits=final)
    return nc


_PROG = {}


def _c(a):
    return np.ascontiguousarray(a)


def kernel(x, positions, mix_norm, w_in, q_a_norm, w_q_b, kv_a_norm, w_kv_b, hg_lower_bounds, hg_out_norm, w_out, ffn_norm,
           dense_w_gate, dense_w_up, dense_w_down, moe_router, moe_w_gate, moe_w_up, moe_w_down, final_norm):
    f32 = lambda a: np.asarray(a, dtype=np.float32)
    consts = const_inputs()
    mc = mixer_consts()
    xf = f32(x).reshape(-1, D)
    posf = np.asarray(positions).reshape(-1).astype(np.int32)
    shared = {"ident": consts["ident"], "ones": consts["ones"], "inv": consts["inv"], "cmask": mc["cmask"], "hmask": mc["hmask"], "resetm": mc["resetm"]}
    for l in range(2):
        shared["w_in%d" % l] = _c(f32(w_in)[l])
        shared["mix_norm%d" % l] = _c(f32(mix_norm)[l])
        shared["w_q_b%d" % l] = _c(f32(w_q_b)[l])
        shared["q_a_norm%d" % l] = _c(f32(q_a_norm)[l])
        shared["w_kv_b%d" % l] = _c(f32(w_kv_b)[l])
        shared["kv_a_norm%d" % l] = _c(f32(kv_a_norm)[l])
        shared["w_out%d" % l] = _c(f32(w_out)[l])
        shared["ffn_norm%d" % l] = _c(f32(ffn_norm)[l]).reshape(1, D)
        shared["gn%d" % l] = _c(f32(hg_out_norm)[l]).reshape(128, 1)
    shared["wg"] = _c(f32(dense_w_gate)[0])
    shared["wu"] = _c(f32(dense_w_up)[0])
    shared["wd"] = _c(f32(dense_w_down)[0])
    shared["router_T"] = _c(f32(moe_router)[0].T)
    shared["mwg"] = _c(f32(moe_w_gate)[0])
    shared["mwu"] = _c(f32(moe_w_up)[0])
    shared["mwd"] = _c(f32(moe_w_down)[0])
    shared["final_norm"] = _c(f32(final_norm)).reshape(1, D)
    lb = f32(hg_lower_bounds)
    maps = []
    for core in range(8):
        m = dict(shared)
        for n in TAGGED:
            a = shared[n]
            m[n] = tag_rows(a.reshape(-1, a.shape[-1]), core)
        m["x"] = _c(xf[core * NTOK:(core + 1) * NTOK])
        m["pos"] = _c(posf[core * NTOK:(core + 1) * NTOK]).reshape(1, NTOK)
        m["idx"] = make_idx(core)
        h = core % 4
        m["lbraw"] = _c(lb[:, h * 128:(h + 1) * 128].T)
        maps.append(m)
    if "nc" not in _PROG:
        _PROG["nc"] = build_fused()
    res = run_bass_kernel_spmd(_PROG["nc"], maps, core_ids=list(range(8)))
    out = np.concatenate([np.asarray(res.results[c]["OUT"], dtype=np.float32) for c in range(8)], axis=0)
    return out.reshape(2, SEQ, D)
```
